# Optimizing a Trainium2 kernel written in Bass

```python
import jax, jax.numpy as jnp
from jax import lax
import numpy as np

D_MODEL = 1024
BATCH = 8
SEQ = 4096
DEPTH = 1

MIX_W = D_MODEL
ATT_HEADS = 8
QK_NOPE = 64
QK_ROPE = 32
V_HEAD = 64
ATT_W = ATT_HEADS * V_HEAD
Q_RANK = D_MODEL // 2
KV_RANK = D_MODEL // 4
ROPE_THETA = 10000.0
Q_BLOCK = 128
CONV_CH = MIX_W - ATT_W
CONV_GROUPS = 8
CONV_K = 31
IN_W = Q_RANK + KV_RANK + QK_ROPE + 2 * CONV_CH
PEER_HEADS = 8
PEER_NKEYS = 128
PEER_EXPERTS = PEER_NKEYS * PEER_NKEYS
PEER_TOPK = 16
PEER_DK = 128
PEER_DK_HALF = PEER_DK // 2
PEER_BLOCK = 128
PLE_DIM = 256
EPS = 1e-6

kernel_name = "hymba_mla_conformer_peer_layer"


def rmsnorm(x, g):
    xf = x.astype(jnp.float32)
    y = xf * lax.rsqrt(jnp.mean(xf * xf, axis=-1, keepdims=True) + EPS)
    return (y * g.astype(jnp.float32)).astype(x.dtype)


def layernorm(x, g, b):
    xf = x.astype(jnp.float32)
    mu = jnp.mean(xf, axis=-1, keepdims=True)
    var = jnp.mean(jnp.square(xf - mu), axis=-1, keepdims=True)
    y = (xf - mu) * lax.rsqrt(var + EPS)
    return (y * g.astype(jnp.float32) + b.astype(jnp.float32)).astype(x.dtype)


def rope(x, positions):
    half = x.shape[-1] // 2
    freqs = ROPE_THETA ** (-jnp.arange(half, dtype=jnp.float32) / half)
    ang = positions.astype(jnp.float32)[..., None] * freqs
    cos = jnp.cos(ang)[:, :, None, :]
    sin = jnp.sin(ang)[:, :, None, :]
    xf = x.astype(jnp.float32)
    x1, x2 = xf[..., :half], xf[..., half:]
    out = jnp.concatenate([x1 * cos - x2 * sin, x2 * cos + x1 * sin], axis=-1)
    return out.astype(x.dtype)


def causal_attention(q, k, v):
    B, S, H, Dk = q.shape
    Dv = v.shape[-1]
    nb = S // Q_BLOCK
    scale = Dk ** -0.5
    qb = q.reshape(B, nb, Q_BLOCK, H, Dk).transpose(1, 0, 2, 3, 4)
    k_pos = jnp.arange(S)

    def one_block(args):
        qi, bi = args
        s = jnp.einsum('bqhd,bkhd->bhqk', qi, k).astype(jnp.float32) * scale
        q_pos = bi * Q_BLOCK + jnp.arange(Q_BLOCK)
        mask = k_pos[None, :] <= q_pos[:, None]
        s = jnp.where(mask[None, None], s, jnp.finfo(jnp.float32).min)
        w = jax.nn.softmax(s, axis=-1).astype(v.dtype)
        return jnp.einsum('bhqk,bkhd->bqhd', w, v)

    out = lax.map(one_block, (qb, jnp.arange(nb)))
    return out.transpose(1, 0, 2, 3, 4).reshape(B, S, H * Dv)


def mla_group(q_lat, kv_lat, k_pe, positions, q_norm, w_uq, kv_norm, w_ukv):
    B, S, _ = q_lat.shape
    q = (rmsnorm(q_lat, q_norm) @ w_uq).reshape(B, S, ATT_HEADS, QK_NOPE + QK_ROPE)
    q_nope, q_pe = q[..., :QK_NOPE], q[..., QK_NOPE:]
    q_pe = rope(q_pe, positions)
    kv = (rmsnorm(kv_lat, kv_norm) @ w_ukv).reshape(B, S, ATT_HEADS, QK_NOPE + V_HEAD)
    k_nope, v = kv[..., :QK_NOPE], kv[..., QK_NOPE:]
    k_pe = rope(k_pe[:, :, None, :], positions)
    k_pe = jnp.broadcast_to(k_pe, (B, S, ATT_HEADS, QK_ROPE))
    q_full = jnp.concatenate([q_nope, q_pe], axis=-1)
    k_full = jnp.concatenate([k_nope, k_pe], axis=-1)
    return causal_attention(q_full, k_full, v)


def conformer_conv_group(u, conv_w, conv_b, ln_g, ln_b):
    a, gate = jnp.split(u, 2, axis=-1)
    y = a * jax.nn.sigmoid(gate)
    y = lax.conv_general_dilated(
        y, conv_w[:, None, :].astype(y.dtype), window_strides=(1,),
        padding=[(CONV_K - 1, 0)], dimension_numbers=('NWC', 'WIO', 'NWC'),
        feature_group_count=CONV_CH) + conv_b
    y = layernorm(y, ln_g, ln_b)
    return jax.nn.silu(y)


def peer(xn, w_q, sub_keys, u_tab, v_tab):
    B, S, D = xn.shape
    T = B * S
    xt = xn.reshape(T // PEER_BLOCK, PEER_BLOCK, D)

    def one_block(xb):
        P = xb.shape[0]
        q = (xb @ w_q).reshape(P, PEER_HEADS, 2, PEER_DK_HALF)
        s = jnp.einsum('phcd,hcnd->phcn', q, sub_keys).astype(jnp.float32)
        v1, i1 = lax.top_k(s[:, :, 0], PEER_TOPK)
        v2, i2 = lax.top_k(s[:, :, 1], PEER_TOPK)
        cand = (v1[..., :, None] + v2[..., None, :]).reshape(P, PEER_HEADS, PEER_TOPK * PEER_TOPK)
        cand_idx = (i1[..., :, None] * PEER_NKEYS + i2[..., None, :]).reshape(P, PEER_HEADS, PEER_TOPK * PEER_TOPK)
        best, pos = lax.top_k(cand, PEER_TOPK)
        e = jnp.take_along_axis(cand_idx, pos, axis=-1)
        g = jax.nn.softmax(best, axis=-1)
        u = u_tab[e]
        act = jax.nn.gelu(jnp.einsum('pd,phkd->phk', xb, u).astype(jnp.float32), approximate=False)
        coef = (g * act).astype(xb.dtype)
        return jnp.einsum('phk,phkd->pd', coef, v_tab[e])

    return lax.map(one_block, xt).reshape(B, S, D)


def setup_inputs(seed: int = 0) -> dict:
    key = jax.random.key(seed)
    ks = jax.random.split(key, 32)
    f = jnp.float32

    def nrm(k, shape, scale):
        return jax.random.normal(k, shape, f) * scale

    def gain(k, shape):
        return 1.0 + 0.02 * jax.random.normal(k, shape, f)

    L = DEPTH
    x = jax.random.normal(ks[0], (BATCH, SEQ, D_MODEL), f)
    p = jax.random.normal(ks[1], (DEPTH, BATCH, SEQ, PLE_DIM), f)
    offs = jax.random.randint(ks[2], (BATCH, 1), 0, 1024, dtype=jnp.int32)
    positions = offs + jnp.arange(SEQ, dtype=jnp.int32)[None, :]
    return {
        "x": x,
        "p": p,
        "positions": positions,
        "attn_norm": gain(ks[3], (L, D_MODEL)),
        "w_in": nrm(ks[4], (L, D_MODEL, IN_W), D_MODEL ** -0.5),
        "q_norm": gain(ks[5], (L, Q_RANK)),
        "w_uq": nrm(ks[6], (L, Q_RANK, ATT_HEADS * (QK_NOPE + QK_ROPE)), Q_RANK ** -0.5),
        "kv_norm": gain(ks[7], (L, KV_RANK)),
        "w_ukv": nrm(ks[8], (L, KV_RANK, ATT_HEADS * (QK_NOPE + V_HEAD)), KV_RANK ** -0.5),
        "conv_w": nrm(ks[9], (L, CONV_K, CONV_CH), CONV_K ** -0.5),
        "conv_b": nrm(ks[10], (L, CONV_CH), 0.02),
        "conv_ln_g": gain(ks[11], (L, CONV_CH)),
        "conv_ln_b": nrm(ks[12], (L, CONV_CH), 0.02),
        "attn_out_norm": gain(ks[13], (L, ATT_W)),
        "conv_out_norm": gain(ks[14], (L, CONV_CH)),
        "w_out": nrm(ks[15], (L, MIX_W, D_MODEL), MIX_W ** -0.5),
        "ffn_norm": gain(ks[16], (L, D_MODEL)),
        "peer_wq": nrm(ks[17], (L, D_MODEL, PEER_HEADS * PEER_DK), D_MODEL ** -0.5),
        "peer_keys": nrm(ks[18], (L, PEER_HEADS, 2, PEER_NKEYS, PEER_DK_HALF), PEER_DK_HALF ** -0.5),
        "peer_u": nrm(ks[19], (L, PEER_EXPERTS, D_MODEL), D_MODEL ** -0.5),
        "peer_v": nrm(ks[20], (L, PEER_EXPERTS, D_MODEL), (PEER_HEADS ** -0.5)),
        "pl_norm": gain(ks[21], (L, D_MODEL)),
        "pl_gate_w": nrm(ks[22], (L, D_MODEL, D_MODEL), D_MODEL ** -0.5),
        "pl_gate_b": nrm(ks[23], (L, D_MODEL), 0.02),
        "pl_proj": nrm(ks[24], (L, PLE_DIM, D_MODEL), PLE_DIM ** -0.5),
        "final_norm": gain(ks[25], (D_MODEL,)),
    }


def reference(x, p, positions, attn_norm, w_in, q_norm, w_uq, kv_norm, w_ukv,
              conv_w, conv_b, conv_ln_g, conv_ln_b, attn_out_norm, conv_out_norm,
              w_out, ffn_norm, peer_wq, peer_keys, peer_u, peer_v,
              pl_norm, pl_gate_w, pl_gate_b, pl_proj, final_norm):
    h = x
    split_pts = [Q_RANK, Q_RANK + KV_RANK, Q_RANK + KV_RANK + QK_ROPE]
    for i in range(DEPTH):
        hn = rmsnorm(h, attn_norm[i])
        z = hn @ w_in[i]
        q_lat, kv_lat, k_pe, conv_in = jnp.split(z, split_pts, axis=-1)
        att = mla_group(q_lat, kv_lat, k_pe, positions, q_norm[i], w_uq[i], kv_norm[i], w_ukv[i])
        cnv = conformer_conv_group(conv_in, conv_w[i], conv_b[i], conv_ln_g[i], conv_ln_b[i])
        mix = jnp.concatenate([rmsnorm(att, attn_out_norm[i]), rmsnorm(cnv, conv_out_norm[i])], axis=-1)
        h = h + mix @ w_out[i]
        h = h + peer(rmsnorm(h, ffn_norm[i]), peer_wq[i], peer_keys[i], peer_u[i], peer_v[i])
        gate = jax.nn.sigmoid(rmsnorm(h, pl_norm[i]) @ pl_gate_w[i] + pl_gate_b[i])
        h = h + gate * (p[i] @ pl_proj[i])
    return rmsnorm(h, final_norm)
```

```python
import contextlib
import numpy as np
import concourse.bass as bass
import concourse.mybir as mybir
from concourse.bass_utils import run_bass_kernel_spmd

F32 = mybir.dt.float32
BF16 = mybir.dt.bfloat16
I32 = mybir.dt.int32
U32 = mybir.dt.uint32
AF = mybir.ActivationFunctionType
ALU = mybir.AluOpType
AX = mybir.AxisListType

T = 4096
D = 1024
NT = 32
NCH = 8
EPS = 1e-6
ATT_SCALE = 96 ** -0.5
ENGS = ["pe", "act", "dve", "pool", "sp"]
SAME_ENGINE_RAW = True
DOT_SPLIT = False

C_GA = 0
C_GQ = 8
C_GKV = 12
C_CB = 14
C_LNG = 18
C_LNB = 22
C_CG = 26
C_GAO = 30
C_GPL = 34
C_FREQ = 42
C_CW = 44
NCOL = C_CW + 4 * 31


class Sched:
    def __init__(self, nc):
        self.nc = nc
        self.ops = {e: [] for e in ENGS}
        self.res = {}
        self.seen = {e: {} for e in ENGS}
        self.chan_n = {}
        self.pending = {e: [] for e in ENGS}
        self.final_waits = []

    def _need(self, eng, prod, waits):
        if prod is None:
            return
        kind, key, n = prod
        if kind == "e" and key == eng:
            if key == "pe" or not SAME_ENGINE_RAW:
                return
        k = (kind, key)
        if self.seen[eng].get(k, -1) >= n:
            return
        self.seen[eng][k] = n
        waits.append(prod)

    def _deps(self, eng, reads, writes, waits):
        for r in reads:
            st = self.res.setdefault(r, {"w": None, "r": {}})
            self._need(eng, st["w"], waits)
        for w in writes:
            st = self.res.setdefault(w, {"w": None, "r": {}})
            p = st["w"]
            if p is not None and not (p[0] == "e" and p[1] == eng):
                self._need(eng, p, waits)
            for k, rp in st["r"].items():
                if rp[0] == "e" and rp[1] == eng:
                    continue
                self._need(eng, rp, waits)

    def _commit(self, tok, reads, writes):
        for r in reads:
            self.res[r]["r"][(tok[0], tok[1])] = tok
        for w in writes:
            st = self.res[w]
            st["w"] = tok
            st["r"] = {}

    def _take_pending(self, eng):
        waits = list(self.pending[eng])
        self.pending[eng] = []
        for p in waits:
            k = (p[0], p[1])
            self.seen[eng][k] = max(self.seen[eng].get(k, -1), p[2])
        return waits

    def op(self, eng, fn, reads=(), writes=()):
        waits = self._take_pending(eng)
        self._deps(eng, reads, writes, waits)
        idx = len(self.ops[eng])
        self.ops[eng].append({"fn": fn, "waits": waits, "sig": False, "dma": None})
        self._commit(("e", eng, idx), reads, writes)

    def dma(self, eng, fn, ch, reads=(), writes=(), final=False):
        waits = self._take_pending(eng)
        n = self.chan_n.get(ch, 0)
        if n > 0:
            self._need(eng, ("d", ch, n), waits)
        self._deps(eng, reads, writes, waits)
        n += 1
        self.chan_n[ch] = n
        self.ops[eng].append({"fn": fn, "waits": waits, "sig": False, "dma": ch})
        self._commit(("d", ch, n), reads, writes)
        if final and ch not in self.final_waits:
            self.final_waits.append(ch)

    def barrier(self):
        prods = []
        for e in ENGS:
            for i in range(len(self.ops[e]) - 1, -1, -1):
                if self.ops[e][i]["dma"] is None:
                    prods.append(("e", e, i))
                    break
        for ch, n in self.chan_n.items():
            if not ch.startswith("tb"):
                prods.append(("d", ch, n))
        for e in ENGS:
            self.pending[e] = [p for p in prods if not (p[0] == "e" and p[1] == e)]
        self.res = {k: v for k, v in self.res.items() if k.startswith("tab")}

    def emit(self):
        nc = self.nc
        for e in ENGS:
            for o in self.ops[e]:
                for (kind, key, n) in o["waits"]:
                    if kind == "e":
                        self.ops[key][n]["sig"] = True
        cnt = {}
        for e in ENGS:
            c = 0
            for o in self.ops[e]:
                if o["sig"]:
                    c += 1
                o["cnt"] = c
            cnt[e] = c
        with contextlib.ExitStack() as st:
            esem = {e: st.enter_context(nc.semaphore("s_" + e)) for e in ENGS if cnt[e] > 0}
            csem = {ch: st.enter_context(nc.semaphore("c_%s" % (ch,))) for ch in self.chan_n}
            block = st.enter_context(nc.Block())
            handles = {"pe": block.tensor, "act": block.scalar, "dve": block.vector,
                       "pool": block.gpsimd, "sp": block.sync}

            def make(e):
                def body(eng):
                    for o in self.ops[e]:
                        for (kind, key, n) in o["waits"]:
                            if kind == "e":
                                eng.wait_ge(esem[key], self.ops[key][n]["cnt"])
                            else:
                                eng.wait_ge(csem[key], 16 * n)
                        ins = o["fn"](eng)
                        if o["dma"] is not None:
                            ins.then_inc(csem[o["dma"]], 16)
                        elif o["sig"]:
                            ins.then_inc(esem[e], 1)
                    if e == "sp":
                        for ch in self.final_waits:
                            eng.wait_ge(csem[ch], 16 * self.chan_n[ch])
                return body

            for e in ENGS:
                if self.ops[e] or e == "sp":
                    handles[e](make(e))
        return {e: len(self.ops[e]) for e in ENGS}, cnt


class Arena:
    def __init__(self, base_ap, nbytes):
        self.base = base_ap
        self.nbytes = nbytes
        self.allocs = []

    def alloc(self, shape, dt, phases):
        esz = {F32: 4, BF16: 2, I32: 4, U32: 4}[dt]
        n = 1
        for s in shape[1:]:
            n *= s
        size = (n * esz + 31) // 32 * 32
        phases = set(phases)
        cands = sorted({0} | {o + s for (o, s, p) in self.allocs})
        for off in cands:
            ok = off + size <= self.nbytes
            if ok:
                for (o, s, p) in self.allocs:
                    if p & phases and off < o + s and o < off + size:
                        ok = False
                        break
            if ok:
                self.allocs.append((off, size, phases))
                v = self.base[:, off // 4:(off + size) // 4]
                if dt != F32:
                    v = v.bitcast(dt)
                v = v[:, 0:n]
                if len(shape) == 3:
                    v = v.rearrange("p (a b) -> p a b", a=shape[1])
                elif len(shape) == 4:
                    v = v.rearrange("p (a b c) -> p a b c", a=shape[1], b=shape[2])
                return v
        raise RuntimeError("arena full: %s %s %s" % (shape, dt, phases))


def build_program(upto=4, debug=False):
    nc = bass.Bass("TRN2", target_bir_lowering=False)

    def din(name, shape, dt=F32):
        return nc.dram_tensor(name, shape, dt, kind="ExternalInput").ap()

    x_d = din("x", [T, D])
    p_d = din("p", [T, 256])
    pos_d = din("pos", [32, T], I32)
    w_in_d = din("w_in", [D, 1824])
    w_uq_d = din("w_uq", [512, 768])
    w_ukv_d = din("w_ukv", [256, 1024])
    w_out_d = din("w_out", [D, D])
    wq_d = din("peer_wq", [D, D])
    keysT_d = din("keysT", [128, 8, 128])
    u_d = din("peer_u", [16384, D])
    v_d = din("peer_v", [16384, D])
    gw_d = din("pl_gate_w", [D, D])
    pw_d = din("pl_proj", [256, D])
    cols_d = din("cols", [128, NCOL])
    rows_d = din("rows", [3, 128, D])
    cst_d = din("cst", [3, 128, 128])
    out_d = nc.dram_tensor("out", [T, D], F32, kind="ExternalOutput").ap()
    tab_d = nc.dram_tensor("peer_tab", [16384, 2 * D], BF16, kind="Internal").ap()
    dbg_d = None
    if debug:
        dbg_d = {
            "d_qnT": nc.dram_tensor("d_qnT", [128, 4 * T], BF16, kind="ExternalOutput").ap(),
            "d_kvnT": nc.dram_tensor("d_kvnT", [128, 2 * T], BF16, kind="ExternalOutput").ap(),
            "d_kpeT": nc.dram_tensor("d_kpeT", [128, T], BF16, kind="ExternalOutput").ap(),
            "d_V": nc.dram_tensor("d_V", [128, NT * 8 * 65], BF16, kind="ExternalOutput").ap(),
            "d_att": nc.dram_tensor("d_att", [128, NT * 512], BF16, kind="ExternalOutput").ap(),
            "d_cnT": nc.dram_tensor("d_cnT", [128, 4 * T], BF16, kind="ExternalOutput").ap(),
        }

    ARENA_BYTES = 212800
    with contextlib.ExitStack() as st:
        arena_t = st.enter_context(nc.sbuf_tensor("arena", [128, ARENA_BYTES // 4], F32))
        banks = [st.enter_context(nc.psum_tensor("bank%d" % i, [128, 512], F32)) for i in range(8)]
        A = Arena(arena_t[:, :], ARENA_BYTES)
        S = Sched(nc)
        ALLP = {1, 2, 3, 4}

        def bk(i):
            return banks[i][:, :]

        def bkb(i):
            return banks[i][:, :].bitcast(BF16)

        cols = A.alloc([128, NCOL], F32, ALLP)
        ident = A.alloc([128, 128], BF16, ALLP)
        tri = A.alloc([128, 128], BF16, {1, 2})
        ones = A.alloc([128, 128], BF16, {1, 2, 3})
        iota16 = A.alloc([128, 16], F32, ALLP)
        scr = A.alloc([128, 64], F32, ALLP)

        S.dma("sp", lambda e: e.dma_start(out=cols, in_=cols_d), "c0", writes=["cols"])
        S.dma("pool", lambda e: e.dma_start(out=ident, in_=cst_d[0]), "c1", writes=["ident"])
        S.dma("pool", lambda e: e.dma_start(out=tri, in_=cst_d[1]), "c2", writes=["tri"])
        S.dma("sp", lambda e: e.dma_start(out=iota16, in_=cst_d[2][:, 0:16]), "c3", writes=["iota16"])
        S.op("dve", lambda e: e.memset(ones, 1.0), writes=["ones"])

        def col(c0, n=1):
            return cols[:, c0:c0 + n]

        att = A.alloc([128, NT, 512], BF16, {2, 3, 4})
        cnT = A.alloc([128, 4, T], BF16, {3, 4})
        qnT = A.alloc([128, 4, T], BF16, {1, 2})
        kvnT = A.alloc([128, 2, T], BF16, {1, 2})
        kpeT = A.alloc([128, T], BF16, {1, 2})
        Vt = A.alloc([128, NT, 8, 65], BF16, {1, 2})
        cosT = A.alloc([128, T], BF16, {1, 2})
        sinT = A.alloc([128, T], BF16, {1, 2})
        w_uq = A.alloc([128, 4, 768], BF16, {1, 2})
        w_uqr = A.alloc([128, 4, 8, 96], BF16, {1, 2})
        w_ukv = A.alloc([128, 2, 1024], BF16, {1, 2})

        def rstd_from_ss(ss_ap, n_feat, out_ap, tag):
            S.op("act", lambda e: e.activation(out=out_ap, in_=ss_ap, func=AF.Sqrt, scale=1.0 / n_feat, bias=EPS),
                 reads=[tag + "_ss"], writes=[tag + "_r"])
            S.op("dve", lambda e: e.reciprocal(out=out_ap, in_=out_ap), reads=[tag + "_r"], writes=[tag + "_r"])

        def x_to_hnT(c, xt, xsb, junkb, hnT, ssx, hname="hnT"):
            for t4 in range(4):
                g = 4 * c + t4
                par = g % 2
                S.dma("sp", lambda e, g=g, par=par: e.dma_start(out=xt[par], in_=x_d[g * 128:(g + 1) * 128, :]),
                      "x%d" % par, writes=["xt%d" % par])
                S.op("act", lambda e, par=par: e.activation(out=junkb, in_=xt[par], func=AF.Square,
                                                            accum_out=ssx[:, par:par + 1]),
                     reads=["xt%d" % par], writes=["junkb", "x%d_ss" % par])
                rstd_from_ss(ssx[:, par:par + 1], D, ssx[:, 2 + par:3 + par], "x%d" % par)
                S.op("dve", lambda e, par=par: e.tensor_scalar(out=xsb, in0=xt[par], scalar1=ssx[:, 2 + par:3 + par],
                                                               scalar2=None, op0=ALU.mult),
                     reads=["xt%d" % par, "x%d_r" % par], writes=["xsb"])
                Tb = bkb(0).rearrange("p (a b) -> p a b", a=8)
                for kc in range(8):
                    S.op("pe", lambda e, kc=kc: e.transpose(out=Tb[:, kc, :], in_=xsb[:, kc * 128:(kc + 1) * 128],
                                                            identity=ident),
                         reads=["xsb", "ident"], writes=["b0"])
                S.op("dve", lambda e, t4=t4: e.tensor_tensor(
                    out=hnT[:, :, t4 * 128:(t4 + 1) * 128], in0=Tb,
                    in1=col(C_GA, 8).unsqueeze(2).to_broadcast([128, 8, 128]), op=ALU.mult),
                    reads=["b0", "cols"], writes=[hname])

        def norm_fm(zbanks, n_oc, n_feat, gcol, dst, c, sq, sd, tag):
            for oc in range(n_oc):
                S.op("act", lambda e, oc=oc: e.activation(out=sq[:, oc, :], in_=bk(zbanks[oc]), func=AF.Square),
                     reads=["b%d" % zbanks[oc]], writes=["sq"])
            for oc in range(n_oc):
                S.op("pe", lambda e, oc=oc: e.matmul(bk(5), lhsT=ones, rhs=sq[:, oc, :], start=(oc == 0),
                                                     stop=(oc == n_oc - 1)),
                     reads=["sq", "ones"], writes=["b5"])
            S.op("act", lambda e: e.activation(out=sd, in_=bk(5), func=AF.Sqrt, scale=1.0 / n_feat, bias=EPS),
                 reads=["b5"], writes=["sd"])
            S.op("dve", lambda e: e.reciprocal(out=sd, in_=sd), reads=["sd"], writes=["sd"])
            for oc in range(n_oc):
                S.op("dve", lambda e, oc=oc: e.scalar_tensor_tensor(
                    out=dst[:, oc, c * 512:(c + 1) * 512], in0=bk(zbanks[oc]), scalar=col(gcol + oc), in1=sd,
                    op0=ALU.mult, op1=ALU.mult),
                    reads=["b%d" % zbanks[oc], "sd", "cols"], writes=[tag])

        w_in1 = A.alloc([128, 8, 800], BF16, {1})
        wkpe = A.alloc([128, 8, 96], BF16, {1})
        wkper = A.alloc([128, 8, 96], BF16, {1})
        xt = [A.alloc([128, D], F32, {1}), A.alloc([128, D], F32, {1})]
        xsb = A.alloc([128, D], BF16, {1})
        junkb = A.alloc([128, D], BF16, {1})
        hnT2 = [A.alloc([128, 8, 512], BF16, {1}) for _ in range(2)]
        sq = A.alloc([128, 4, 512], BF16, {1})
        sd = A.alloc([128, 512], F32, {1})
        tmpa = A.alloc([128, 512], F32, {1})
        tmpb = A.alloc([128, 512], F32, {1})
        ssx = scr[:, 0:4]

        w_in_v = w_in_d.rearrange("(k p) c -> p k c", p=128)
        S.dma("pool", lambda e: e.dma_start(out=w_in1, in_=w_in_v[:, :, 0:800]), "w0", writes=["w_in1"])
        S.op("dve", lambda e: e.memset(wkpe, 0.0), writes=["wkpe"])
        S.op("dve", lambda e: e.memset(wkper, 0.0), writes=["wkper"])
        S.dma("pool", lambda e: e.dma_start(out=wkpe[:, :, 64:96], in_=w_in_v[:, :, 768:800]), "w1", writes=["wkpe"])
        S.dma("pool", lambda e: e.dma_start(out=wkper[:, :, 64:80], in_=w_in_v[:, :, 784:800]), "w2", writes=["wkper"])
        S.dma("pool", lambda e: e.dma_start(out=wkper[:, :, 80:96], in_=w_in_v[:, :, 768:784]), "w3", writes=["wkper"])
        S.op("dve", lambda e: e.tensor_scalar(out=wkper[:, :, 64:80], in0=wkper[:, :, 64:80], scalar1=-1.0,
                                              scalar2=None, op0=ALU.mult), reads=["wkper"], writes=["wkper"])
        S.dma("pool", lambda e: e.dma_start(out=w_uq, in_=w_uq_d.rearrange("(k p) c -> p k c", p=128)), "w4",
              writes=["w_uq"])
        S.dma("pool", lambda e: e.dma_start(out=w_ukv, in_=w_ukv_d.rearrange("(k p) c -> p k c", p=128)), "w5",
              writes=["w_ukv"])
        S.op("dve", lambda e: e.memset(w_uqr, 0.0), writes=["w_uqr"])
        w_uq_h = w_uq.rearrange("p k (h c) -> p k h c", h=8)
        S.op("dve", lambda e: e.tensor_scalar(out=w_uqr[:, :, :, 64:80], in0=w_uq_h[:, :, :, 80:96], scalar1=-1.0,
                                              scalar2=None, op0=ALU.mult), reads=["w_uq", "w_uqr"], writes=["w_uqr"])
        S.op("dve", lambda e: e.tensor_copy(out=w_uqr[:, :, :, 80:96], in_=w_uq_h[:, :, :, 64:80]),
             reads=["w_uq", "w_uqr"], writes=["w_uqr"])
        for k in range(8):
            rs = slice(k * 2048, (k + 1) * 2048)
            S.dma("pool", lambda e, rs=rs: e.dma_start(out=tab_d[rs, 0:D], in_=u_d[rs, :]), "tbu%d" % k, writes=["tabu%d" % k])
            S.dma("pool", lambda e, rs=rs: e.dma_start(out=tab_d[rs, D:2 * D], in_=v_d[rs, :]), "tbv%d" % k, writes=["tabv%d" % k])
        S.op("dve", lambda e: e.memset(Vt[:, :, :, 64:65], 1.0), writes=["Vones"])

        posi = xt[0].bitcast(I32)
        ya = xt[1]
        ki = tmpa.bitcast(I32)
        R = slice(64, 96)
        for blk in range(8):
            cs = slice(blk * 512, (blk + 1) * 512)
            S.dma("sp", lambda e, cs=cs: e.dma_start(out=posi[R, 0:512], in_=pos_d[:, cs]), "x0", writes=["posi"])
            S.op("dve", lambda e: e.tensor_copy(out=ya[R, 0:512], in_=posi[R, 0:512]), reads=["posi"], writes=["ya"])
            S.op("dve", lambda e: e.tensor_scalar(out=ya[R, 0:512], in0=ya[R, 0:512], scalar1=cols[R, C_FREQ:C_FREQ + 1],
                                                  scalar2=1.0 / (2 * np.pi), op0=ALU.mult, op1=ALU.mult),
                 reads=["ya", "cols"], writes=["ya"])
            for which, tab, shift in (("s", sinT, 0.0), ("c", cosT, 0.25)):
                if shift:
                    S.op("dve", lambda e: e.tensor_scalar(out=ya[R, 512:1024], in0=ya[R, 0:512], scalar1=0.25,
                                                          scalar2=None, op0=ALU.add), reads=["ya"], writes=["yb"])
                    src = ya[R, 512:1024]
                    rname = "yb"
                else:
                    src = ya[R, 0:512]
                    rname = "ya"
                S.op("dve", lambda e, src=src: e.tensor_copy(out=ki[R, :], in_=src), reads=[rname], writes=["ki"])
                S.op("dve", lambda e: e.tensor_copy(out=tmpb[R, :], in_=ki[R, :]), reads=["ki"], writes=["tmpb"])
                S.op("dve", lambda e, src=src: e.tensor_tensor(out=tmpb[R, :], in0=src, in1=tmpb[R, :], op=ALU.subtract),
                     reads=[rname, "tmpb"], writes=["tmpb"])
                S.op("act", lambda e, tab=tab, cs=cs: e.activation(out=tab[R, cs], in_=tmpb[R, :], func=AF.Sin,
                                                                   scale=6.28318),
                     reads=["tmpb"], writes=["rope" + which])

        S.barrier()
        if upto >= 1:
            for c in range(NCH):
                hnT = hnT2[c % 2]
                HN = "hnT%d" % (c % 2)
                x_to_hnT(c, xt, xsb, junkb, hnT, ssx, HN)
                for oc in range(4):
                    for kc in range(8):
                        S.op("pe", lambda e, oc=oc, kc=kc, hnT=hnT: e.matmul(bk(1 + oc), lhsT=w_in1[:, kc, oc * 128:(oc + 1) * 128],
                                                                    rhs=hnT[:, kc, :], start=(kc == 0), stop=(kc == 7)),
                             reads=[HN, "w_in1"], writes=["b%d" % (1 + oc)])
                norm_fm([1, 2, 3, 4], 4, 512, C_GQ, qnT, c, sq, sd, "qnT")
                for oc in range(2):
                    for kc in range(8):
                        S.op("pe", lambda e, oc=oc, kc=kc, hnT=hnT: e.matmul(bk(1 + oc), lhsT=w_in1[:, kc, 512 + oc * 128:512 + (oc + 1) * 128],
                                                                    rhs=hnT[:, kc, :], start=(kc == 0), stop=(kc == 7)),
                             reads=[HN, "w_in1"], writes=["b%d" % (1 + oc)])
                norm_fm([1, 2], 2, 256, C_GKV, kvnT, c, sq, sd, "kvnT")
                for kc in range(8):
                    S.op("pe", lambda e, kc=kc, hnT=hnT: e.matmul(bk(6)[0:96, :], lhsT=wkpe[:, kc, :], rhs=hnT[:, kc, :],
                                                         start=(kc == 0), stop=(kc == 7)),
                         reads=[HN, "wkpe"], writes=["b6"])
                for kc in range(8):
                    S.op("pe", lambda e, kc=kc, hnT=hnT: e.matmul(bk(7)[0:96, :], lhsT=wkper[:, kc, :], rhs=hnT[:, kc, :],
                                                         start=(kc == 0), stop=(kc == 7)),
                         reads=[HN, "wkper"], writes=["b7"])
                cs = slice(c * 512, (c + 1) * 512)
                S.op("dve", lambda e, cs=cs: e.tensor_tensor(out=tmpa[R, :], in0=bk(6)[R, :], in1=cosT[R, cs], op=ALU.mult),
                     reads=["b6", "ropec"], writes=["tmpa"])
                S.op("dve", lambda e, cs=cs: e.tensor_tensor(out=tmpb[R, :], in0=bk(7)[R, :], in1=sinT[R, cs], op=ALU.mult),
                     reads=["b7", "ropes"], writes=["tmpb"])
                S.op("dve", lambda e, cs=cs: e.tensor_tensor(out=kpeT[R, cs], in0=tmpa[R, :], in1=tmpb[R, :], op=ALU.add),
                     reads=["tmpa", "tmpb"], writes=["kpeT"])
                w_ukv_h = w_ukv.rearrange("p k (h c) -> p k h c", h=8)
                for t4 in range(4):
                    g = 4 * c + t4
                    vb = 6 + (t4 % 2)
                    for kc in range(2):
                        S.op("pe", lambda e, kc=kc, g=g, vb=vb: e.matmul(
                            bk(vb).rearrange("p (h c) -> p h c", h=8), lhsT=kvnT[:, kc, g * 128:(g + 1) * 128],
                            rhs=w_ukv_h[:, kc, :, 64:128], start=(kc == 0), stop=(kc == 1)),
                            reads=["kvnT", "w_ukv"], writes=["b%d" % vb])
                    S.op("act", lambda e, g=g, vb=vb: e.copy(out=Vt[:, g, :, 0:64],
                                                            in_=bk(vb).rearrange("p (h c) -> p h c", h=8)),
                         reads=["b%d" % vb], writes=["V"])
        S.barrier()

        QT = [A.alloc([128, T], BF16, {2}), A.alloc([128, T], BF16, {2})]
        KT = [A.alloc([128, T], BF16, {2}), A.alloc([128, T], BF16, {2})]
        NPT = 5
        PT = [A.alloc([128, 512], BF16, {2}) for _ in range(NPT)]
        sqq = A.alloc([128, 512], BF16, {2})
        t2a = A.alloc([128, 512], F32, {2})
        t2b = A.alloc([128, 512], F32, {2})
        mxq = A.alloc([128, 16], F32, {2})
        negm = A.alloc([128, 8], F32, {2})
        rec = A.alloc([128, 8], F32, {2})
        w_ukv_h = w_ukv.rearrange("p k (h c) -> p k h c", h=8)

        def build_qk(h):
            hp = h % 2
            qt, kt = QT[hp], KT[hp]
            S.op("pool", lambda e: e.tensor_copy(out=kt[R, :], in_=kpeT[R, :]), reads=["kpeT"], writes=["KT%d" % hp])
            for c in range(NCH):
                cs = slice(c * 512, (c + 1) * 512)
                for kc in range(4):
                    S.op("pe", lambda e, kc=kc, cs=cs: e.matmul(bk(0)[0:96, :], lhsT=w_uq[:, kc, h * 96:(h + 1) * 96],
                                                                rhs=qnT[:, kc, cs], start=(kc == 0), stop=(kc == 3)),
                         reads=["qnT", "w_uq"], writes=["b0"])
                for kc in range(4):
                    S.op("pe", lambda e, kc=kc, cs=cs: e.matmul(bk(1)[0:96, :], lhsT=w_uqr[:, kc, h, :],
                                                                rhs=qnT[:, kc, cs], start=(kc == 0), stop=(kc == 3)),
                         reads=["qnT", "w_uqr"], writes=["b1"])
                for kc in range(2):
                    S.op("pe", lambda e, kc=kc, cs=cs: e.matmul(bk(2)[0:64, :], lhsT=w_ukv_h[:, kc, h, 0:64],
                                                                rhs=kvnT[:, kc, cs], start=(kc == 0), stop=(kc == 1)),
                         reads=["kvnT", "w_ukv"], writes=["b2"])
                S.op("act", lambda e, cs=cs: e.copy(out=qt[0:64, cs], in_=bk(0)[0:64, :]), reads=["b0"],
                     writes=["QT%d" % hp])
                S.op("dve", lambda e, cs=cs: e.tensor_tensor(out=t2a[R, :], in0=bk(0)[R, :], in1=cosT[R, cs], op=ALU.mult),
                     reads=["b0", "ropec"], writes=["t2a"])
                S.op("dve", lambda e, cs=cs: e.tensor_tensor(out=t2b[R, :], in0=bk(1)[R, :], in1=sinT[R, cs], op=ALU.mult),
                     reads=["b1", "ropes"], writes=["t2b"])
                S.op("dve", lambda e, cs=cs: e.tensor_tensor(out=qt[R, cs], in0=t2a[R, :], in1=t2b[R, :], op=ALU.add),
                     reads=["t2a", "t2b"], writes=["QT%d" % hp])
                S.op("act", lambda e, cs=cs: e.copy(out=kt[0:64, cs], in_=bk(2)[0:64, :]), reads=["b2"],
                     writes=["KT%d" % hp])
                for which, src, col0 in (("q", qt, 0), ("k", kt, 8)):
                    S.op("act", lambda e, src=src, cs=cs: e.activation(out=sqq[0:96, :], in_=src[0:96, cs], func=AF.Square),
                         reads=[("QT%d" if which == "q" else "KT%d") % hp], writes=["sqq"])
                    S.op("pe", lambda e: e.matmul(bk(2), lhsT=ones[0:96, :], rhs=sqq[0:96, :], start=True, stop=True),
                         reads=["sqq", "ones"], writes=["b2"])
                    S.op("dve", lambda e, col0=col0, c=c: e.reduce_max(out=mxq[:, col0 + c:col0 + c + 1], in_=bk(2), axis=AX.X),
                         reads=["b2"], writes=["mxq"])
            S.op("dve", lambda e: e.reduce_max(out=scr[:, 8:9], in_=mxq[:, 0:8], axis=AX.X), reads=["mxq"], writes=["scr8"])
            S.op("dve", lambda e: e.reduce_max(out=scr[:, 9:10], in_=mxq[:, 8:16], axis=AX.X), reads=["mxq"], writes=["scr9"])
            S.op("dve", lambda e: e.tensor_tensor(out=scr[:, 10:11], in0=scr[:, 8:9], in1=scr[:, 9:10], op=ALU.mult),
                 reads=["scr8", "scr9"], writes=["scr10"])
            S.op("act", lambda e: e.activation(out=scr[:, 11:12], in_=scr[:, 10:11], func=AF.Sqrt), reads=["scr10"],
                 writes=["scr11"])
            S.op("dve", lambda e: e.tensor_scalar(out=negm[:, h:h + 1], in0=scr[:, 11:12], scalar1=-ATT_SCALE, scalar2=None,
                                                  op0=ALU.mult), reads=["scr11"], writes=["negm%d" % h])

        def attn(h):
            hp = h % 2
            qt, kt = QT[hp], KT[hp]
            steps = []
            for j in range(NCH):
                for i in range(4 * j + 4):
                    steps.append((j, i))

            def emit_S(n):
                j, i = steps[n]
                q0 = max(512 * j, 128 * i)
                w = 512 * j + 512 - q0
                sb_ = 3 + (n % 3)
                pt = PT[n % NPT]
                ptn = "PT%d" % (n % NPT)
                S.op("pe", lambda e: e.matmul(bk(sb_)[:, 0:w], lhsT=kt[0:96, i * 128:(i + 1) * 128], rhs=qt[0:96, q0:q0 + w],
                                              start=True, stop=True),
                     reads=["QT%d" % hp, "KT%d" % hp], writes=["b%d" % sb_])
                S.op("act", lambda e: e.activation(out=pt[:, 0:w], in_=bk(sb_)[:, 0:w], func=AF.Exp, scale=ATT_SCALE,
                                                   bias=negm[:, h:h + 1]),
                     reads=["b%d" % sb_, "negm%d" % h], writes=[ptn])
                if 128 * i >= 512 * j:
                    S.op("pool", lambda e: e.tensor_tensor(out=pt[:, 0:128], in0=pt[:, 0:128], in1=tri, op=ALU.mult),
                         reads=[ptn, "tri"], writes=[ptn])

            def emit_PV(n):
                j, i = steps[n]
                q0 = max(512 * j, 128 * i)
                r0 = (q0 - 512 * j) // 128
                pt = PT[n % NPT]
                ptn = "PT%d" % (n % NPT)
                ob = 6 + (j % 2)
                O = bk(ob)[:, 0:260].rearrange("p (r c) -> p r c", r=4)
                for rr in range(r0, 4):
                    S.op("pe", lambda e, rr=rr: e.matmul(O[:, rr, :], lhsT=pt[:, (rr - r0) * 128:(rr - r0 + 1) * 128], rhs=Vt[:, i, h, :],
                                                         start=(i == 0 and rr == 0), stop=(i == 4 * j + 3 and rr == 3)),
                         reads=[ptn, "V", "Vones"], writes=["b%d" % ob])
                if i == 4 * j + 3:
                    S.op("dve", lambda e: e.reciprocal(out=rec[:, 0:4], in_=O[:, :, 64]), reads=["b%d" % ob], writes=["rec"])
                    S.op("dve", lambda e: e.tensor_tensor(out=att[:, 4 * j:4 * j + 4, h * 64:(h + 1) * 64], in0=O[:, :, 0:64],
                                                          in1=rec[:, 0:4].unsqueeze(2).to_broadcast([128, 4, 64]), op=ALU.mult),
                         reads=["b%d" % ob, "rec"], writes=["att"])

            emit_S(0)
            emit_S(1)
            for n in range(len(steps)):
                if n + 2 < len(steps):
                    emit_S(n + 2)
                emit_PV(n)

        if upto >= 2:
            build_qk(0)
            for h in range(8):
                if h + 1 < 8:
                    build_qk(h + 1)
                attn(h)
        if debug and upto <= 2:
            for nm, src in (("d_qnT", qnT), ("d_kvnT", kvnT), ("d_kpeT", kpeT), ("d_V", Vt), ("d_att", att)):
                flat = src
                if len(src.shape) == 3:
                    flat = src.rearrange("p a b -> p (a b)")
                elif len(src.shape) == 4:
                    flat = src.rearrange("p a b c -> p (a b c)")
                S.barrier()
                S.dma("sp", lambda e, nm=nm, flat=flat: e.dma_start(out=dbg_d[nm], in_=flat), "dbg", final=True)
        S.barrier()

        w_in3 = A.alloc([128, 8, 1024], BF16, {3})
        dg = A.alloc([128, 4, 31, 128], BF16, {3})
        xt3 = [A.alloc([128, D], F32, {3}), A.alloc([128, D], F32, {3})]
        xsb3 = A.alloc([128, D], BF16, {3})
        junkb3 = A.alloc([128, D], BF16, {3})
        hnT32 = [A.alloc([128, 8, 512], BF16, {3}) for _ in range(2)]
        glu = [A.alloc([128, 4, 544], BF16, {3}), A.alloc([128, 4, 544], BF16, {3})]
        sig = A.alloc([128, 512], F32, {3})
        y = A.alloc([128, 4, 512], F32, {3})
        ybf = A.alloc([128, 4, 512], BF16, {3})
        mean = A.alloc([128, 512], F32, {3})
        sd3 = A.alloc([128, 512], F32, {3})
        ssx3 = scr[:, 16:20]
        if upto >= 3:
            S.dma("pool", lambda e: e.dma_start(out=w_in3, in_=w_in_v[:, :, 800:1824]), "w0", writes=["w_in3"])
            cw = cols[:, C_CW:C_CW + 124].rearrange("p (j k) -> p j k", j=4)
            for j in range(4):
                for k in range(31):
                    eng = "dve" if (k % 2 == 0) else "pool"
                    S.op(eng, lambda e, j=j, k=k: e.tensor_scalar(out=dg[:, j, k, :], in0=ident, scalar1=cw[:, j, k:k + 1],
                                                                  scalar2=None, op0=ALU.mult),
                         reads=["ident", "cols"], writes=["dg"])
            S.op("pool", lambda e: e.memset(glu[1][:, :, 0:32], 0.0), writes=["glu1"])
            for c in range(NCH):
                gp = c % 2
                hnT3 = hnT32[c % 2]
                HN3 = "hnT%d" % (c % 2)
                x_to_hnT(c, xt3, xsb3, junkb3, hnT3, ssx3, HN3)
                if c > 0:
                    S.op("pool", lambda e, gp=gp: e.tensor_copy(out=glu[gp][:, :, 0:32], in_=glu[1 - gp][:, :, 512:544]),
                         reads=["glu%d" % (1 - gp)], writes=["glu%d" % gp])
                else:
                    S.op("pool", lambda e: e.memset(glu[0][:, :, 0:32], 0.0), writes=["glu0"])
                for j in range(4):
                    for kc in range(8):
                        S.op("pe", lambda e, j=j, kc=kc, hnT3=hnT3: e.matmul(bk(1), lhsT=w_in3[:, kc, j * 128:(j + 1) * 128],
                                                                  rhs=hnT3[:, kc, :], start=(kc == 0), stop=(kc == 7)),
                             reads=[HN3, "w_in3"], writes=["b1"])
                    for kc in range(8):
                        S.op("pe", lambda e, j=j, kc=kc, hnT3=hnT3: e.matmul(bk(2), lhsT=w_in3[:, kc, 512 + j * 128:512 + (j + 1) * 128],
                                                                  rhs=hnT3[:, kc, :], start=(kc == 0), stop=(kc == 7)),
                             reads=[HN3, "w_in3"], writes=["b2"])
                    S.op("act", lambda e: e.activation(out=sig, in_=bk(2), func=AF.Sigmoid), reads=["b2"], writes=["sig"])
                    S.op("dve", lambda e, j=j, gp=gp: e.tensor_tensor(out=glu[gp][:, j, 32:544], in0=bk(1), in1=sig, op=ALU.mult),
                         reads=["b1", "sig"], writes=["glu%d" % gp])
                    yb = 3 + (j % 2)
                    for k in range(31):
                        S.op("pe", lambda e, j=j, k=k, gp=gp, yb=yb: e.matmul(bk(yb), lhsT=dg[:, j, k, :],
                                                                              rhs=glu[gp][:, j, 2 + k:2 + k + 512],
                                                                              start=(k == 0), stop=(k == 30)),
                             reads=["glu%d" % gp, "dg"], writes=["b%d" % yb])
                    S.op("act", lambda e, j=j, yb=yb: e.activation(out=y[:, j, :], in_=bk(yb), func=AF.Identity,
                                                                  bias=col(C_CB + j)),
                         reads=["b%d" % yb, "cols"], writes=["y%d" % j])
                    S.op("act", lambda e, j=j: e.copy(out=ybf[:, j, :], in_=y[:, j, :]), reads=["y%d" % j], writes=["ybf"])
                for j in range(4):
                    S.op("pe", lambda e, j=j: e.matmul(bk(5), lhsT=ones, rhs=ybf[:, j, :], start=(j == 0), stop=(j == 3)),
                         reads=["ybf", "ones"], writes=["b5"])
                S.op("act", lambda e: e.activation(out=mean, in_=bk(5), func=AF.Copy, scale=1.0 / 512), reads=["b5"],
                     writes=["mean"])
                for j in range(4):
                    S.op("dve", lambda e, j=j: e.tensor_tensor(out=y[:, j, :], in0=y[:, j, :], in1=mean, op=ALU.subtract),
                         reads=["y%d" % j, "mean"], writes=["y%d" % j])
                    S.op("act", lambda e, j=j: e.activation(out=ybf[:, j, :], in_=y[:, j, :], func=AF.Square),
                         reads=["y%d" % j], writes=["ybf"])
                for j in range(4):
                    S.op("pe", lambda e, j=j: e.matmul(bk(5), lhsT=ones, rhs=ybf[:, j, :], start=(j == 0), stop=(j == 3)),
                         reads=["ybf", "ones"], writes=["b5"])
                S.op("act", lambda e: e.activation(out=sd3, in_=bk(5), func=AF.Sqrt, scale=1.0 / 512, bias=EPS),
                     reads=["b5"], writes=["sd3"])
                S.op("dve", lambda e: e.reciprocal(out=sd3, in_=sd3), reads=["sd3"], writes=["sd3"])
                for j in range(4):
                    S.op("dve", lambda e, j=j: e.tensor_tensor(out=y[:, j, :], in0=y[:, j, :], in1=sd3, op=ALU.mult),
                         reads=["y%d" % j, "sd3"], writes=["y%d" % j])
                    S.op("act", lambda e, j=j: e.activation(out=y[:, j, :], in_=y[:, j, :], func=AF.Silu,
                                                            scale=col(C_LNG + j), bias=col(C_LNB + j)),
                         reads=["y%d" % j, "cols"], writes=["y%d" % j])
                    S.op("act", lambda e, j=j: e.activation(out=ybf[:, j, :], in_=y[:, j, :], func=AF.Square),
                         reads=["y%d" % j], writes=["ybf"])
                for j in range(4):
                    S.op("pe", lambda e, j=j: e.matmul(bk(5), lhsT=ones, rhs=ybf[:, j, :], start=(j == 0), stop=(j == 3)),
                         reads=["ybf", "ones"], writes=["b5"])
                S.op("act", lambda e: e.activation(out=sd3, in_=bk(5), func=AF.Sqrt, scale=1.0 / 512, bias=EPS),
                     reads=["b5"], writes=["sd3"])
                S.op("dve", lambda e: e.reciprocal(out=sd3, in_=sd3), reads=["sd3"], writes=["sd3"])
                for j in range(4):
                    S.op("dve", lambda e, j=j, c=c: e.scalar_tensor_tensor(
                        out=cnT[:, j, c * 512:(c + 1) * 512], in0=y[:, j, :], scalar=col(C_CG + j), in1=sd3,
                        op0=ALU.mult, op1=ALU.mult), reads=["y%d" % j, "sd3", "cols"], writes=["cnT"])
        if debug and upto == 3:
            S.barrier()
            S.dma("sp", lambda e: e.dma_start(out=dbg_d["d_cnT"], in_=cnT.rearrange("p a b -> p (a b)")), "dbg", final=True)
            S.dma("sp", lambda e: e.dma_start(out=dbg_d["d_att"], in_=att.rearrange("p a b -> p (a b)")), "dbg", final=True)
        S.barrier()

        if upto >= 4:
            w_out = A.alloc([128, 8, D], BF16, {4})
            wq = A.alloc([128, 8, D], BF16, {4})
            kbd = A.alloc([128, 8, 256], BF16, {4})
            gw = A.alloc([128, 8, D], BF16, {4})
            pw = A.alloc([128, 2, D], BF16, {4})
            g_ffn = A.alloc([128, D], BF16, {4})
            g_b = A.alloc([128, D], F32, {4})
            g_fin = A.alloc([128, D], F32, {4})
            NB, GRP = 9, 2
            gball = A.alloc([128, NB, 2 * D], BF16, {4})
            gb = [gball[:, b, :] for b in range(NB)]
            dgs = [A.alloc([128, 128], BF16, {4}) for _ in range(4)]
            gl = A.alloc([128, 128], F32, {4})
            h1s = [A.alloc([128, D], F32, {4}) for _ in range(2)]
            xnbs = [A.alloc([128, D], BF16, {4}) for _ in range(2)]
            tmp = A.alloc([128, D], F32, {4})
            bfa = A.alloc([128, D], BF16, {4})
            xT = A.alloc([128, 8, 128], BF16, {4})
            mp = A.alloc([128, 256], F32, {4})
            mixT = mp.bitcast(BF16).rearrange("p (a b) -> p a b", a=4)
            tk = A.alloc([128, 2048], F32, {4})
            sc = tk.rearrange("p (a b) -> p a b", a=16)
            cand = tk.rearrange("p (h c) -> p h c", h=8)
            oh = tk.rearrange("p (h a b) -> p h a b", h=8, a=16)
            wk = A.alloc([128, 256], F32, {4})
            m16 = A.alloc([128, 16, 16], F32, {4})
            ix16 = A.alloc([128, 16, 16], U32, {4})
            ixf = ix16.bitcast(F32)
            best = A.alloc([128, 8, 16], F32, {4})
            posu = A.alloc([128, 8, 16], U32, {4})
            posf = A.alloc([128, 8, 16], F32, {4})
            ki4 = posu.bitcast(I32)
            k1f = A.alloc([128, 8, 16], F32, {4})
            k2f = A.alloc([128, 8, 16], F32, {4})
            e1 = A.alloc([128, 8, 16], F32, {4})
            e2 = posf
            eidxs = [A.alloc([128, 128], I32, {4}) for _ in range(2)]
            gate16s = [A.alloc([128, 8, 16], F32, {4}) for _ in range(2)]
            actv = A.alloc([128, 128], F32, {4})
            coef = A.alloc([128, 128], F32, {4})
            pt_ = mp
            pbf = A.alloc([128, 256], BF16, {4})
            pT = A.alloc([128, 2, 128], BF16, {4})
            gsum = A.alloc([128, 8], F32, {4})
            ss4 = scr[:, 24:40]

            def wload(dst, src, ch, name):
                S.dma("pool", lambda e: e.dma_start(out=dst, in_=src.rearrange("(k p) c -> p k c", p=128)), ch, writes=[name])
            wload(w_out, w_out_d, "w0", "w_out")
            wload(wq, wq_d, "w1", "wq")
            wload(gw, gw_d, "w2", "gw")
            wload(pw, pw_d, "w3", "pw")
            S.op("dve", lambda e: e.memset(kbd, 0.0), writes=["kbd"])
            S.dma("pool", lambda e: e.dma_start(out=kbd[0:64, :, 0:128], in_=keysT_d[0:64]), "w4", writes=["kbd"])
            S.dma("pool", lambda e: e.dma_start(out=kbd[64:128, :, 128:256], in_=keysT_d[64:128]), "w5", writes=["kbd"])
            S.dma("pool", lambda e: e.dma_start(out=g_ffn, in_=rows_d[0]), "c0", writes=["g_ffn"])
            S.dma("sp", lambda e: e.dma_start(out=g_b, in_=rows_d[1]), "c1", writes=["g_b"])
            S.dma("sp", lambda e: e.dma_start(out=g_fin, in_=rows_d[2]), "c2", writes=["g_fin"])

            class Rec:
                def __init__(self):
                    self.q = []
                COST = {"dve": 0.4, "act": 0.12, "pe": 0.04, "pool": 0.3}
                def op(self, *a, **k):
                    c = k.pop("cost", self.COST.get(a[0], 0.1))
                    self.q.append((lambda: S.op(*a, **k), c))
                def dma(self, *a, **k):
                    self.q.append((lambda: S.dma(*a, **k), 0.05))
                def brk(self):
                    self.q.append(None)

            def rms_tile(X, src, srcname, k, n_feat, junk, junkname):
                X.op("act", lambda e: e.activation(out=junk, in_=src, func=AF.Square, accum_out=ss4[:, k:k + 1]),
                     reads=[srcname], writes=[junkname, "r%d_ss" % k])
                X.op("act", lambda e: e.activation(out=ss4[:, k + 8:k + 9], in_=ss4[:, k:k + 1], func=AF.Sqrt, scale=1.0 / n_feat, bias=EPS),
                     reads=["r%d_ss" % k], writes=["r%d_r" % k])
                X.brk()
                X.op("dve", lambda e: e.reciprocal(out=ss4[:, k + 8:k + 9], in_=ss4[:, k + 8:k + 9]), reads=["r%d_r" % k], writes=["r%d_r" % k])
                return ss4[:, k + 8:k + 9], "r%d_r" % k

            Tb = bkb(0).rearrange("p (a b) -> p a b", a=8)
            Tp = bkb(3).rearrange("p (a b) -> p a b", a=8)

            def A_ops(g):
                X = Rec()
                pp = g % 2
                H, Hn = h1s[pp], "h1_%d" % pp
                xnb, xnbn = xnbs[pp], "xnb_%d" % pp
                eidx, eidxn = eidxs[pp], "eidx_%d" % pp
                gate16, gaten = gate16s[pp], "gate_%d" % pp
                ts_ = slice(g * 128, (g + 1) * 128)
                X.dma("sp", lambda e: e.dma_start(out=H, in_=x_d[ts_, :]), "x0", writes=[Hn])
                att_t = att[:, g, :]
                ra, ran = rms_tile(X, att_t, "att", 0, 512, bfa[:, 0:512], "bfa")
                X.op("dve", lambda e: e.tensor_scalar(out=bfa[:, 0:512], in0=att_t, scalar1=ra, scalar2=None, op0=ALU.mult),
                     reads=["att", ran], writes=["bfa"])
                for kc in range(4):
                    X.op("pe", lambda e, kc=kc: e.transpose(out=Tb[:, kc, :], in_=bfa[:, kc * 128:(kc + 1) * 128], identity=ident),
                         reads=["bfa", "ident"], writes=["b0"])
                X.brk()
                X.op("dve", lambda e: e.tensor_tensor(out=mixT, in0=Tb[:, 0:4, :],
                                                      in1=col(C_GAO, 4).unsqueeze(2).to_broadcast([128, 4, 128]), op=ALU.mult),
                     reads=["b0", "cols"], writes=["mixT"], cost=0.6)
                for half in range(2):
                    for kc in range(8):
                        lhs = mixT[:, kc, :] if kc < 4 else cnT[:, kc - 4, ts_]
                        X.op("pe", lambda e, half=half, kc=kc, lhs=lhs: e.matmul(bk(3 + half), lhsT=lhs,
                                                                                 rhs=w_out[:, kc, half * 512:(half + 1) * 512],
                                                                                 start=(kc == 0), stop=(kc == 7)),
                             reads=["mixT", "cnT", "w_out"], writes=["b%d" % (3 + half)])
                X.brk()
                for half in range(2):
                    X.op("dve", lambda e, half=half: e.tensor_tensor(out=H[:, half * 512:(half + 1) * 512],
                                                                     in0=H[:, half * 512:(half + 1) * 512], in1=bk(3 + half), op=ALU.add),
                         reads=[Hn, "b%d" % (3 + half)], writes=[Hn], cost=0.7)
                r1, r1n = rms_tile(X, H, Hn, 1, D, tmp, "tmp")
                X.op("dve", lambda e: e.scalar_tensor_tensor(out=xnb, in0=H, scalar=r1, in1=g_ffn, op0=ALU.mult, op1=ALU.mult),
                     reads=[Hn, r1n, "g_ffn"], writes=[xnbn], cost=1.2)
                for kc in range(8):
                    X.op("pe", lambda e, kc=kc: e.transpose(out=Tb[:, kc, :], in_=xnb[:, kc * 128:(kc + 1) * 128], identity=ident),
                         reads=[xnbn, "ident"], writes=["b0"])
                X.brk()
                X.op("act", lambda e: e.copy(out=xT, in_=Tb), reads=["b0"], writes=["xT"])
                for half in range(2):
                    for kc in range(8):
                        X.op("pe", lambda e, half=half, kc=kc: e.matmul(bk(3 + half), lhsT=xT[:, kc, :],
                                                                        rhs=wq[:, kc, half * 512:(half + 1) * 512],
                                                                        start=(kc == 0), stop=(kc == 7)),
                             reads=["xT", "wq"], writes=["b%d" % (3 + half)])
                X.brk()
                for half in range(2):
                    X.op("act", lambda e, half=half: e.copy(out=bfa[:, half * 512:(half + 1) * 512], in_=bk(3 + half)),
                         reads=["b%d" % (3 + half)], writes=["bfa"])
                for kc in range(8):
                    X.op("pe", lambda e, kc=kc: e.transpose(out=Tb[:, kc, :], in_=bfa[:, kc * 128:(kc + 1) * 128], identity=ident),
                         reads=["bfa", "ident"], writes=["b0"])
                X.brk()
                X.op("dve", lambda e: e.tensor_copy(out=xT, in_=Tb), reads=["b0"], writes=["xT"], cost=1.0)
                for hh in range(8):
                    sbk = 4 + hh // 2
                    X.op("pe", lambda e, hh=hh, sbk=sbk: e.matmul(bk(sbk)[:, (hh % 2) * 256:(hh % 2 + 1) * 256], lhsT=xT[:, hh, :],
                                                                 rhs=kbd[:, hh, :], start=True, stop=True),
                         reads=["xT", "kbd"], writes=["b%d" % sbk])
                X.brk()
                for b4 in range(4):
                    X.op("act", lambda e, b4=b4: e.copy(out=tk[:, b4 * 512:(b4 + 1) * 512], in_=bk(4 + b4)),
                         reads=["b%d" % (4 + b4)], writes=["tk"])
                X.brk()
                for hc in range(16):
                    X.op("dve", lambda e, hc=hc: e.max(out=m16[:, hc, 0:8], in_=sc[:, hc, :]), reads=["tk"], writes=["m16"])
                    X.op("dve", lambda e, hc=hc: e.max_index(out=ix16[:, hc, 0:8], in_max=m16[:, hc, 0:8], in_values=sc[:, hc, :]),
                         reads=["tk", "m16"], writes=["ix16"])
                    X.op("dve", lambda e, hc=hc: e.match_replace(out=wk[:, 0:128], in_to_replace=m16[:, hc, 0:8],
                                                                 in_values=sc[:, hc, :], imm_value=-1e30),
                         reads=["tk", "m16"], writes=["wk"])
                    X.op("dve", lambda e, hc=hc: e.max(out=m16[:, hc, 8:16], in_=wk[:, 0:128]), reads=["wk"], writes=["m16"])
                    X.op("dve", lambda e, hc=hc: e.max_index(out=ix16[:, hc, 8:16], in_max=m16[:, hc, 8:16], in_values=wk[:, 0:128]),
                         reads=["wk", "m16"], writes=["ix16"])
                m4 = m16.rearrange("p (h c) k -> p h c k", c=2)
                X.op("dve", lambda e: e.tensor_tensor(out=cand.rearrange("p h (a b) -> p h a b", a=16),
                                                      in0=m4[:, :, 0, :].unsqueeze(3).to_broadcast([128, 8, 16, 16]),
                                                      in1=m4[:, :, 1, :].unsqueeze(2).to_broadcast([128, 8, 16, 16]), op=ALU.add),
                     reads=["m16", "tk"], writes=["tk"], cost=2.2)
                for hh in range(8):
                    X.op("dve", lambda e, hh=hh: e.max(out=best[:, hh, 0:8], in_=cand[:, hh, :]), reads=["tk"], writes=["best"])
                    X.op("dve", lambda e, hh=hh: e.max_index(out=posu[:, hh, 0:8], in_max=best[:, hh, 0:8], in_values=cand[:, hh, :]),
                         reads=["tk", "best"], writes=["posu"])
                    X.op("dve", lambda e, hh=hh: e.match_replace(out=wk, in_to_replace=best[:, hh, 0:8], in_values=cand[:, hh, :],
                                                                 imm_value=-1e30), reads=["tk", "best"], writes=["wk"])
                    X.op("dve", lambda e, hh=hh: e.max(out=best[:, hh, 8:16], in_=wk), reads=["wk"], writes=["best"])
                    X.op("dve", lambda e, hh=hh: e.max_index(out=posu[:, hh, 8:16], in_max=best[:, hh, 8:16], in_values=wk),
                         reads=["wk", "best"], writes=["posu"])
                X.op("dve", lambda e: e.tensor_copy(out=posf, in_=posu), reads=["posu"], writes=["posf"])
                X.op("dve", lambda e: e.tensor_copy(out=ixf, in_=ix16), reads=["ix16"], writes=["ix16"])
                X.op("dve", lambda e: e.tensor_scalar(out=k1f, in0=posf, scalar1=-7.5, scalar2=0.0625, op0=ALU.add, op1=ALU.mult),
                     reads=["posf"], writes=["k1f"])
                X.op("dve", lambda e: e.tensor_copy(out=ki4, in_=k1f), reads=["k1f", "posf"], writes=["posu"])
                X.op("dve", lambda e: e.tensor_copy(out=k1f, in_=ki4), reads=["posu"], writes=["k1f"])
                X.op("dve", lambda e: e.scalar_tensor_tensor(out=k2f, in0=k1f, scalar=-16.0, in1=posf, op0=ALU.mult, op1=ALU.add),
                     reads=["k1f", "posf"], writes=["k2f"])
                ix4 = ixf.rearrange("p (h c) k -> p h c k", c=2)
                io_b = iota16.unsqueeze(1).unsqueeze(1).to_broadcast([128, 8, 16, 16])
                for kf, cc, eo, nm in ((k1f, 0, e1, "e1"), (k2f, 1, e2, "posf")):
                    X.op("dve", lambda e, kf=kf: e.tensor_tensor(out=oh, in0=kf.unsqueeze(3).to_broadcast([128, 8, 16, 16]),
                                                                 in1=io_b, op=ALU.is_equal),
                         reads=["k1f", "k2f", "iota16", "tk"], writes=["tk"], cost=2.2)
                    X.op("dve", lambda e, cc=cc: e.tensor_tensor(out=oh, in0=oh,
                                                                 in1=ix4[:, :, cc, :].unsqueeze(2).to_broadcast([128, 8, 16, 16]),
                                                                 op=ALU.mult), reads=["tk", "ix16"], writes=["tk"], cost=2.2)
                    X.op("dve", lambda e, eo=eo: e.reduce_sum(out=eo, in_=oh, axis=AX.X), reads=["tk"], writes=[nm], cost=2.2)
                X.op("dve", lambda e: e.scalar_tensor_tensor(out=e1, in0=e1, scalar=128.0, in1=e2, op0=ALU.mult, op1=ALU.add),
                     reads=["e1", "posf"], writes=["e1"])
                X.op("dve", lambda e: e.tensor_copy(out=eidx, in_=e1.rearrange("p h k -> p (h k)")), reads=["e1"], writes=[eidxn])
                X.op("dve", lambda e: e.tensor_tensor(out=gate16, in0=best, in1=best[:, :, 0:1].to_broadcast([128, 8, 16]),
                                                      op=ALU.subtract), reads=["best"], writes=[gaten])
                X.op("act", lambda e: e.activation(out=gate16, in_=gate16, func=AF.Exp), reads=[gaten], writes=[gaten])
                X.brk()
                X.op("dve", lambda e: e.reduce_sum(out=gsum, in_=gate16, axis=AX.X), reads=[gaten], writes=["gsum"])
                X.op("dve", lambda e: e.reciprocal(out=gsum, in_=gsum), reads=["gsum"], writes=["gsum"])
                X.op("dve", lambda e: e.tensor_tensor(out=gate16, in0=gate16, in1=gsum.unsqueeze(2).to_broadcast([128, 8, 16]),
                                                      op=ALU.mult), reads=[gaten, "gsum"], writes=[gaten])
                return X.q

            TABS = ["tabu%d" % k for k in range(8)] + ["tabv%d" % k for k in range(8)]

            def B_emit(g, side, per_group):
                pp = g % 2
                H, Hn = h1s[pp], "h1_%d" % pp
                xnb, xnbn = xnbs[pp], "xnb_%d" % pp
                eidx, eidxn = eidxs[pp], "eidx_%d" % pp
                gflat, gaten = gate16s[pp].rearrange("p h k -> p (h k)"), "gate_%d" % pp

                def consume(k):
                    gs = slice(k * GRP, (k + 1) * GRP)
                    S.op("act", lambda e: e.activation(out=gl[:, gs], in_=actv[:, gs], func=AF.Gelu), reads=["actv"], writes=["gl"])
                    S.op("dve", lambda e: e.tensor_tensor(out=coef[:, gs], in0=gl[:, gs], in1=gflat[:, gs], op=ALU.mult),
                         reads=["gl", gaten], writes=["coef"])
                    for s2 in range(k * GRP, (k + 1) * GRP):
                        b2 = s2 % NB
                        dgi = s2 % 4
                        S.op("act", lambda e, s2=s2, dgi=dgi: e.activation(out=dgs[dgi], in_=ident, func=AF.Copy, scale=coef[:, s2:s2 + 1]),
                             reads=["ident", "coef"], writes=["dg%d" % dgi])
                        for half in range(2):
                            S.op("pe", lambda e, s2=s2, b2=b2, dgi=dgi, half=half: e.matmul(
                                bk(1 + half), lhsT=dgs[dgi], rhs=gb[b2][:, D + half * 512:D + (half + 1) * 512],
                                start=(s2 == 0), stop=(s2 == 127)),
                                reads=["dg%d" % dgi, "gb%d" % b2], writes=["b%d" % (1 + half)])

                for s_ in range(128):
                    b = s_ % NB
                    S.dma("pool", lambda e, s_=s_, b=b: e.indirect_dma_start(
                        out=gb[b], out_offset=None, in_=tab_d, in_offset=bass.IndirectOffsetOnAxis(ap=eidx[:, s_:s_ + 1], axis=0)),
                        "g%d" % b, reads=[eidxn] + TABS, writes=["gb%d" % b])
                    if s_ % 2 == 0 or not DOT_SPLIT:
                        S.op("dve", lambda e, s_=s_, b=b: e.scalar_tensor_tensor(out=gb[b][:, 0:D], in0=gb[b][:, 0:D], scalar=1.0, in1=xnb,
                                                                                 op0=ALU.mult, op1=ALU.mult, accum_out=actv[:, s_:s_ + 1]),
                             reads=["gb%d" % b, xnbn], writes=["gb%d" % b, "actv"])
                    else:
                        S.op("dve", lambda e, b=b: e.tensor_tensor(out=gb[b][:, 0:D], in0=gb[b][:, 0:D], in1=xnb, op=ALU.mult),
                             reads=["gb%d" % b, xnbn], writes=["gb%d" % b])
                        S.op("act", lambda e, s_=s_, b=b: e.activation(out=gb[b][:, 0:D], in_=gb[b][:, 0:D], func=AF.Copy,
                                                                       accum_out=actv[:, s_:s_ + 1]),
                             reads=["gb%d" % b], writes=["gb%d" % b, "actv"])
                    if (s_ + 1) % GRP == 0:
                        consume(s_ // GRP)
                        acc_c = 0.0
                        while side:
                            it = side.pop(0)
                            if it is None:
                                if acc_c >= 0.35 * per_group:
                                    break
                                continue
                            it[0]()
                            acc_c += it[1]
                            if acc_c >= per_group:
                                break
                while side:
                    it = side.pop(0)
                    if it is not None:
                        it[0]()
                for half in range(2):
                    S.op("dve", lambda e, half=half: e.tensor_tensor(out=H[:, half * 512:(half + 1) * 512],
                                                                     in0=H[:, half * 512:(half + 1) * 512], in1=bk(1 + half), op=ALU.add),
                         reads=[Hn, "b%d" % (1 + half)], writes=[Hn])

            def C_ops(g):
                X = Rec()
                pp = g % 2
                H, Hn = h1s[pp], "h1_%d" % pp
                ts_ = slice(g * 128, (g + 1) * 128)
                X.dma("sp", lambda e: e.dma_start(out=pt_, in_=p_d[ts_, :]), "x1", writes=["mixT"])
                r2, r2n = rms_tile(X, H, Hn, 2, D, tmp, "tmp")
                X.op("dve", lambda e: e.tensor_scalar(out=bfa, in0=H, scalar1=r2, scalar2=None, op0=ALU.mult),
                     reads=[Hn, r2n], writes=["bfa"], cost=0.8)
                X.op("act", lambda e: e.copy(out=pbf, in_=pt_), reads=["mixT"], writes=["pbf"])
                for kc in range(8):
                    X.op("pe", lambda e, kc=kc: e.transpose(out=Tb[:, kc, :], in_=bfa[:, kc * 128:(kc + 1) * 128], identity=ident),
                         reads=["bfa", "ident"], writes=["b0"])
                for kc in range(2):
                    X.op("pe", lambda e, kc=kc: e.transpose(out=Tp[:, kc, :], in_=pbf[:, kc * 128:(kc + 1) * 128], identity=ident),
                         reads=["pbf", "ident"], writes=["b3"])
                X.brk()
                X.op("dve", lambda e: e.tensor_tensor(out=xT, in0=Tb, in1=col(C_GPL, 8).unsqueeze(2).to_broadcast([128, 8, 128]),
                                                      op=ALU.mult), reads=["b0", "cols"], writes=["xT"], cost=1.0)
                X.op("act", lambda e: e.copy(out=pT, in_=Tp[:, 0:2, :]), reads=["b3"], writes=["pT"])
                for half in range(2):
                    hs = slice(half * 512, (half + 1) * 512)
                    gbk = 5 + half
                    for kc in range(8):
                        X.op("pe", lambda e, hs=hs, kc=kc, gbk=gbk: e.matmul(bk(gbk), lhsT=xT[:, kc, :], rhs=gw[:, kc, hs],
                                                                             start=(kc == 0), stop=(kc == 7)),
                             reads=["xT", "gw"], writes=["b%d" % gbk])
                X.brk()
                for half in range(2):
                    hs = slice(half * 512, (half + 1) * 512)
                    gbk = 5 + half
                    X.op("dve", lambda e, hs=hs, gbk=gbk: e.tensor_tensor(out=tmp[:, hs], in0=bk(gbk), in1=g_b[:, hs], op=ALU.add),
                         reads=["b%d" % gbk, "g_b", "tmp"], writes=["tmp"], cost=0.7)
                    X.op("act", lambda e, hs=hs: e.activation(out=tmp[:, hs], in_=tmp[:, hs], func=AF.Sigmoid), reads=["tmp"],
                         writes=["tmp"])
                    for kc in range(2):
                        X.op("pe", lambda e, hs=hs, kc=kc, gbk=gbk: e.matmul(bk(gbk), lhsT=pT[:, kc, :], rhs=pw[:, kc, hs],
                                                                             start=(kc == 0), stop=(kc == 1)),
                             reads=["pT", "pw"], writes=["b%d" % gbk])
                X.brk()
                for half in range(2):
                    hs = slice(half * 512, (half + 1) * 512)
                    gbk = 5 + half
                    X.op("dve", lambda e, hs=hs, gbk=gbk: e.tensor_tensor(out=tmp[:, hs], in0=tmp[:, hs], in1=bk(gbk), op=ALU.mult),
                         reads=["b%d" % gbk, "tmp"], writes=["tmp"], cost=0.7)
                X.op("dve", lambda e: e.tensor_tensor(out=H, in0=H, in1=tmp, op=ALU.add), reads=[Hn, "tmp"], writes=[Hn], cost=1.1)
                r3, r3n = rms_tile(X, H, Hn, 3, D, tmp, "tmp")
                X.op("dve", lambda e: e.scalar_tensor_tensor(out=tmp, in0=H, scalar=r3, in1=g_fin, op0=ALU.mult, op1=ALU.mult),
                     reads=[Hn, r3n, "g_fin", "tmp"], writes=["tmp"], cost=1.2)
                X.dma("sp", lambda e: e.dma_start(out=out_d[ts_, :], in_=tmp), "st", reads=["tmp"], final=True)
                return X.q

            for it in A_ops(0):
                if it is not None:
                    it[0]()
            for g in range(NT):
                side = []
                if g > 0:
                    side += C_ops(g - 1)
                if g + 1 < NT:
                    side += A_ops(g + 1)
                tot_c = sum(it[1] for it in side if it is not None)
                per_group = 1.15 * tot_c / (128 // GRP - 3)
                B_emit(g, side, per_group)
            for it in C_ops(NT - 1):
                if it is not None:
                    it[0]()
        else:
            z = A.alloc([128, D], F32, {4})
            S.op("dve", lambda e: e.memset(z, 0.0), writes=["z"])
            S.dma("sp", lambda e: e.dma_start(out=out_d[0:128, :], in_=z), "st", reads=["z"], final=True)

        stats = S.emit()
        print("program ops per engine:", stats)
    return nc


def make_in_maps(inputs):
    f = np.float32
    g = lambda k: np.asarray(inputs[k])
    x = g("x").astype(f, copy=False)
    p = g("p").astype(f, copy=False)[0]
    pos = g("positions").astype(np.int32, copy=False)

    def colmaj(v):
        v = np.asarray(v, f).reshape(-1, 128)
        return np.ascontiguousarray(v.T)

    cols = np.zeros((128, NCOL), f)
    cols[:, C_GA:C_GA + 8] = colmaj(g("attn_norm")[0])
    cols[:, C_GQ:C_GQ + 4] = colmaj(g("q_norm")[0])
    cols[:, C_GKV:C_GKV + 2] = colmaj(g("kv_norm")[0])
    cols[:, C_CB:C_CB + 4] = colmaj(g("conv_b")[0])
    cols[:, C_LNG:C_LNG + 4] = colmaj(g("conv_ln_g")[0])
    cols[:, C_LNB:C_LNB + 4] = colmaj(g("conv_ln_b")[0])
    cols[:, C_CG:C_CG + 4] = colmaj(g("conv_out_norm")[0])
    cols[:, C_GAO:C_GAO + 4] = colmaj(g("attn_out_norm")[0])
    cols[:, C_GPL:C_GPL + 8] = colmaj(g("pl_norm")[0])
    half = 16
    freqs = (np.float32(10000.0) ** (-np.arange(half, dtype=f) / np.float32(half))).astype(f)
    for pp in range(64, 96):
        cols[pp, C_FREQ] = freqs[(pp - 64) % 16]
    cw = np.asarray(g("conv_w")[0], f)
    cols[:, C_CW:C_CW + 124] = cw.reshape(31, 4, 128).transpose(2, 1, 0).reshape(128, 124)
    rows = np.stack([np.broadcast_to(np.asarray(g(k), f).reshape(-1)[None, :], (128, D))
                     for k in ("ffn_norm", "pl_gate_b", "final_norm")]).astype(f)
    rows = np.ascontiguousarray(rows)
    cst = np.zeros((3, 128, 128), f)
    cst[0] = np.eye(128, dtype=f)
    cst[1] = np.triu(np.ones((128, 128), f))
    cst[2, :, 0:16] = np.arange(16, dtype=f)[None, :]
    keysT = np.ascontiguousarray(np.asarray(g("peer_keys")[0], f).transpose(1, 3, 0, 2).reshape(128, 8, 128))
    shared = {
        "w_in": np.ascontiguousarray(g("w_in")[0], f), "w_uq": np.ascontiguousarray(g("w_uq")[0], f),
        "w_ukv": np.ascontiguousarray(g("w_ukv")[0], f), "w_out": np.ascontiguousarray(g("w_out")[0], f),
        "peer_wq": np.ascontiguousarray(g("peer_wq")[0], f), "keysT": keysT,
        "peer_u": np.ascontiguousarray(g("peer_u")[0], f), "peer_v": np.ascontiguousarray(g("peer_v")[0], f),
        "pl_gate_w": np.ascontiguousarray(g("pl_gate_w")[0], f), "pl_proj": np.ascontiguousarray(g("pl_proj")[0], f),
        "cols": cols, "rows": rows, "cst": cst,
    }
    maps = []
    for b in range(8):
        m = dict(shared)
        m["x"] = np.ascontiguousarray(x[b])
        m["p"] = np.ascontiguousarray(p[b])
        m["pos"] = np.ascontiguousarray(np.broadcast_to(pos[b][None, :], (32, T))).astype(np.int32)
        maps.append(m)
    return maps


_NC_CACHE = {}


def kernel(**inputs):
    maps = make_in_maps(inputs)
    if "nc" not in _NC_CACHE:
        _NC_CACHE["nc"] = build_program()
    nc = _NC_CACHE["nc"]
    res = run_bass_kernel_spmd(nc, maps, core_ids=list(range(8)))
    out = np.stack([np.asarray(r["out"], np.float32) for r in res.results], axis=0)
    return out.reshape(8, T, D)
```

```python
import contextlib
import numpy as np
import concourse.bass as bass
import concourse.mybir as mybir
from concourse.bass_utils import run_bass_kernel_spmd

F32 = mybir.dt.float32
BF16 = mybir.dt.bfloat16
I32 = mybir.dt.int32
U32 = mybir.dt.uint32
AF = mybir.ActivationFunctionType
ALU = mybir.AluOpType
AX = mybir.AxisListType

T = 4096
D = 1024
NT = 32
NCH = 8
EPS = 1e-6
ATT_SCALE = 96 ** -0.5
ENGS = ["pe", "act", "dve", "pool", "sp"]
SAME_ENGINE_RAW = True
SAME_ENGINE_WAR = True
DOT_SPLIT = False

C_GA = 0
C_GQ = 8
C_GKV = 12
C_CB = 14
C_LNG = 18
C_LNB = 22
C_CG = 26
C_GAO = 30
C_GPL = 34
C_FREQ = 42
C_CW = 44
NCOL = C_CW + 4 * 31


class Sched:
    def __init__(self, nc):
        self.nc = nc
        self.ops = {e: [] for e in ENGS}
        self.res = {}
        self.seen = {e: {} for e in ENGS}
        self.chan_n = {}
        self.pending = {e: [] for e in ENGS}
        self.final_waits = []

    def _need(self, eng, prod, waits):
        if prod is None:
            return
        kind, key, n = prod
        if kind == "e" and key == eng:
            if key == "pe" or not SAME_ENGINE_RAW:
                return
        k = (kind, key)
        if self.seen[eng].get(k, -1) >= n:
            return
        self.seen[eng][k] = n
        waits.append(prod)

    def _deps(self, eng, reads, writes, waits):
        for r in reads:
            st = self.res.setdefault(r, {"w": None, "r": {}})
            self._need(eng, st["w"], waits)
        for w in writes:
            st = self.res.setdefault(w, {"w": None, "r": {}})
            p = st["w"]
            if p is not None and (SAME_ENGINE_WAR or not (p[0] == "e" and p[1] == eng)):
                self._need(eng, p, waits)
            for k, rp in st["r"].items():
                if rp[0] == "e" and rp[1] == eng and not SAME_ENGINE_WAR:
                    continue
                self._need(eng, rp, waits)

    def _commit(self, tok, reads, writes):
        for r in reads:
            self.res[r]["r"][(tok[0], tok[1])] = tok
        for w in writes:
            st = self.res[w]
            st["w"] = tok
            st["r"] = {}

    def _take_pending(self, eng):
        waits = list(self.pending[eng])
        self.pending[eng] = []
        for p in waits:
            k = (p[0], p[1])
            self.seen[eng][k] = max(self.seen[eng].get(k, -1), p[2])
        return waits

    def op(self, eng, fn, reads=(), writes=()):
        waits = self._take_pending(eng)
        self._deps(eng, reads, writes, waits)
        idx = len(self.ops[eng])
        self.ops[eng].append({"fn": fn, "waits": waits, "sig": False, "dma": None})
        self._commit(("e", eng, idx), reads, writes)

    def dma(self, eng, fn, ch, reads=(), writes=(), final=False):
        waits = self._take_pending(eng)
        n = self.chan_n.get(ch, 0)
        if n > 0:
            self._need(eng, ("d", ch, n), waits)
        self._deps(eng, reads, writes, waits)
        n += 1
        self.chan_n[ch] = n
        self.ops[eng].append({"fn": fn, "waits": waits, "sig": False, "dma": ch})
        self._commit(("d", ch, n), reads, writes)
        if final and ch not in self.final_waits:
            self.final_waits.append(ch)

    def barrier(self):
        prods = []
        for e in ENGS:
            for i in range(len(self.ops[e]) - 1, -1, -1):
                if self.ops[e][i]["dma"] is None:
                    prods.append(("e", e, i))
                    break
        for ch, n in self.chan_n.items():
            if not ch.startswith("tb"):
                prods.append(("d", ch, n))
        for e in ENGS:
            self.pending[e] = [p for p in prods if not (p[0] == "e" and p[1] == e)]
        self.res = {k: v for k, v in self.res.items() if k.startswith("tab")}

    def emit(self):
        nc = self.nc
        for e in ENGS:
            for o in self.ops[e]:
                for (kind, key, n) in o["waits"]:
                    if kind == "e":
                        self.ops[key][n]["sig"] = True
        cnt = {}
        for e in ENGS:
            c = 0
            for o in self.ops[e]:
                if o["sig"]:
                    c += 1
                o["cnt"] = c
            cnt[e] = c
        with contextlib.ExitStack() as st:
            esem = {e: st.enter_context(nc.semaphore("s_" + e)) for e in ENGS if cnt[e] > 0}
            csem = {ch: st.enter_context(nc.semaphore("c_%s" % (ch,))) for ch in self.chan_n}
            block = st.enter_context(nc.Block())
            handles = {"pe": block.tensor, "act": block.scalar, "dve": block.vector,
                       "pool": block.gpsimd, "sp": block.sync}

            def make(e):
                def body(eng):
                    for o in self.ops[e]:
                        for (kind, key, n) in o["waits"]:
                            if kind == "e":
                                eng.wait_ge(esem[key], self.ops[key][n]["cnt"])
                            else:
                                eng.wait_ge(csem[key], 16 * n)
                        ins = o["fn"](eng)
                        if o["dma"] is not None:
                            ins.then_inc(csem[o["dma"]], 16)
                        elif o["sig"]:
                            ins.then_inc(esem[e], 1)
                    if e == "sp":
                        for ch in self.final_waits:
                            eng.wait_ge(csem[ch], 16 * self.chan_n[ch])
                return body

            for e in ENGS:
                if self.ops[e] or e == "sp":
                    handles[e](make(e))
        return {e: len(self.ops[e]) for e in ENGS}, cnt


class Arena:
    def __init__(self, base_ap, nbytes):
        self.base = base_ap
        self.nbytes = nbytes
        self.allocs = []

    def alloc(self, shape, dt, phases):
        esz = {F32: 4, BF16: 2, I32: 4, U32: 4}[dt]
        n = 1
        for s in shape[1:]:
            n *= s
        size = (n * esz + 31) // 32 * 32
        phases = set(phases)
        cands = sorted({0} | {o + s for (o, s, p) in self.allocs})
        for off in cands:
            ok = off + size <= self.nbytes
            if ok:
                for (o, s, p) in self.allocs:
                    if p & phases and off < o + s and o < off + size:
                        ok = False
                        break
            if ok:
                self.allocs.append((off, size, phases))
                v = self.base[:, off // 4:(off + size) // 4]
                if dt != F32:
                    v = v.bitcast(dt)
                v = v[:, 0:n]
                if len(shape) == 3:
                    v = v.rearrange("p (a b) -> p a b", a=shape[1])
                elif len(shape) == 4:
                    v = v.rearrange("p (a b c) -> p a b c", a=shape[1], b=shape[2])
                return v
        raise RuntimeError("arena full: %s %s %s" % (shape, dt, phases))


def build_program(upto=4, debug=False):
    nc = bass.Bass("TRN2", target_bir_lowering=False)

    def din(name, shape, dt=F32):
        return nc.dram_tensor(name, shape, dt, kind="ExternalInput").ap()

    x_d = din("x", [T, D])
    p_d = din("p", [T, 256])
    pos_d = din("pos", [32, T], I32)
    w_in_d = din("w_in", [D, 1824])
    w_uq_d = din("w_uq", [512, 768])
    w_ukv_d = din("w_ukv", [256, 1024])
    w_out_d = din("w_out", [D, D])
    wq_d = din("peer_wq", [D, D])
    keysT_d = din("keysT", [128, 8, 128])
    u_d = din("peer_u", [16384, D])
    v_d = din("peer_v", [16384, D])
    gw_d = din("pl_gate_w", [D, D])
    pw_d = din("pl_proj", [256, D])
    cols_d = din("cols", [128, NCOL])
    rows_d = din("rows", [3, 128, D])
    cst_d = din("cst", [3, 128, 128])
    out_d = nc.dram_tensor("out", [T, D], F32, kind="ExternalOutput").ap()
    tab_d = nc.dram_tensor("peer_tab", [16384, 2 * D], BF16, kind="Internal").ap()
    dbg_d = None
    if debug:
        dbg_d = {
            "d_qnT": nc.dram_tensor("d_qnT", [128, 4 * T], BF16, kind="ExternalOutput").ap(),
            "d_kvnT": nc.dram_tensor("d_kvnT", [128, 2 * T], BF16, kind="ExternalOutput").ap(),
            "d_kpeT": nc.dram_tensor("d_kpeT", [128, T], BF16, kind="ExternalOutput").ap(),
            "d_V": nc.dram_tensor("d_V", [128, NT * 8 * 65], BF16, kind="ExternalOutput").ap(),
            "d_att": nc.dram_tensor("d_att", [128, NT * 512], BF16, kind="ExternalOutput").ap(),
            "d_cnT": nc.dram_tensor("d_cnT", [128, 4 * T], BF16, kind="ExternalOutput").ap(),
        }

    ARENA_BYTES = 212800
    with contextlib.ExitStack() as st:
        arena_t = st.enter_context(nc.sbuf_tensor("arena", [128, ARENA_BYTES // 4], F32))
        banks = [st.enter_context(nc.psum_tensor("bank%d" % i, [128, 512], F32)) for i in range(8)]
        A = Arena(arena_t[:, :], ARENA_BYTES)
        S = Sched(nc)
        ALLP = {1, 2, 3, 4}

        def bk(i):
            return banks[i][:, :]

        def bkb(i):
            return banks[i][:, :].bitcast(BF16)

        cols = A.alloc([128, NCOL], F32, ALLP)
        ident = A.alloc([128, 128], BF16, ALLP)
        tri = A.alloc([128, 128], BF16, {1, 2})
        ones = A.alloc([128, 128], BF16, {1, 2, 3})
        iota16 = A.alloc([128, 16], F32, ALLP)
        scr = A.alloc([128, 64], F32, ALLP)

        S.dma("sp", lambda e: e.dma_start(out=cols, in_=cols_d), "c0", writes=["cols"])
        S.dma("pool", lambda e: e.dma_start(out=ident, in_=cst_d[0]), "c1", writes=["ident"])
        S.dma("pool", lambda e: e.dma_start(out=tri, in_=cst_d[1]), "c2", writes=["tri"])
        S.dma("sp", lambda e: e.dma_start(out=iota16, in_=cst_d[2][:, 0:16]), "c3", writes=["iota16"])
        S.op("dve", lambda e: e.memset(ones, 1.0), writes=["ones"])

        def col(c0, n=1):
            return cols[:, c0:c0 + n]

        att = A.alloc([128, NT, 512], BF16, {2, 3, 4})
        cnT = A.alloc([128, 4, T], BF16, {3, 4})
        qnT = A.alloc([128, 4, T], BF16, {1, 2})
        kvnT = A.alloc([128, 2, T], BF16, {1, 2})
        kpeT = A.alloc([128, T], BF16, {1, 2})
        Vt = A.alloc([128, NT, 8, 65], BF16, {1, 2})
        cosT = A.alloc([128, T], BF16, {1, 2})
        sinT = A.alloc([128, T], BF16, {1, 2})
        w_uq = A.alloc([128, 4, 768], BF16, {1, 2})
        w_uqr = A.alloc([128, 4, 8, 96], BF16, {1, 2})
        w_ukv = A.alloc([128, 2, 1024], BF16, {1, 2})

        def rstd_from_ss(ss_ap, n_feat, out_ap, tag):
            S.op("act", lambda e: e.activation(out=out_ap, in_=ss_ap, func=AF.Sqrt, scale=1.0 / n_feat, bias=EPS),
                 reads=[tag + "_ss"], writes=[tag + "_r"])
            S.op("dve", lambda e: e.reciprocal(out=out_ap, in_=out_ap), reads=[tag + "_r"], writes=[tag + "_r"])

        def x_to_hnT(c, xt, xsb, junkb, hnT, ssx, hname="hnT"):
            for t4 in range(4):
                g = 4 * c + t4
                par = g % 2
                S.dma("sp", lambda e, g=g, par=par: e.dma_start(out=xt[par], in_=x_d[g * 128:(g + 1) * 128, :]),
                      "x%d" % par, writes=["xt%d" % par])
                S.op("act", lambda e, par=par: e.activation(out=junkb, in_=xt[par], func=AF.Square,
                                                            accum_out=ssx[:, par:par + 1]),
                     reads=["xt%d" % par], writes=["junkb", "x%d_ss" % par])
                rstd_from_ss(ssx[:, par:par + 1], D, ssx[:, 2 + par:3 + par], "x%d" % par)
                S.op("dve", lambda e, par=par: e.tensor_scalar(out=xsb, in0=xt[par], scalar1=ssx[:, 2 + par:3 + par],
                                                               scalar2=None, op0=ALU.mult),
                     reads=["xt%d" % par, "x%d_r" % par], writes=["xsb"])
                Tb = bkb(0).rearrange("p (a b) -> p a b", a=8)
                for kc in range(8):
                    S.op("pe", lambda e, kc=kc: e.transpose(out=Tb[:, kc, :], in_=xsb[:, kc * 128:(kc + 1) * 128],
                                                            identity=ident),
                         reads=["xsb", "ident"], writes=["b0"])
                S.op("dve", lambda e, t4=t4: e.tensor_tensor(
                    out=hnT[:, :, t4 * 128:(t4 + 1) * 128], in0=Tb,
                    in1=col(C_GA, 8).unsqueeze(2).to_broadcast([128, 8, 128]), op=ALU.mult),
                    reads=["b0", "cols"], writes=[hname])

        def norm_fm(zbanks, n_oc, n_feat, gcol, dst, c, sq, sd, tag):
            for oc in range(n_oc):
                S.op("act", lambda e, oc=oc: e.activation(out=sq[:, oc, :], in_=bk(zbanks[oc]), func=AF.Square),
                     reads=["b%d" % zbanks[oc]], writes=["sq"])
            for oc in range(n_oc):
                S.op("pe", lambda e, oc=oc: e.matmul(bk(5), lhsT=ones, rhs=sq[:, oc, :], start=(oc == 0),
                                                     stop=(oc == n_oc - 1)),
                     reads=["sq", "ones"], writes=["b5"])
            S.op("act", lambda e: e.activation(out=sd, in_=bk(5), func=AF.Sqrt, scale=1.0 / n_feat, bias=EPS),
                 reads=["b5"], writes=["sd"])
            S.op("dve", lambda e: e.reciprocal(out=sd, in_=sd), reads=["sd"], writes=["sd"])
            for oc in range(n_oc):
                S.op("dve", lambda e, oc=oc: e.scalar_tensor_tensor(
                    out=dst[:, oc, c * 512:(c + 1) * 512], in0=bk(zbanks[oc]), scalar=col(gcol + oc), in1=sd,
                    op0=ALU.mult, op1=ALU.mult),
                    reads=["b%d" % zbanks[oc], "sd", "cols"], writes=[tag])

        w_in1 = A.alloc([128, 8, 800], BF16, {1})
        wkpe = A.alloc([128, 8, 96], BF16, {1})
        wkper = A.alloc([128, 8, 96], BF16, {1})
        xt = [A.alloc([128, D], F32, {1}), A.alloc([128, D], F32, {1})]
        xsb = A.alloc([128, D], BF16, {1})
        junkb = A.alloc([128, D], BF16, {1})
        hnT2 = [A.alloc([128, 8, 512], BF16, {1}) for _ in range(2)]
        sq = A.alloc([128, 4, 512], BF16, {1})
        sd = A.alloc([128, 512], F32, {1})
        tmpa = A.alloc([128, 512], F32, {1})
        tmpb = A.alloc([128, 512], F32, {1})
        ssx = scr[:, 0:4]

        w_in_v = w_in_d.rearrange("(k p) c -> p k c", p=128)
        S.dma("pool", lambda e: e.dma_start(out=w_in1, in_=w_in_v[:, :, 0:800]), "w0", writes=["w_in1"])
        S.op("dve", lambda e: e.memset(wkpe, 0.0), writes=["wkpe"])
        S.op("dve", lambda e: e.memset(wkper, 0.0), writes=["wkper"])
        S.dma("pool", lambda e: e.dma_start(out=wkpe[:, :, 64:96], in_=w_in_v[:, :, 768:800]), "w1", writes=["wkpe"])
        S.dma("pool", lambda e: e.dma_start(out=wkper[:, :, 64:80], in_=w_in_v[:, :, 784:800]), "w2", writes=["wkper"])
        S.dma("pool", lambda e: e.dma_start(out=wkper[:, :, 80:96], in_=w_in_v[:, :, 768:784]), "w3", writes=["wkper"])
        S.op("dve", lambda e: e.tensor_scalar(out=wkper[:, :, 64:80], in0=wkper[:, :, 64:80], scalar1=-1.0,
                                              scalar2=None, op0=ALU.mult), reads=["wkper"], writes=["wkper"])
        S.dma("pool", lambda e: e.dma_start(out=w_uq, in_=w_uq_d.rearrange("(k p) c -> p k c", p=128)), "w4",
              writes=["w_uq"])
        S.dma("pool", lambda e: e.dma_start(out=w_ukv, in_=w_ukv_d.rearrange("(k p) c -> p k c", p=128)), "w5",
              writes=["w_ukv"])
        S.op("dve", lambda e: e.memset(w_uqr, 0.0), writes=["w_uqr"])
        w_uq_h = w_uq.rearrange("p k (h c) -> p k h c", h=8)
        S.op("dve", lambda e: e.tensor_scalar(out=w_uqr[:, :, :, 64:80], in0=w_uq_h[:, :, :, 80:96], scalar1=-1.0,
                                              scalar2=None, op0=ALU.mult), reads=["w_uq", "w_uqr"], writes=["w_uqr"])
        S.op("dve", lambda e: e.tensor_copy(out=w_uqr[:, :, :, 80:96], in_=w_uq_h[:, :, :, 64:80]),
             reads=["w_uq", "w_uqr"], writes=["w_uqr"])
        for k in range(8):
            rs = slice(k * 2048, (k + 1) * 2048)
            S.dma("pool", lambda e, rs=rs: e.dma_start(out=tab_d[rs, 0:D], in_=u_d[rs, :]), "tbu%d" % k, writes=["tabu%d" % k])
            S.dma("pool", lambda e, rs=rs: e.dma_start(out=tab_d[rs, D:2 * D], in_=v_d[rs, :]), "tbv%d" % k, writes=["tabv%d" % k])
        S.op("dve", lambda e: e.memset(Vt[:, :, :, 64:65], 1.0), writes=["Vones"])

        posi = xt[0].bitcast(I32)
        ya = xt[1]
        ki = tmpa.bitcast(I32)
        R = slice(64, 96)
        for blk in range(8):
            cs = slice(blk * 512, (blk + 1) * 512)
            S.dma("sp", lambda e, cs=cs: e.dma_start(out=posi[R, 0:512], in_=pos_d[:, cs]), "x0", writes=["posi"])
            S.op("dve", lambda e: e.tensor_copy(out=ya[R, 0:512], in_=posi[R, 0:512]), reads=["posi"], writes=["ya"])
            S.op("dve", lambda e: e.tensor_scalar(out=ya[R, 0:512], in0=ya[R, 0:512], scalar1=cols[R, C_FREQ:C_FREQ + 1],
                                                  scalar2=1.0 / (2 * np.pi), op0=ALU.mult, op1=ALU.mult),
                 reads=["ya", "cols"], writes=["ya"])
            for which, tab, shift in (("s", sinT, 0.0), ("c", cosT, 0.25)):
                if shift:
                    S.op("dve", lambda e: e.tensor_scalar(out=ya[R, 512:1024], in0=ya[R, 0:512], scalar1=0.25,
                                                          scalar2=None, op0=ALU.add), reads=["ya"], writes=["yb"])
                    src = ya[R, 512:1024]
                    rname = "yb"
                else:
                    src = ya[R, 0:512]
                    rname = "ya"
                S.op("dve", lambda e, src=src: e.tensor_copy(out=ki[R, :], in_=src), reads=[rname], writes=["ki"])
                S.op("dve", lambda e: e.tensor_copy(out=tmpb[R, :], in_=ki[R, :]), reads=["ki"], writes=["tmpb"])
                S.op("dve", lambda e, src=src: e.tensor_tensor(out=tmpb[R, :], in0=src, in1=tmpb[R, :], op=ALU.subtract),
                     reads=[rname, "tmpb"], writes=["tmpb"])
                S.op("act", lambda e, tab=tab, cs=cs: e.activation(out=tab[R, cs], in_=tmpb[R, :], func=AF.Sin,
                                                                   scale=6.28318),
                     reads=["tmpb"], writes=["rope" + which])

        S.barrier()
        if upto >= 1:
            for c in range(NCH):
                hnT = hnT2[c % 2]
                HN = "hnT%d" % (c % 2)
                x_to_hnT(c, xt, xsb, junkb, hnT, ssx, HN)
                for oc in range(4):
                    for kc in range(8):
                        S.op("pe", lambda e, oc=oc, kc=kc, hnT=hnT: e.matmul(bk(1 + oc), lhsT=w_in1[:, kc, oc * 128:(oc + 1) * 128],
                                                                    rhs=hnT[:, kc, :], start=(kc == 0), stop=(kc == 7)),
                             reads=[HN, "w_in1"], writes=["b%d" % (1 + oc)])
                norm_fm([1, 2, 3, 4], 4, 512, C_GQ, qnT, c, sq, sd, "qnT")
                for oc in range(2):
                    for kc in range(8):
                        S.op("pe", lambda e, oc=oc, kc=kc, hnT=hnT: e.matmul(bk(1 + oc), lhsT=w_in1[:, kc, 512 + oc * 128:512 + (oc + 1) * 128],
                                                                    rhs=hnT[:, kc, :], start=(kc == 0), stop=(kc == 7)),
                             reads=[HN, "w_in1"], writes=["b%d" % (1 + oc)])
                norm_fm([1, 2], 2, 256, C_GKV, kvnT, c, sq, sd, "kvnT")
                for kc in range(8):
                    S.op("pe", lambda e, kc=kc, hnT=hnT: e.matmul(bk(6)[0:96, :], lhsT=wkpe[:, kc, :], rhs=hnT[:, kc, :],
                                                         start=(kc == 0), stop=(kc == 7)),
                         reads=[HN, "wkpe"], writes=["b6"])
                for kc in range(8):
                    S.op("pe", lambda e, kc=kc, hnT=hnT: e.matmul(bk(7)[0:96, :], lhsT=wkper[:, kc, :], rhs=hnT[:, kc, :],
                                                         start=(kc == 0), stop=(kc == 7)),
                         reads=[HN, "wkper"], writes=["b7"])
                cs = slice(c * 512, (c + 1) * 512)
                S.op("dve", lambda e, cs=cs: e.tensor_tensor(out=tmpa[R, :], in0=bk(6)[R, :], in1=cosT[R, cs], op=ALU.mult),
                     reads=["b6", "ropec"], writes=["tmpa"])
                S.op("dve", lambda e, cs=cs: e.tensor_tensor(out=tmpb[R, :], in0=bk(7)[R, :], in1=sinT[R, cs], op=ALU.mult),
                     reads=["b7", "ropes"], writes=["tmpb"])
                S.op("dve", lambda e, cs=cs: e.tensor_tensor(out=kpeT[R, cs], in0=tmpa[R, :], in1=tmpb[R, :], op=ALU.add),
                     reads=["tmpa", "tmpb"], writes=["kpeT"])
                w_ukv_h = w_ukv.rearrange("p k (h c) -> p k h c", h=8)
                for t4 in range(4):
                    g = 4 * c + t4
                    vb = 6 + (t4 % 2)
                    for kc in range(2):
                        S.op("pe", lambda e, kc=kc, g=g, vb=vb: e.matmul(
                            bk(vb).rearrange("p (h c) -> p h c", h=8), lhsT=kvnT[:, kc, g * 128:(g + 1) * 128],
                            rhs=w_ukv_h[:, kc, :, 64:128], start=(kc == 0), stop=(kc == 1)),
                            reads=["kvnT", "w_ukv"], writes=["b%d" % vb])
                    S.op("act", lambda e, g=g, vb=vb: e.copy(out=Vt[:, g, :, 0:64],
                                                            in_=bk(vb).rearrange("p (h c) -> p h c", h=8)),
                         reads=["b%d" % vb], writes=["V"])
        S.barrier()

        QT = [A.alloc([128, T], BF16, {2}), A.alloc([128, T], BF16, {2})]
        KT = [A.alloc([128, T], BF16, {2}), A.alloc([128, T], BF16, {2})]
        NPT = 5
        PT = [A.alloc([128, 512], BF16, {2}) for _ in range(NPT)]
        sqq = A.alloc([128, 512], BF16, {2})
        t2a = A.alloc([128, 512], F32, {2})
        t2b = A.alloc([128, 512], F32, {2})
        mxq = A.alloc([128, 16], F32, {2})
        negm = A.alloc([128, 8], F32, {2})
        rec = A.alloc([128, 8], F32, {2})
        w_ukv_h = w_ukv.rearrange("p k (h c) -> p k h c", h=8)

        def build_qk(h):
            hp = h % 2
            qt, kt = QT[hp], KT[hp]
            S.op("pool", lambda e: e.tensor_copy(out=kt[R, :], in_=kpeT[R, :]), reads=["kpeT"], writes=["KT%d" % hp])
            for c in range(NCH):
                cs = slice(c * 512, (c + 1) * 512)
                for kc in range(4):
                    S.op("pe", lambda e, kc=kc, cs=cs: e.matmul(bk(0)[0:96, :], lhsT=w_uq[:, kc, h * 96:(h + 1) * 96],
                                                                rhs=qnT[:, kc, cs], start=(kc == 0), stop=(kc == 3)),
                         reads=["qnT", "w_uq"], writes=["b0"])
                for kc in range(4):
                    S.op("pe", lambda e, kc=kc, cs=cs: e.matmul(bk(1)[0:96, :], lhsT=w_uqr[:, kc, h, :],
                                                                rhs=qnT[:, kc, cs], start=(kc == 0), stop=(kc == 3)),
                         reads=["qnT", "w_uqr"], writes=["b1"])
                for kc in range(2):
                    S.op("pe", lambda e, kc=kc, cs=cs: e.matmul(bk(2)[0:64, :], lhsT=w_ukv_h[:, kc, h, 0:64],
                                                                rhs=kvnT[:, kc, cs], start=(kc == 0), stop=(kc == 1)),
                         reads=["kvnT", "w_ukv"], writes=["b2"])
                S.op("act", lambda e, cs=cs: e.copy(out=qt[0:64, cs], in_=bk(0)[0:64, :]), reads=["b0"],
                     writes=["QT%d" % hp])
                S.op("dve", lambda e, cs=cs: e.tensor_tensor(out=t2a[R, :], in0=bk(0)[R, :], in1=cosT[R, cs], op=ALU.mult),
                     reads=["b0", "ropec"], writes=["t2a"])
                S.op("dve", lambda e, cs=cs: e.tensor_tensor(out=t2b[R, :], in0=bk(1)[R, :], in1=sinT[R, cs], op=ALU.mult),
                     reads=["b1", "ropes"], writes=["t2b"])
                S.op("dve", lambda e, cs=cs: e.tensor_tensor(out=qt[R, cs], in0=t2a[R, :], in1=t2b[R, :], op=ALU.add),
                     reads=["t2a", "t2b"], writes=["QT%d" % hp])
                S.op("act", lambda e, cs=cs: e.copy(out=kt[0:64, cs], in_=bk(2)[0:64, :]), reads=["b2"],
                     writes=["KT%d" % hp])
                for which, src, col0 in (("q", qt, 0), ("k", kt, 8)):
                    S.op("act", lambda e, src=src, cs=cs: e.activation(out=sqq[0:96, :], in_=src[0:96, cs], func=AF.Square),
                         reads=[("QT%d" if which == "q" else "KT%d") % hp], writes=["sqq"])
                    S.op("pe", lambda e: e.matmul(bk(2), lhsT=ones[0:96, :], rhs=sqq[0:96, :], start=True, stop=True),
                         reads=["sqq", "ones"], writes=["b2"])
                    S.op("dve", lambda e, col0=col0, c=c: e.reduce_max(out=mxq[:, col0 + c:col0 + c + 1], in_=bk(2), axis=AX.X),
                         reads=["b2"], writes=["mxq"])
            S.op("dve", lambda e: e.reduce_max(out=scr[:, 8:9], in_=mxq[:, 0:8], axis=AX.X), reads=["mxq"], writes=["scr8"])
            S.op("dve", lambda e: e.reduce_max(out=scr[:, 9:10], in_=mxq[:, 8:16], axis=AX.X), reads=["mxq"], writes=["scr9"])
            S.op("dve", lambda e: e.tensor_tensor(out=scr[:, 10:11], in0=scr[:, 8:9], in1=scr[:, 9:10], op=ALU.mult),
                 reads=["scr8", "scr9"], writes=["scr10"])
            S.op("act", lambda e: e.activation(out=scr[:, 11:12], in_=scr[:, 10:11], func=AF.Sqrt), reads=["scr10"],
                 writes=["scr11"])
            S.op("dve", lambda e: e.tensor_scalar(out=negm[:, h:h + 1], in0=scr[:, 11:12], scalar1=-ATT_SCALE, scalar2=None,
                                                  op0=ALU.mult), reads=["scr11"], writes=["negm%d" % h])

        def attn(h):
            hp = h % 2
            qt, kt = QT[hp], KT[hp]
            steps = []
            for j in range(NCH):
                for i in range(4 * j + 4):
                    steps.append((j, i))

            def emit_S(n):
                j, i = steps[n]
                q0 = max(512 * j, 128 * i)
                w = 512 * j + 512 - q0
                sb_ = 3 + (n % 3)
                pt = PT[n % NPT]
                ptn = "PT%d" % (n % NPT)
                S.op("pe", lambda e: e.matmul(bk(sb_)[:, 0:w], lhsT=kt[0:96, i * 128:(i + 1) * 128], rhs=qt[0:96, q0:q0 + w],
                                              start=True, stop=True),
                     reads=["QT%d" % hp, "KT%d" % hp], writes=["b%d" % sb_])
                S.op("act", lambda e: e.activation(out=pt[:, 0:w], in_=bk(sb_)[:, 0:w], func=AF.Exp, scale=ATT_SCALE,
                                                   bias=negm[:, h:h + 1]),
                     reads=["b%d" % sb_, "negm%d" % h], writes=[ptn])
                if 128 * i >= 512 * j:
                    S.op("pool", lambda e: e.tensor_tensor(out=pt[:, 0:128], in0=pt[:, 0:128], in1=tri, op=ALU.mult),
                         reads=[ptn, "tri"], writes=[ptn])

            def emit_PV(n):
                j, i = steps[n]
                q0 = max(512 * j, 128 * i)
                r0 = (q0 - 512 * j) // 128
                pt = PT[n % NPT]
                ptn = "PT%d" % (n % NPT)
                ob = 6 + (j % 2)
                O = bk(ob)[:, 0:260].rearrange("p (r c) -> p r c", r=4)
                for rr in range(r0, 4):
                    S.op("pe", lambda e, rr=rr: e.matmul(O[:, rr, :], lhsT=pt[:, (rr - r0) * 128:(rr - r0 + 1) * 128], rhs=Vt[:, i, h, :],
                                                         start=(i == 0 and rr == 0), stop=(i == 4 * j + 3 and rr == 3)),
                         reads=[ptn, "V", "Vones"], writes=["b%d" % ob])
                if i == 4 * j + 3:
                    S.op("dve", lambda e: e.reciprocal(out=rec[:, 0:4], in_=O[:, :, 64]), reads=["b%d" % ob], writes=["rec"])
                    S.op("dve", lambda e: e.tensor_tensor(out=att[:, 4 * j:4 * j + 4, h * 64:(h + 1) * 64], in0=O[:, :, 0:64],
                                                          in1=rec[:, 0:4].unsqueeze(2).to_broadcast([128, 4, 64]), op=ALU.mult),
                         reads=["b%d" % ob, "rec"], writes=["att"])

            emit_S(0)
            emit_S(1)
            for n in range(len(steps)):
                if n + 2 < len(steps):
                    emit_S(n + 2)
                emit_PV(n)

        if upto >= 2:
            build_qk(0)
            for h in range(8):
                if h + 1 < 8:
                    build_qk(h + 1)
                attn(h)
        if debug and upto <= 2:
            for nm, src in (("d_qnT", qnT), ("d_kvnT", kvnT), ("d_kpeT", kpeT), ("d_V", Vt), ("d_att", att)):
                flat = src
                if len(src.shape) == 3:
                    flat = src.rearrange("p a b -> p (a b)")
                elif len(src.shape) == 4:
                    flat = src.rearrange("p a b c -> p (a b c)")
                S.barrier()
                S.dma("sp", lambda e, nm=nm, flat=flat: e.dma_start(out=dbg_d[nm], in_=flat), "dbg", final=True)
        S.barrier()

        w_in3 = A.alloc([128, 8, 1024], BF16, {3})
        dg = A.alloc([128, 4, 31, 128], BF16, {3})
        xt3 = [A.alloc([128, D], F32, {3}), A.alloc([128, D], F32, {3})]
        xsb3 = A.alloc([128, D], BF16, {3})
        junkb3 = A.alloc([128, D], BF16, {3})
        hnT32 = [A.alloc([128, 8, 512], BF16, {3}) for _ in range(2)]
        glu = [A.alloc([128, 4, 544], BF16, {3}), A.alloc([128, 4, 544], BF16, {3})]
        sig = A.alloc([128, 512], F32, {3})
        y = A.alloc([128, 4, 512], F32, {3})
        ybf = A.alloc([128, 4, 512], BF16, {3})
        mean = A.alloc([128, 512], F32, {3})
        sd3 = A.alloc([128, 512], F32, {3})
        ssx3 = scr[:, 16:20]
        if upto >= 3:
            S.dma("pool", lambda e: e.dma_start(out=w_in3, in_=w_in_v[:, :, 800:1824]), "w0", writes=["w_in3"])
            cw = cols[:, C_CW:C_CW + 124].rearrange("p (j k) -> p j k", j=4)
            for j in range(4):
                for k in range(31):
                    eng = "dve" if (k % 2 == 0) else "pool"
                    S.op(eng, lambda e, j=j, k=k: e.tensor_scalar(out=dg[:, j, k, :], in0=ident, scalar1=cw[:, j, k:k + 1],
                                                                  scalar2=None, op0=ALU.mult),
                         reads=["ident", "cols"], writes=["dg"])
            S.op("pool", lambda e: e.memset(glu[1][:, :, 0:32], 0.0), writes=["glu1"])
            for c in range(NCH):
                gp = c % 2
                hnT3 = hnT32[c % 2]
                HN3 = "hnT%d" % (c % 2)
                x_to_hnT(c, xt3, xsb3, junkb3, hnT3, ssx3, HN3)
                if c > 0:
                    S.op("pool", lambda e, gp=gp: e.tensor_copy(out=glu[gp][:, :, 0:32], in_=glu[1 - gp][:, :, 512:544]),
                         reads=["glu%d" % (1 - gp)], writes=["glu%d" % gp])
                else:
                    S.op("pool", lambda e: e.memset(glu[0][:, :, 0:32], 0.0), writes=["glu0"])
                for j in range(4):
                    for kc in range(8):
                        S.op("pe", lambda e, j=j, kc=kc, hnT3=hnT3: e.matmul(bk(1), lhsT=w_in3[:, kc, j * 128:(j + 1) * 128],
                                                                  rhs=hnT3[:, kc, :], start=(kc == 0), stop=(kc == 7)),
                             reads=[HN3, "w_in3"], writes=["b1"])
                    for kc in range(8):
                        S.op("pe", lambda e, j=j, kc=kc, hnT3=hnT3: e.matmul(bk(2), lhsT=w_in3[:, kc, 512 + j * 128:512 + (j + 1) * 128],
                                                                  rhs=hnT3[:, kc, :], start=(kc == 0), stop=(kc == 7)),
                             reads=[HN3, "w_in3"], writes=["b2"])
                    S.op("act", lambda e: e.activation(out=sig, in_=bk(2), func=AF.Sigmoid), reads=["b2"], writes=["sig"])
                    S.op("dve", lambda e, j=j, gp=gp: e.tensor_tensor(out=glu[gp][:, j, 32:544], in0=bk(1), in1=sig, op=ALU.mult),
                         reads=["b1", "sig"], writes=["glu%d" % gp])
                    yb = 3 + (j % 2)
                    for k in range(31):
                        S.op("pe", lambda e, j=j, k=k, gp=gp, yb=yb: e.matmul(bk(yb), lhsT=dg[:, j, k, :],
                                                                              rhs=glu[gp][:, j, 2 + k:2 + k + 512],
                                                                              start=(k == 0), stop=(k == 30)),
                             reads=["glu%d" % gp, "dg"], writes=["b%d" % yb])
                    S.op("act", lambda e, j=j, yb=yb: e.activation(out=y[:, j, :], in_=bk(yb), func=AF.Identity,
                                                                  bias=col(C_CB + j)),
                         reads=["b%d" % yb, "cols"], writes=["y%d" % j])
                    S.op("act", lambda e, j=j: e.copy(out=ybf[:, j, :], in_=y[:, j, :]), reads=["y%d" % j], writes=["ybf"])
                for j in range(4):
                    S.op("pe", lambda e, j=j: e.matmul(bk(5), lhsT=ones, rhs=ybf[:, j, :], start=(j == 0), stop=(j == 3)),
                         reads=["ybf", "ones"], writes=["b5"])
                S.op("act", lambda e: e.activation(out=mean, in_=bk(5), func=AF.Copy, scale=1.0 / 512), reads=["b5"],
                     writes=["mean"])
                for j in range(4):
                    S.op("dve", lambda e, j=j: e.tensor_tensor(out=y[:, j, :], in0=y[:, j, :], in1=mean, op=ALU.subtract),
                         reads=["y%d" % j, "mean"], writes=["y%d" % j])
                    S.op("act", lambda e, j=j: e.activation(out=ybf[:, j, :], in_=y[:, j, :], func=AF.Square),
                         reads=["y%d" % j], writes=["ybf"])
                for j in range(4):
                    S.op("pe", lambda e, j=j: e.matmul(bk(5), lhsT=ones, rhs=ybf[:, j, :], start=(j == 0), stop=(j == 3)),
                         reads=["ybf", "ones"], writes=["b5"])
                S.op("act", lambda e: e.activation(out=sd3, in_=bk(5), func=AF.Sqrt, scale=1.0 / 512, bias=EPS),
                     reads=["b5"], writes=["sd3"])
                S.op("dve", lambda e: e.reciprocal(out=sd3, in_=sd3), reads=["sd3"], writes=["sd3"])
                for j in range(4):
                    S.op("dve", lambda e, j=j: e.tensor_tensor(out=y[:, j, :], in0=y[:, j, :], in1=sd3, op=ALU.mult),
                         reads=["y%d" % j, "sd3"], writes=["y%d" % j])
                    S.op("act", lambda e, j=j: e.activation(out=y[:, j, :], in_=y[:, j, :], func=AF.Silu,
                                                            scale=col(C_LNG + j), bias=col(C_LNB + j)),
                         reads=["y%d" % j, "cols"], writes=["y%d" % j])
                    S.op("act", lambda e, j=j: e.activation(out=ybf[:, j, :], in_=y[:, j, :], func=AF.Square),
                         reads=["y%d" % j], writes=["ybf"])
                for j in range(4):
                    S.op("pe", lambda e, j=j: e.matmul(bk(5), lhsT=ones, rhs=ybf[:, j, :], start=(j == 0), stop=(j == 3)),
                         reads=["ybf", "ones"], writes=["b5"])
                S.op("act", lambda e: e.activation(out=sd3, in_=bk(5), func=AF.Sqrt, scale=1.0 / 512, bias=EPS),
                     reads=["b5"], writes=["sd3"])
                S.op("dve", lambda e: e.reciprocal(out=sd3, in_=sd3), reads=["sd3"], writes=["sd3"])
                for j in range(4):
                    S.op("dve", lambda e, j=j, c=c: e.scalar_tensor_tensor(
                        out=cnT[:, j, c * 512:(c + 1) * 512], in0=y[:, j, :], scalar=col(C_CG + j), in1=sd3,
                        op0=ALU.mult, op1=ALU.mult), reads=["y%d" % j, "sd3", "cols"], writes=["cnT"])
        if debug and upto == 3:
            S.barrier()
            S.dma("sp", lambda e: e.dma_start(out=dbg_d["d_cnT"], in_=cnT.rearrange("p a b -> p (a b)")), "dbg", final=True)
            S.dma("sp", lambda e: e.dma_start(out=dbg_d["d_att"], in_=att.rearrange("p a b -> p (a b)")), "dbg", final=True)
        S.barrier()

        if upto >= 4:
            w_out = A.alloc([128, 8, D], BF16, {4})
            wq = A.alloc([128, 8, D], BF16, {4})
            kbd = A.alloc([128, 8, 256], BF16, {4})
            gw = A.alloc([128, 8, D], BF16, {4})
            pw = A.alloc([128, 2, D], BF16, {4})
            g_ffn = A.alloc([128, D], BF16, {4})
            g_b = A.alloc([128, D], F32, {4})
            g_fin = A.alloc([128, D], F32, {4})
            NB, GRP = 9, 2
            gball = A.alloc([128, NB, 2 * D], BF16, {4})
            gb = [gball[:, b, :] for b in range(NB)]
            dgs = [A.alloc([128, 128], BF16, {4}) for _ in range(4)]
            gl = A.alloc([128, 128], F32, {4})
            h1s = [A.alloc([128, D], F32, {4}) for _ in range(2)]
            xnbs = [A.alloc([128, D], BF16, {4}) for _ in range(2)]
            tmp = A.alloc([128, D], F32, {4})
            bfa = A.alloc([128, D], BF16, {4})
            xT = A.alloc([128, 8, 128], BF16, {4})
            mp = A.alloc([128, 256], F32, {4})
            mixT = mp.bitcast(BF16).rearrange("p (a b) -> p a b", a=4)
            tk = A.alloc([128, 2048], F32, {4})
            sc = tk.rearrange("p (a b) -> p a b", a=16)
            cand = tk.rearrange("p (h c) -> p h c", h=8)
            oh = tk.rearrange("p (h a b) -> p h a b", h=8, a=16)
            wk = A.alloc([128, 256], F32, {4})
            m16 = A.alloc([128, 16, 16], F32, {4})
            ix16 = A.alloc([128, 16, 16], U32, {4})
            ixf = ix16.bitcast(F32)
            best = A.alloc([128, 8, 16], F32, {4})
            posu = A.alloc([128, 8, 16], U32, {4})
            posf = A.alloc([128, 8, 16], F32, {4})
            ki4 = posu.bitcast(I32)
            k1f = A.alloc([128, 8, 16], F32, {4})
            k2f = A.alloc([128, 8, 16], F32, {4})
            e1 = A.alloc([128, 8, 16], F32, {4})
            e2 = posf
            eidxs = [A.alloc([128, 128], I32, {4}) for _ in range(2)]
            gate16s = [A.alloc([128, 8, 16], F32, {4}) for _ in range(2)]
            actv = A.alloc([128, 128], F32, {4})
            coef = A.alloc([128, 128], F32, {4})
            pt_ = mp
            pbf = A.alloc([128, 256], BF16, {4})
            pT = A.alloc([128, 2, 128], BF16, {4})
            gsum = A.alloc([128, 8], F32, {4})
            ss4 = scr[:, 24:40]

            def wload(dst, src, ch, name):
                S.dma("pool", lambda e: e.dma_start(out=dst, in_=src.rearrange("(k p) c -> p k c", p=128)), ch, writes=[name])
            wload(w_out, w_out_d, "w0", "w_out")
            wload(wq, wq_d, "w1", "wq")
            wload(gw, gw_d, "w2", "gw")
            wload(pw, pw_d, "w3", "pw")
            S.op("dve", lambda e: e.memset(kbd, 0.0), writes=["kbd"])
            S.dma("pool", lambda e: e.dma_start(out=kbd[0:64, :, 0:128], in_=keysT_d[0:64]), "w4", writes=["kbd"])
            S.dma("pool", lambda e: e.dma_start(out=kbd[64:128, :, 128:256], in_=keysT_d[64:128]), "w5", writes=["kbd"])
            S.dma("pool", lambda e: e.dma_start(out=g_ffn, in_=rows_d[0]), "c0", writes=["g_ffn"])
            S.dma("sp", lambda e: e.dma_start(out=g_b, in_=rows_d[1]), "c1", writes=["g_b"])
            S.dma("sp", lambda e: e.dma_start(out=g_fin, in_=rows_d[2]), "c2", writes=["g_fin"])

            class Rec:
                def __init__(self):
                    self.q = []
                def op(self, *a, **k):
                    self.q.append(lambda: S.op(*a, **k))
                def dma(self, *a, **k):
                    self.q.append(lambda: S.dma(*a, **k))
                def brk(self):
                    self.q.append(None)

            def rms_tile(X, src, srcname, k, n_feat, junk, junkname):
                X.op("act", lambda e: e.activation(out=junk, in_=src, func=AF.Square, accum_out=ss4[:, k:k + 1]),
                     reads=[srcname], writes=[junkname, "r%d_ss" % k])
                X.op("act", lambda e: e.activation(out=ss4[:, k + 8:k + 9], in_=ss4[:, k:k + 1], func=AF.Sqrt, scale=1.0 / n_feat, bias=EPS),
                     reads=["r%d_ss" % k], writes=["r%d_r" % k])
                X.brk()
                X.op("dve", lambda e: e.reciprocal(out=ss4[:, k + 8:k + 9], in_=ss4[:, k + 8:k + 9]), reads=["r%d_r" % k], writes=["r%d_r" % k])
                return ss4[:, k + 8:k + 9], "r%d_r" % k

            Tb = bkb(0).rearrange("p (a b) -> p a b", a=8)
            Tp = bkb(3).rearrange("p (a b) -> p a b", a=8)

            def A_ops(g):
                X = Rec()
                pp = g % 2
                H, Hn = h1s[pp], "h1_%d" % pp
                xnb, xnbn = xnbs[pp], "xnb_%d" % pp
                eidx, eidxn = eidxs[pp], "eidx_%d" % pp
                gate16, gaten = gate16s[pp], "gate_%d" % pp
                ts_ = slice(g * 128, (g + 1) * 128)
                X.dma("sp", lambda e: e.dma_start(out=H, in_=x_d[ts_, :]), "x0", writes=[Hn])
                att_t = att[:, g, :]
                ra, ran = rms_tile(X, att_t, "att", 0, 512, bfa[:, 0:512], "bfa")
                X.op("dve", lambda e: e.tensor_scalar(out=bfa[:, 0:512], in0=att_t, scalar1=ra, scalar2=None, op0=ALU.mult),
                     reads=["att", ran], writes=["bfa"])
                for kc in range(4):
                    X.op("pe", lambda e, kc=kc: e.transpose(out=Tb[:, kc, :], in_=bfa[:, kc * 128:(kc + 1) * 128], identity=ident),
                         reads=["bfa", "ident"], writes=["b0"])
                X.brk()
                X.op("dve", lambda e: e.tensor_tensor(out=mixT, in0=Tb[:, 0:4, :],
                                                      in1=col(C_GAO, 4).unsqueeze(2).to_broadcast([128, 4, 128]), op=ALU.mult),
                     reads=["b0", "cols"], writes=["mixT"])
                for half in range(2):
                    for kc in range(8):
                        lhs = mixT[:, kc, :] if kc < 4 else cnT[:, kc - 4, ts_]
                        X.op("pe", lambda e, half=half, kc=kc, lhs=lhs: e.matmul(bk(3 + half), lhsT=lhs,
                                                                                 rhs=w_out[:, kc, half * 512:(half + 1) * 512],
                                                                                 start=(kc == 0), stop=(kc == 7)),
                             reads=["mixT", "cnT", "w_out"], writes=["b%d" % (3 + half)])
                X.brk()
                for half in range(2):
                    X.op("dve", lambda e, half=half: e.tensor_tensor(out=H[:, half * 512:(half + 1) * 512],
                                                                     in0=H[:, half * 512:(half + 1) * 512], in1=bk(3 + half), op=ALU.add),
                         reads=[Hn, "b%d" % (3 + half)], writes=[Hn])
                r1, r1n = rms_tile(X, H, Hn, 1, D, tmp, "tmp")
                X.op("dve", lambda e: e.scalar_tensor_tensor(out=xnb, in0=H, scalar=r1, in1=g_ffn, op0=ALU.mult, op1=ALU.mult),
                     reads=[Hn, r1n, "g_ffn"], writes=[xnbn])
                for kc in range(8):
                    X.op("pe", lambda e, kc=kc: e.transpose(out=Tb[:, kc, :], in_=xnb[:, kc * 128:(kc + 1) * 128], identity=ident),
                         reads=[xnbn, "ident"], writes=["b0"])
                X.brk()
                X.op("act", lambda e: e.copy(out=xT, in_=Tb), reads=["b0"], writes=["xT"])
                for half in range(2):
                    for kc in range(8):
                        X.op("pe", lambda e, half=half, kc=kc: e.matmul(bk(3 + half), lhsT=xT[:, kc, :],
                                                                        rhs=wq[:, kc, half * 512:(half + 1) * 512],
                                                                        start=(kc == 0), stop=(kc == 7)),
                             reads=["xT", "wq"], writes=["b%d" % (3 + half)])
                X.brk()
                for half in range(2):
                    X.op("act", lambda e, half=half: e.copy(out=bfa[:, half * 512:(half + 1) * 512], in_=bk(3 + half)),
                         reads=["b%d" % (3 + half)], writes=["bfa"])
                for kc in range(8):
                    X.op("pe", lambda e, kc=kc: e.transpose(out=Tb[:, kc, :], in_=bfa[:, kc * 128:(kc + 1) * 128], identity=ident),
                         reads=["bfa", "ident"], writes=["b0"])
                X.brk()
                X.op("dve", lambda e: e.tensor_copy(out=xT, in_=Tb), reads=["b0"], writes=["xT"])
                for hh in range(8):
                    sbk = 4 + hh // 2
                    X.op("pe", lambda e, hh=hh, sbk=sbk: e.matmul(bk(sbk)[:, (hh % 2) * 256:(hh % 2 + 1) * 256], lhsT=xT[:, hh, :],
                                                                 rhs=kbd[:, hh, :], start=True, stop=True),
                         reads=["xT", "kbd"], writes=["b%d" % sbk])
                X.brk()
                for b4 in range(4):
                    X.op("act", lambda e, b4=b4: e.copy(out=tk[:, b4 * 512:(b4 + 1) * 512], in_=bk(4 + b4)),
                         reads=["b%d" % (4 + b4)], writes=["tk"])
                X.brk()
                for hc in range(16):
                    X.op("dve", lambda e, hc=hc: e.max(out=m16[:, hc, 0:8], in_=sc[:, hc, :]), reads=["tk"], writes=["m16"])
                    X.op("dve", lambda e, hc=hc: e.max_index(out=ix16[:, hc, 0:8], in_max=m16[:, hc, 0:8], in_values=sc[:, hc, :]),
                         reads=["tk", "m16"], writes=["ix16"])
                    X.op("dve", lambda e, hc=hc: e.match_replace(out=wk[:, 0:128], in_to_replace=m16[:, hc, 0:8],
                                                                 in_values=sc[:, hc, :], imm_value=-1e30),
                         reads=["tk", "m16"], writes=["wk"])
                    X.op("dve", lambda e, hc=hc: e.max(out=m16[:, hc, 8:16], in_=wk[:, 0:128]), reads=["wk"], writes=["m16"])
                    X.op("dve", lambda e, hc=hc: e.max_index(out=ix16[:, hc, 8:16], in_max=m16[:, hc, 8:16], in_values=wk[:, 0:128]),
                         reads=["wk", "m16"], writes=["ix16"])
                m4 = m16.rearrange("p (h c) k -> p h c k", c=2)
                X.op("dve", lambda e: e.tensor_tensor(out=cand.rearrange("p h (a b) -> p h a b", a=16),
                                                      in0=m4[:, :, 0, :].unsqueeze(3).to_broadcast([128, 8, 16, 16]),
                                                      in1=m4[:, :, 1, :].unsqueeze(2).to_broadcast([128, 8, 16, 16]), op=ALU.add),
                     reads=["m16", "tk"], writes=["tk"])
                for hh in range(8):
                    X.op("dve", lambda e, hh=hh: e.max(out=best[:, hh, 0:8], in_=cand[:, hh, :]), reads=["tk"], writes=["best"])
                    X.op("dve", lambda e, hh=hh: e.max_index(out=posu[:, hh, 0:8], in_max=best[:, hh, 0:8], in_values=cand[:, hh, :]),
                         reads=["tk", "best"], writes=["posu"])
                    X.op("dve", lambda e, hh=hh: e.match_replace(out=wk, in_to_replace=best[:, hh, 0:8], in_values=cand[:, hh, :],
                                                                 imm_value=-1e30), reads=["tk", "best"], writes=["wk"])
                    X.op("dve", lambda e, hh=hh: e.max(out=best[:, hh, 8:16], in_=wk), reads=["wk"], writes=["best"])
                    X.op("dve", lambda e, hh=hh: e.max_index(out=posu[:, hh, 8:16], in_max=best[:, hh, 8:16], in_values=wk),
                         reads=["wk", "best"], writes=["posu"])
                X.op("dve", lambda e: e.tensor_copy(out=posf, in_=posu), reads=["posu"], writes=["posf"])
                X.op("dve", lambda e: e.tensor_copy(out=ixf, in_=ix16), reads=["ix16"], writes=["ix16"])
                X.op("dve", lambda e: e.tensor_scalar(out=k1f, in0=posf, scalar1=-7.5, scalar2=0.0625, op0=ALU.add, op1=ALU.mult),
                     reads=["posf"], writes=["k1f"])
                X.op("dve", lambda e: e.tensor_copy(out=ki4, in_=k1f), reads=["k1f", "posf"], writes=["posu"])
                X.op("dve", lambda e: e.tensor_copy(out=k1f, in_=ki4), reads=["posu"], writes=["k1f"])
                X.op("dve", lambda e: e.scalar_tensor_tensor(out=k2f, in0=k1f, scalar=-16.0, in1=posf, op0=ALU.mult, op1=ALU.add),
                     reads=["k1f", "posf"], writes=["k2f"])
                ix4 = ixf.rearrange("p (h c) k -> p h c k", c=2)
                io_b = iota16.unsqueeze(1).unsqueeze(1).to_broadcast([128, 8, 16, 16])
                for kf, cc, eo, nm in ((k1f, 0, e1, "e1"), (k2f, 1, e2, "posf")):
                    X.op("dve", lambda e, kf=kf: e.tensor_tensor(out=oh, in0=kf.unsqueeze(3).to_broadcast([128, 8, 16, 16]),
                                                                 in1=io_b, op=ALU.is_equal),
                         reads=["k1f", "k2f", "iota16", "tk"], writes=["tk"])
                    X.op("dve", lambda e, cc=cc: e.tensor_tensor(out=oh, in0=oh,
                                                                 in1=ix4[:, :, cc, :].unsqueeze(2).to_broadcast([128, 8, 16, 16]),
                                                                 op=ALU.mult), reads=["tk", "ix16"], writes=["tk"])
                    X.op("dve", lambda e, eo=eo: e.reduce_sum(out=eo, in_=oh, axis=AX.X), reads=["tk"], writes=[nm])
                X.op("dve", lambda e: e.scalar_tensor_tensor(out=e1, in0=e1, scalar=128.0, in1=e2, op0=ALU.mult, op1=ALU.add),
                     reads=["e1", "posf"], writes=["e1"])
                X.op("dve", lambda e: e.tensor_copy(out=eidx, in_=e1.rearrange("p h k -> p (h k)")), reads=["e1"], writes=[eidxn])
                X.op("dve", lambda e: e.tensor_tensor(out=gate16, in0=best, in1=best[:, :, 0:1].to_broadcast([128, 8, 16]),
                                                      op=ALU.subtract), reads=["best"], writes=[gaten])
                X.op("act", lambda e: e.activation(out=gate16, in_=gate16, func=AF.Exp), reads=[gaten], writes=[gaten])
                X.brk()
                X.op("dve", lambda e: e.reduce_sum(out=gsum, in_=gate16, axis=AX.X), reads=[gaten], writes=["gsum"])
                X.op("dve", lambda e: e.reciprocal(out=gsum, in_=gsum), reads=["gsum"], writes=["gsum"])
                X.op("dve", lambda e: e.tensor_tensor(out=gate16, in0=gate16, in1=gsum.unsqueeze(2).to_broadcast([128, 8, 16]),
                                                      op=ALU.mult), reads=[gaten, "gsum"], writes=[gaten])
                return X.q

            TABS = ["tabu%d" % k for k in range(8)] + ["tabv%d" % k for k in range(8)]

            def B_emit(g, side, per_group):
                pp = g % 2
                H, Hn = h1s[pp], "h1_%d" % pp
                xnb, xnbn = xnbs[pp], "xnb_%d" % pp
                eidx, eidxn = eidxs[pp], "eidx_%d" % pp
                gflat, gaten = gate16s[pp].rearrange("p h k -> p (h k)"), "gate_%d" % pp

                def consume(k):
                    gs = slice(k * GRP, (k + 1) * GRP)
                    S.op("act", lambda e: e.activation(out=gl[:, gs], in_=actv[:, gs], func=AF.Gelu), reads=["actv"], writes=["gl"])
                    S.op("dve", lambda e: e.tensor_tensor(out=coef[:, gs], in0=gl[:, gs], in1=gflat[:, gs], op=ALU.mult),
                         reads=["gl", gaten], writes=["coef"])
                    for s2 in range(k * GRP, (k + 1) * GRP):
                        b2 = s2 % NB
                        dgi = s2 % 4
                        S.op("act", lambda e, s2=s2, dgi=dgi: e.activation(out=dgs[dgi], in_=ident, func=AF.Copy, scale=coef[:, s2:s2 + 1]),
                             reads=["ident", "coef"], writes=["dg%d" % dgi])
                        for half in range(2):
                            S.op("pe", lambda e, s2=s2, b2=b2, dgi=dgi, half=half: e.matmul(
                                bk(1 + half), lhsT=dgs[dgi], rhs=gb[b2][:, D + half * 512:D + (half + 1) * 512],
                                start=(s2 == 0), stop=(s2 == 127)),
                                reads=["dg%d" % dgi, "gb%d" % b2], writes=["b%d" % (1 + half)])

                for s_ in range(128):
                    b = s_ % NB
                    S.dma("pool", lambda e, s_=s_, b=b: e.indirect_dma_start(
                        out=gb[b], out_offset=None, in_=tab_d, in_offset=bass.IndirectOffsetOnAxis(ap=eidx[:, s_:s_ + 1], axis=0)),
                        "g%d" % b, reads=[eidxn] + TABS, writes=["gb%d" % b])
                    if s_ % 2 == 0 or not DOT_SPLIT:
                        S.op("dve", lambda e, s_=s_, b=b: e.scalar_tensor_tensor(out=gb[b][:, 0:D], in0=gb[b][:, 0:D], scalar=1.0, in1=xnb,
                                                                                 op0=ALU.mult, op1=ALU.mult, accum_out=actv[:, s_:s_ + 1]),
                             reads=["gb%d" % b, xnbn], writes=["gb%d" % b, "actv"])
                    else:
                        S.op("dve", lambda e, b=b: e.tensor_tensor(out=gb[b][:, 0:D], in0=gb[b][:, 0:D], in1=xnb, op=ALU.mult),
                             reads=["gb%d" % b, xnbn], writes=["gb%d" % b])
                        S.op("act", lambda e, s_=s_, b=b: e.activation(out=gb[b][:, 0:D], in_=gb[b][:, 0:D], func=AF.Copy,
                                                                       accum_out=actv[:, s_:s_ + 1]),
                             reads=["gb%d" % b], writes=["gb%d" % b, "actv"])
                    if (s_ + 1) % GRP == 0:
                        consume(s_ // GRP)
                        cnt = 0
                        while side:
                            th = side.pop(0)
                            if th is None:
                                if cnt >= 2:
                                    break
                                continue
                            th()
                            cnt += 1
                            if cnt >= per_group:
                                break
                while side:
                    th = side.pop(0)
                    if th is not None:
                        th()
                for half in range(2):
                    S.op("dve", lambda e, half=half: e.tensor_tensor(out=H[:, half * 512:(half + 1) * 512],
                                                                     in0=H[:, half * 512:(half + 1) * 512], in1=bk(1 + half), op=ALU.add),
                         reads=[Hn, "b%d" % (1 + half)], writes=[Hn])

            def C_ops(g):
                X = Rec()
                pp = g % 2
                H, Hn = h1s[pp], "h1_%d" % pp
                ts_ = slice(g * 128, (g + 1) * 128)
                X.dma("sp", lambda e: e.dma_start(out=pt_, in_=p_d[ts_, :]), "x1", writes=["mixT"])
                r2, r2n = rms_tile(X, H, Hn, 2, D, tmp, "tmp")
                X.op("dve", lambda e: e.tensor_scalar(out=bfa, in0=H, scalar1=r2, scalar2=None, op0=ALU.mult),
                     reads=[Hn, r2n], writes=["bfa"])
                X.op("act", lambda e: e.copy(out=pbf, in_=pt_), reads=["mixT"], writes=["pbf"])
                for kc in range(8):
                    X.op("pe", lambda e, kc=kc: e.transpose(out=Tb[:, kc, :], in_=bfa[:, kc * 128:(kc + 1) * 128], identity=ident),
                         reads=["bfa", "ident"], writes=["b0"])
                for kc in range(2):
                    X.op("pe", lambda e, kc=kc: e.transpose(out=Tp[:, kc, :], in_=pbf[:, kc * 128:(kc + 1) * 128], identity=ident),
                         reads=["pbf", "ident"], writes=["b3"])
                X.brk()
                X.op("dve", lambda e: e.tensor_tensor(out=xT, in0=Tb, in1=col(C_GPL, 8).unsqueeze(2).to_broadcast([128, 8, 128]),
                                                      op=ALU.mult), reads=["b0", "cols"], writes=["xT"])
                X.op("act", lambda e: e.copy(out=pT, in_=Tp[:, 0:2, :]), reads=["b3"], writes=["pT"])
                for half in range(2):
                    hs = slice(half * 512, (half + 1) * 512)
                    gbk = 5 + half
                    for kc in range(8):
                        X.op("pe", lambda e, hs=hs, kc=kc, gbk=gbk: e.matmul(bk(gbk), lhsT=xT[:, kc, :], rhs=gw[:, kc, hs],
                                                                             start=(kc == 0), stop=(kc == 7)),
                             reads=["xT", "gw"], writes=["b%d" % gbk])
                X.brk()
                for half in range(2):
                    hs = slice(half * 512, (half + 1) * 512)
                    gbk = 5 + half
                    X.op("dve", lambda e, hs=hs, gbk=gbk: e.tensor_tensor(out=tmp[:, hs], in0=bk(gbk), in1=g_b[:, hs], op=ALU.add),
                         reads=["b%d" % gbk, "g_b", "tmp"], writes=["tmp"])
                    X.op("act", lambda e, hs=hs: e.activation(out=tmp[:, hs], in_=tmp[:, hs], func=AF.Sigmoid), reads=["tmp"],
                         writes=["tmp"])
                    for kc in range(2):
                        X.op("pe", lambda e, hs=hs, kc=kc, gbk=gbk: e.matmul(bk(gbk), lhsT=pT[:, kc, :], rhs=pw[:, kc, hs],
                                                                             start=(kc == 0), stop=(kc == 1)),
                             reads=["pT", "pw"], writes=["b%d" % gbk])
                X.brk()
                for half in range(2):
                    hs = slice(half * 512, (half + 1) * 512)
                    gbk = 5 + half
                    X.op("dve", lambda e, hs=hs, gbk=gbk: e.tensor_tensor(out=tmp[:, hs], in0=tmp[:, hs], in1=bk(gbk), op=ALU.mult),
                         reads=["b%d" % gbk, "tmp"], writes=["tmp"])
                X.op("dve", lambda e: e.tensor_tensor(out=H, in0=H, in1=tmp, op=ALU.add), reads=[Hn, "tmp"], writes=[Hn])
                r3, r3n = rms_tile(X, H, Hn, 3, D, tmp, "tmp")
                X.op("dve", lambda e: e.scalar_tensor_tensor(out=tmp, in0=H, scalar=r3, in1=g_fin, op0=ALU.mult, op1=ALU.mult),
                     reads=[Hn, r3n, "g_fin", "tmp"], writes=["tmp"])
                X.dma("sp", lambda e: e.dma_start(out=out_d[ts_, :], in_=tmp), "st", reads=["tmp"], final=True)
                return X.q

            for th in A_ops(0):
                if th is not None:
                    th()
            for g in range(NT):
                side = []
                if g > 0:
                    side += C_ops(g - 1)
                if g + 1 < NT:
                    side += A_ops(g + 1)
                n_real = sum(1 for th in side if th is not None)
                per_group = max(4, int(1.25 * n_real / (128 // GRP - 4)) + 1)
                B_emit(g, side, per_group)
            for th in C_ops(NT - 1):
                if th is not None:
                    th()
        else:
            z = A.alloc([128, D], F32, {4})
            S.op("dve", lambda e: e.memset(z, 0.0), writes=["z"])
            S.dma("sp", lambda e: e.dma_start(out=out_d[0:128, :], in_=z), "st", reads=["z"], final=True)

        stats = S.emit()
        print("program ops per engine:", stats)
    return nc


def make_in_maps(inputs):
    f = np.float32
    g = lambda k: np.asarray(inputs[k])
    x = g("x").astype(f, copy=False)
    p = g("p").astype(f, copy=False)[0]
    pos = g("positions").astype(np.int32, copy=False)

    def colmaj(v):
        v = np.asarray(v, f).reshape(-1, 128)
        return np.ascontiguousarray(v.T)

    cols = np.zeros((128, NCOL), f)
    cols[:, C_GA:C_GA + 8] = colmaj(g("attn_norm")[0])
    cols[:, C_GQ:C_GQ + 4] = colmaj(g("q_norm")[0])
    cols[:, C_GKV:C_GKV + 2] = colmaj(g("kv_norm")[0])
    cols[:, C_CB:C_CB + 4] = colmaj(g("conv_b")[0])
    cols[:, C_LNG:C_LNG + 4] = colmaj(g("conv_ln_g")[0])
    cols[:, C_LNB:C_LNB + 4] = colmaj(g("conv_ln_b")[0])
    cols[:, C_CG:C_CG + 4] = colmaj(g("conv_out_norm")[0])
    cols[:, C_GAO:C_GAO + 4] = colmaj(g("attn_out_norm")[0])
    cols[:, C_GPL:C_GPL + 8] = colmaj(g("pl_norm")[0])
    half = 16
    freqs = (np.float32(10000.0) ** (-np.arange(half, dtype=f) / np.float32(half))).astype(f)
    for pp in range(64, 96):
        cols[pp, C_FREQ] = freqs[(pp - 64) % 16]
    cw = np.asarray(g("conv_w")[0], f)
    cols[:, C_CW:C_CW + 124] = cw.reshape(31, 4, 128).transpose(2, 1, 0).reshape(128, 124)
    rows = np.stack([np.broadcast_to(np.asarray(g(k), f).reshape(-1)[None, :], (128, D))
                     for k in ("ffn_norm", "pl_gate_b", "final_norm")]).astype(f)
    rows = np.ascontiguousarray(rows)
    cst = np.zeros((3, 128, 128), f)
    cst[0] = np.eye(128, dtype=f)
    cst[1] = np.triu(np.ones((128, 128), f))
    cst[2, :, 0:16] = np.arange(16, dtype=f)[None, :]
    keysT = np.ascontiguousarray(np.asarray(g("peer_keys")[0], f).transpose(1, 3, 0, 2).reshape(128, 8, 128))
    shared = {
        "w_in": np.ascontiguousarray(g("w_in")[0], f), "w_uq": np.ascontiguousarray(g("w_uq")[0], f),
        "w_ukv": np.ascontiguousarray(g("w_ukv")[0], f), "w_out": np.ascontiguousarray(g("w_out")[0], f),
        "peer_wq": np.ascontiguousarray(g("peer_wq")[0], f), "keysT": keysT,
        "peer_u": np.ascontiguousarray(g("peer_u")[0], f), "peer_v": np.ascontiguousarray(g("peer_v")[0], f),
        "pl_gate_w": np.ascontiguousarray(g("pl_gate_w")[0], f), "pl_proj": np.ascontiguousarray(g("pl_proj")[0], f),
        "cols": cols, "rows": rows, "cst": cst,
    }
    maps = []
    for b in range(8):
        m = dict(shared)
        m["x"] = np.ascontiguousarray(x[b])
        m["p"] = np.ascontiguousarray(p[b])
        m["pos"] = np.ascontiguousarray(np.broadcast_to(pos[b][None, :], (32, T))).astype(np.int32)
        maps.append(m)
    return maps


_NC_CACHE = {}


def kernel(**inputs):
    maps = make_in_maps(inputs)
    if "nc" not in _NC_CACHE:
        _NC_CACHE["nc"] = build_program()
    nc = _NC_CACHE["nc"]
    res = run_bass_kernel_spmd(nc, maps, core_ids=list(range(8)))
    out = np.stack([np.asarray(r["out"], np.float32) for r in res.results], axis=0)
    return out.reshape(8, T, D)
```

```python
import contextlib
import numpy as np
import concourse.bass as bass
import concourse.mybir as mybir
from concourse.bass_utils import run_bass_kernel_spmd

F32 = mybir.dt.float32
BF16 = mybir.dt.bfloat16
I32 = mybir.dt.int32
U32 = mybir.dt.uint32
AF = mybir.ActivationFunctionType
ALU = mybir.AluOpType
AX = mybir.AxisListType

T = 4096
D = 1024
NT = 32
NCH = 8
EPS = 1e-6
ATT_SCALE = 96 ** -0.5
ENGS = ["pe", "act", "dve", "pool", "sp"]
SAME_ENGINE_RAW = True
SAME_ENGINE_WAR = False
DOT_SPLIT = False

C_GA = 0
C_GQ = 8
C_GKV = 12
C_CB = 14
C_LNG = 18
C_LNB = 22
C_CG = 26
C_GAO = 30
C_GPL = 34
C_FREQ = 42
C_CW = 44
NCOL = C_CW + 4 * 31


class Sched:
    def __init__(self, nc):
        self.nc = nc
        self.ops = {e: [] for e in ENGS}
        self.res = {}
        self.seen = {e: {} for e in ENGS}
        self.chan_n = {}
        self.pending = {e: [] for e in ENGS}
        self.final_waits = []

    def _need(self, eng, prod, waits):
        if prod is None:
            return
        kind, key, n = prod
        if kind == "e" and key == eng:
            if key == "pe" or not SAME_ENGINE_RAW:
                return
        k = (kind, key)
        if self.seen[eng].get(k, -1) >= n:
            return
        self.seen[eng][k] = n
        waits.append(prod)

    def _deps(self, eng, reads, writes, waits):
        for r in reads:
            st = self.res.setdefault(r, {"w": None, "r": {}})
            self._need(eng, st["w"], waits)
        for w in writes:
            st = self.res.setdefault(w, {"w": None, "r": {}})
            p = st["w"]
            if p is not None and (SAME_ENGINE_WAR or not (p[0] == "e" and p[1] == eng)):
                self._need(eng, p, waits)
            for k, rp in st["r"].items():
                if rp[0] == "e" and rp[1] == eng and not SAME_ENGINE_WAR:
                    continue
                self._need(eng, rp, waits)

    def _commit(self, tok, reads, writes):
        for r in reads:
            self.res[r]["r"][(tok[0], tok[1])] = tok
        for w in writes:
            st = self.res[w]
            st["w"] = tok
            st["r"] = {}

    def _take_pending(self, eng):
        waits = list(self.pending[eng])
        self.pending[eng] = []
        for p in waits:
            k = (p[0], p[1])
            self.seen[eng][k] = max(self.seen[eng].get(k, -1), p[2])
        return waits

    def op(self, eng, fn, reads=(), writes=()):
        waits = self._take_pending(eng)
        self._deps(eng, reads, writes, waits)
        idx = len(self.ops[eng])
        self.ops[eng].append({"fn": fn, "waits": waits, "sig": False, "dma": None})
        self._commit(("e", eng, idx), reads, writes)

    def dma(self, eng, fn, ch, reads=(), writes=(), final=False):
        waits = self._take_pending(eng)
        n = self.chan_n.get(ch, 0)
        if n > 0:
            self._need(eng, ("d", ch, n), waits)
        self._deps(eng, reads, writes, waits)
        n += 1
        self.chan_n[ch] = n
        self.ops[eng].append({"fn": fn, "waits": waits, "sig": False, "dma": ch})
        self._commit(("d", ch, n), reads, writes)
        if final and ch not in self.final_waits:
            self.final_waits.append(ch)

    def barrier(self):
        prods = []
        for e in ENGS:
            for i in range(len(self.ops[e]) - 1, -1, -1):
                if self.ops[e][i]["dma"] is None:
                    prods.append(("e", e, i))
                    break
        for ch, n in self.chan_n.items():
            if not ch.startswith("tb"):
                prods.append(("d", ch, n))
        for e in ENGS:
            self.pending[e] = [p for p in prods if not (p[0] == "e" and p[1] == e)]
        self.res = {k: v for k, v in self.res.items() if k.startswith("tab")}

    def emit(self):
        nc = self.nc
        for e in ENGS:
            for o in self.ops[e]:
                for (kind, key, n) in o["waits"]:
                    if kind == "e":
                        self.ops[key][n]["sig"] = True
        cnt = {}
        for e in ENGS:
            c = 0
            for o in self.ops[e]:
                if o["sig"]:
                    c += 1
                o["cnt"] = c
            cnt[e] = c
        with contextlib.ExitStack() as st:
            esem = {e: st.enter_context(nc.semaphore("s_" + e)) for e in ENGS if cnt[e] > 0}
            csem = {ch: st.enter_context(nc.semaphore("c_%s" % (ch,))) for ch in self.chan_n}
            block = st.enter_context(nc.Block())
            handles = {"pe": block.tensor, "act": block.scalar, "dve": block.vector,
                       "pool": block.gpsimd, "sp": block.sync}

            def make(e):
                def body(eng):
                    for o in self.ops[e]:
                        for (kind, key, n) in o["waits"]:
                            if kind == "e":
                                eng.wait_ge(esem[key], self.ops[key][n]["cnt"])
                            else:
                                eng.wait_ge(csem[key], 16 * n)
                        ins = o["fn"](eng)
                        if o["dma"] is not None:
                            ins.then_inc(csem[o["dma"]], 16)
                        elif o["sig"]:
                            ins.then_inc(esem[e], 1)
                    if e == "sp":
                        for ch in self.final_waits:
                            eng.wait_ge(csem[ch], 16 * self.chan_n[ch])
                return body

            for e in ENGS:
                if self.ops[e] or e == "sp":
                    handles[e](make(e))
        return {e: len(self.ops[e]) for e in ENGS}, cnt


class Arena:
    def __init__(self, base_ap, nbytes):
        self.base = base_ap
        self.nbytes = nbytes
        self.allocs = []

    def alloc(self, shape, dt, phases):
        esz = {F32: 4, BF16: 2, I32: 4, U32: 4}[dt]
        n = 1
        for s in shape[1:]:
            n *= s
        size = (n * esz + 31) // 32 * 32
        phases = set(phases)
        cands = sorted({0} | {o + s for (o, s, p) in self.allocs})
        for off in cands:
            ok = off + size <= self.nbytes
            if ok:
                for (o, s, p) in self.allocs:
                    if p & phases and off < o + s and o < off + size:
                        ok = False
                        break
            if ok:
                self.allocs.append((off, size, phases))
                v = self.base[:, off // 4:(off + size) // 4]
                if dt != F32:
                    v = v.bitcast(dt)
                v = v[:, 0:n]
                if len(shape) == 3:
                    v = v.rearrange("p (a b) -> p a b", a=shape[1])
                elif len(shape) == 4:
                    v = v.rearrange("p (a b c) -> p a b c", a=shape[1], b=shape[2])
                return v
        raise RuntimeError("arena full: %s %s %s" % (shape, dt, phases))


def build_program(upto=4, debug=False):
    nc = bass.Bass("TRN2", target_bir_lowering=False)

    def din(name, shape, dt=F32):
        return nc.dram_tensor(name, shape, dt, kind="ExternalInput").ap()

    x_d = din("x", [T, D])
    p_d = din("p", [T, 256])
    pos_d = din("pos", [32, T], I32)
    w_in_d = din("w_in", [D, 1824])
    w_uq_d = din("w_uq", [512, 768])
    w_ukv_d = din("w_ukv", [256, 1024])
    w_out_d = din("w_out", [D, D])
    wq_d = din("peer_wq", [D, D])
    keysT_d = din("keysT", [128, 8, 128])
    u_d = din("peer_u", [16384, D])
    v_d = din("peer_v", [16384, D])
    gw_d = din("pl_gate_w", [D, D])
    pw_d = din("pl_proj", [256, D])
    cols_d = din("cols", [128, NCOL])
    rows_d = din("rows", [3, 128, D])
    cst_d = din("cst", [3, 128, 128])
    out_d = nc.dram_tensor("out", [T, D], F32, kind="ExternalOutput").ap()
    tab_d = nc.dram_tensor("peer_tab", [16384, 2 * D], BF16, kind="Internal").ap()
    dbg_d = None
    if debug:
        dbg_d = {
            "d_qnT": nc.dram_tensor("d_qnT", [128, 4 * T], BF16, kind="ExternalOutput").ap(),
            "d_kvnT": nc.dram_tensor("d_kvnT", [128, 2 * T], BF16, kind="ExternalOutput").ap(),
            "d_kpeT": nc.dram_tensor("d_kpeT", [128, T], BF16, kind="ExternalOutput").ap(),
            "d_V": nc.dram_tensor("d_V", [128, NT * 8 * 65], BF16, kind="ExternalOutput").ap(),
            "d_att": nc.dram_tensor("d_att", [128, NT * 512], BF16, kind="ExternalOutput").ap(),
            "d_cnT": nc.dram_tensor("d_cnT", [128, 4 * T], BF16, kind="ExternalOutput").ap(),
        }

    ARENA_BYTES = 212800
    with contextlib.ExitStack() as st:
        arena_t = st.enter_context(nc.sbuf_tensor("arena", [128, ARENA_BYTES // 4], F32))
        banks = [st.enter_context(nc.psum_tensor("bank%d" % i, [128, 512], F32)) for i in range(8)]
        A = Arena(arena_t[:, :], ARENA_BYTES)
        S = Sched(nc)
        ALLP = {1, 2, 3, 4}

        def bk(i):
            return banks[i][:, :]

        def bkb(i):
            return banks[i][:, :].bitcast(BF16)

        cols = A.alloc([128, NCOL], F32, ALLP)
        ident = A.alloc([128, 128], BF16, ALLP)
        tri = A.alloc([128, 128], BF16, {1, 2})
        ones = A.alloc([128, 128], BF16, {1, 2, 3})
        iota16 = A.alloc([128, 16], F32, ALLP)
        scr = A.alloc([128, 64], F32, ALLP)

        S.dma("sp", lambda e: e.dma_start(out=cols, in_=cols_d), "c0", writes=["cols"])
        S.dma("pool", lambda e: e.dma_start(out=ident, in_=cst_d[0]), "c1", writes=["ident"])
        S.dma("pool", lambda e: e.dma_start(out=tri, in_=cst_d[1]), "c2", writes=["tri"])
        S.dma("sp", lambda e: e.dma_start(out=iota16, in_=cst_d[2][:, 0:16]), "c3", writes=["iota16"])
        S.op("dve", lambda e: e.memset(ones, 1.0), writes=["ones"])

        def col(c0, n=1):
            return cols[:, c0:c0 + n]

        att = A.alloc([128, NT, 512], BF16, {2, 3, 4})
        cnT = A.alloc([128, 4, T], BF16, {3, 4})
        qnT = A.alloc([128, 4, T], BF16, {1, 2})
        kvnT = A.alloc([128, 2, T], BF16, {1, 2})
        kpeT = A.alloc([128, T], BF16, {1, 2})
        Vt = A.alloc([128, NT, 8, 65], BF16, {1, 2})
        cosT = A.alloc([128, T], BF16, {1, 2})
        sinT = A.alloc([128, T], BF16, {1, 2})
        w_uq = A.alloc([128, 4, 768], BF16, {1, 2})
        w_uqr = A.alloc([128, 4, 8, 96], BF16, {1, 2})
        w_ukv = A.alloc([128, 2, 1024], BF16, {1, 2})

        def rstd_from_ss(ss_ap, n_feat, out_ap, tag):
            S.op("act", lambda e: e.activation(out=out_ap, in_=ss_ap, func=AF.Sqrt, scale=1.0 / n_feat, bias=EPS),
                 reads=[tag + "_ss"], writes=[tag + "_r"])
            S.op("dve", lambda e: e.reciprocal(out=out_ap, in_=out_ap), reads=[tag + "_r"], writes=[tag + "_r"])

        def x_to_hnT(c, xt, xsb, junkb, hnT, ssx, hname="hnT"):
            for t4 in range(4):
                g = 4 * c + t4
                par = g % 2
                S.dma("sp", lambda e, g=g, par=par: e.dma_start(out=xt[par], in_=x_d[g * 128:(g + 1) * 128, :]),
                      "x%d" % par, writes=["xt%d" % par])
                S.op("act", lambda e, par=par: e.activation(out=junkb, in_=xt[par], func=AF.Square,
                                                            accum_out=ssx[:, par:par + 1]),
                     reads=["xt%d" % par], writes=["junkb", "x%d_ss" % par])
                rstd_from_ss(ssx[:, par:par + 1], D, ssx[:, 2 + par:3 + par], "x%d" % par)
                S.op("dve", lambda e, par=par: e.tensor_scalar(out=xsb, in0=xt[par], scalar1=ssx[:, 2 + par:3 + par],
                                                               scalar2=None, op0=ALU.mult),
                     reads=["xt%d" % par, "x%d_r" % par], writes=["xsb"])
                Tb = bkb(0).rearrange("p (a b) -> p a b", a=8)
                for kc in range(8):
                    S.op("pe", lambda e, kc=kc: e.transpose(out=Tb[:, kc, :], in_=xsb[:, kc * 128:(kc + 1) * 128],
                                                            identity=ident),
                         reads=["xsb", "ident"], writes=["b0"])
                S.op("dve", lambda e, t4=t4: e.tensor_tensor(
                    out=hnT[:, :, t4 * 128:(t4 + 1) * 128], in0=Tb,
                    in1=col(C_GA, 8).unsqueeze(2).to_broadcast([128, 8, 128]), op=ALU.mult),
                    reads=["b0", "cols"], writes=[hname])

        def norm_fm(zbanks, n_oc, n_feat, gcol, dst, c, sq, sd, tag):
            for oc in range(n_oc):
                S.op("act", lambda e, oc=oc: e.activation(out=sq[:, oc, :], in_=bk(zbanks[oc]), func=AF.Square),
                     reads=["b%d" % zbanks[oc]], writes=["sq"])
            for oc in range(n_oc):
                S.op("pe", lambda e, oc=oc: e.matmul(bk(5), lhsT=ones, rhs=sq[:, oc, :], start=(oc == 0),
                                                     stop=(oc == n_oc - 1)),
                     reads=["sq", "ones"], writes=["b5"])
            S.op("act", lambda e: e.activation(out=sd, in_=bk(5), func=AF.Sqrt, scale=1.0 / n_feat, bias=EPS),
                 reads=["b5"], writes=["sd"])
            S.op("dve", lambda e: e.reciprocal(out=sd, in_=sd), reads=["sd"], writes=["sd"])
            for oc in range(n_oc):
                S.op("dve", lambda e, oc=oc: e.scalar_tensor_tensor(
                    out=dst[:, oc, c * 512:(c + 1) * 512], in0=bk(zbanks[oc]), scalar=col(gcol + oc), in1=sd,
                    op0=ALU.mult, op1=ALU.mult),
                    reads=["b%d" % zbanks[oc], "sd", "cols"], writes=[tag])

        w_in1 = A.alloc([128, 8, 800], BF16, {1})
        wkpe = A.alloc([128, 8, 96], BF16, {1})
        wkper = A.alloc([128, 8, 96], BF16, {1})
        xt = [A.alloc([128, D], F32, {1}), A.alloc([128, D], F32, {1})]
        xsb = A.alloc([128, D], BF16, {1})
        junkb = A.alloc([128, D], BF16, {1})
        hnT2 = [A.alloc([128, 8, 512], BF16, {1}) for _ in range(2)]
        sq = A.alloc([128, 4, 512], BF16, {1})
        sd = A.alloc([128, 512], F32, {1})
        tmpa = A.alloc([128, 512], F32, {1})
        tmpb = A.alloc([128, 512], F32, {1})
        ssx = scr[:, 0:4]

        w_in_v = w_in_d.rearrange("(k p) c -> p k c", p=128)
        S.dma("pool", lambda e: e.dma_start(out=w_in1, in_=w_in_v[:, :, 0:800]), "w0", writes=["w_in1"])
        S.op("dve", lambda e: e.memset(wkpe, 0.0), writes=["wkpe"])
        S.op("dve", lambda e: e.memset(wkper, 0.0), writes=["wkper"])
        S.dma("pool", lambda e: e.dma_start(out=wkpe[:, :, 64:96], in_=w_in_v[:, :, 768:800]), "w1", writes=["wkpe"])
        S.dma("pool", lambda e: e.dma_start(out=wkper[:, :, 64:80], in_=w_in_v[:, :, 784:800]), "w2", writes=["wkper"])
        S.dma("pool", lambda e: e.dma_start(out=wkper[:, :, 80:96], in_=w_in_v[:, :, 768:784]), "w3", writes=["wkper"])
        S.op("dve", lambda e: e.tensor_scalar(out=wkper[:, :, 64:80], in0=wkper[:, :, 64:80], scalar1=-1.0,
                                              scalar2=None, op0=ALU.mult), reads=["wkper"], writes=["wkper"])
        S.dma("pool", lambda e: e.dma_start(out=w_uq, in_=w_uq_d.rearrange("(k p) c -> p k c", p=128)), "w4",
              writes=["w_uq"])
        S.dma("pool", lambda e: e.dma_start(out=w_ukv, in_=w_ukv_d.rearrange("(k p) c -> p k c", p=128)), "w5",
              writes=["w_ukv"])
        S.op("dve", lambda e: e.memset(w_uqr, 0.0), writes=["w_uqr"])
        w_uq_h = w_uq.rearrange("p k (h c) -> p k h c", h=8)
        S.op("dve", lambda e: e.tensor_scalar(out=w_uqr[:, :, :, 64:80], in0=w_uq_h[:, :, :, 80:96], scalar1=-1.0,
                                              scalar2=None, op0=ALU.mult), reads=["w_uq", "w_uqr"], writes=["w_uqr"])
        S.op("dve", lambda e: e.tensor_copy(out=w_uqr[:, :, :, 80:96], in_=w_uq_h[:, :, :, 64:80]),
             reads=["w_uq", "w_uqr"], writes=["w_uqr"])
        for k in range(8):
            rs = slice(k * 2048, (k + 1) * 2048)
            S.dma("pool", lambda e, rs=rs: e.dma_start(out=tab_d[rs, 0:D], in_=u_d[rs, :]), "tbu%d" % k, writes=["tabu%d" % k])
            S.dma("pool", lambda e, rs=rs: e.dma_start(out=tab_d[rs, D:2 * D], in_=v_d[rs, :]), "tbv%d" % k, writes=["tabv%d" % k])
        S.op("dve", lambda e: e.memset(Vt[:, :, :, 64:65], 1.0), writes=["Vones"])

        posi = xt[0].bitcast(I32)
        ya = xt[1]
        ki = tmpa.bitcast(I32)
        R = slice(64, 96)
        for blk in range(8):
            cs = slice(blk * 512, (blk + 1) * 512)
            S.dma("sp", lambda e, cs=cs: e.dma_start(out=posi[R, 0:512], in_=pos_d[:, cs]), "x0", writes=["posi"])
            S.op("dve", lambda e: e.tensor_copy(out=ya[R, 0:512], in_=posi[R, 0:512]), reads=["posi"], writes=["ya"])
            S.op("dve", lambda e: e.tensor_scalar(out=ya[R, 0:512], in0=ya[R, 0:512], scalar1=cols[R, C_FREQ:C_FREQ + 1],
                                                  scalar2=1.0 / (2 * np.pi), op0=ALU.mult, op1=ALU.mult),
                 reads=["ya", "cols"], writes=["ya"])
            for which, tab, shift in (("s", sinT, 0.0), ("c", cosT, 0.25)):
                if shift:
                    S.op("dve", lambda e: e.tensor_scalar(out=ya[R, 512:1024], in0=ya[R, 0:512], scalar1=0.25,
                                                          scalar2=None, op0=ALU.add), reads=["ya"], writes=["yb"])
                    src = ya[R, 512:1024]
                    rname = "yb"
                else:
                    src = ya[R, 0:512]
                    rname = "ya"
                S.op("dve", lambda e, src=src: e.tensor_copy(out=ki[R, :], in_=src), reads=[rname], writes=["ki"])
                S.op("dve", lambda e: e.tensor_copy(out=tmpb[R, :], in_=ki[R, :]), reads=["ki"], writes=["tmpb"])
                S.op("dve", lambda e, src=src: e.tensor_tensor(out=tmpb[R, :], in0=src, in1=tmpb[R, :], op=ALU.subtract),
                     reads=[rname, "tmpb"], writes=["tmpb"])
                S.op("act", lambda e, tab=tab, cs=cs: e.activation(out=tab[R, cs], in_=tmpb[R, :], func=AF.Sin,
                                                                   scale=6.28318),
                     reads=["tmpb"], writes=["rope" + which])

        S.barrier()
        if upto >= 1:
            for c in range(NCH):
                hnT = hnT2[c % 2]
                HN = "hnT%d" % (c % 2)
                x_to_hnT(c, xt, xsb, junkb, hnT, ssx, HN)
                for oc in range(4):
                    for kc in range(8):
                        S.op("pe", lambda e, oc=oc, kc=kc, hnT=hnT: e.matmul(bk(1 + oc), lhsT=w_in1[:, kc, oc * 128:(oc + 1) * 128],
                                                                    rhs=hnT[:, kc, :], start=(kc == 0), stop=(kc == 7)),
                             reads=[HN, "w_in1"], writes=["b%d" % (1 + oc)])
                norm_fm([1, 2, 3, 4], 4, 512, C_GQ, qnT, c, sq, sd, "qnT")
                for oc in range(2):
                    for kc in range(8):
                        S.op("pe", lambda e, oc=oc, kc=kc, hnT=hnT: e.matmul(bk(1 + oc), lhsT=w_in1[:, kc, 512 + oc * 128:512 + (oc + 1) * 128],
                                                                    rhs=hnT[:, kc, :], start=(kc == 0), stop=(kc == 7)),
                             reads=[HN, "w_in1"], writes=["b%d" % (1 + oc)])
                norm_fm([1, 2], 2, 256, C_GKV, kvnT, c, sq, sd, "kvnT")
                for kc in range(8):
                    S.op("pe", lambda e, kc=kc, hnT=hnT: e.matmul(bk(6)[0:96, :], lhsT=wkpe[:, kc, :], rhs=hnT[:, kc, :],
                                                         start=(kc == 0), stop=(kc == 7)),
                         reads=[HN, "wkpe"], writes=["b6"])
                for kc in range(8):
                    S.op("pe", lambda e, kc=kc, hnT=hnT: e.matmul(bk(7)[0:96, :], lhsT=wkper[:, kc, :], rhs=hnT[:, kc, :],
                                                         start=(kc == 0), stop=(kc == 7)),
                         reads=[HN, "wkper"], writes=["b7"])
                cs = slice(c * 512, (c + 1) * 512)
                S.op("dve", lambda e, cs=cs: e.tensor_tensor(out=tmpa[R, :], in0=bk(6)[R, :], in1=cosT[R, cs], op=ALU.mult),
                     reads=["b6", "ropec"], writes=["tmpa"])
                S.op("dve", lambda e, cs=cs: e.tensor_tensor(out=tmpb[R, :], in0=bk(7)[R, :], in1=sinT[R, cs], op=ALU.mult),
                     reads=["b7", "ropes"], writes=["tmpb"])
                S.op("dve", lambda e, cs=cs: e.tensor_tensor(out=kpeT[R, cs], in0=tmpa[R, :], in1=tmpb[R, :], op=ALU.add),
                     reads=["tmpa", "tmpb"], writes=["kpeT"])
                w_ukv_h = w_ukv.rearrange("p k (h c) -> p k h c", h=8)
                for t4 in range(4):
                    g = 4 * c + t4
                    vb = 6 + (t4 % 2)
                    for kc in range(2):
                        S.op("pe", lambda e, kc=kc, g=g, vb=vb: e.matmul(
                            bk(vb).rearrange("p (h c) -> p h c", h=8), lhsT=kvnT[:, kc, g * 128:(g + 1) * 128],
                            rhs=w_ukv_h[:, kc, :, 64:128], start=(kc == 0), stop=(kc == 1)),
                            reads=["kvnT", "w_ukv"], writes=["b%d" % vb])
                    S.op("act", lambda e, g=g, vb=vb: e.copy(out=Vt[:, g, :, 0:64],
                                                            in_=bk(vb).rearrange("p (h c) -> p h c", h=8)),
                         reads=["b%d" % vb], writes=["V"])
        S.barrier()

        QT = [A.alloc([128, T], BF16, {2}), A.alloc([128, T], BF16, {2})]
        KT = [A.alloc([128, T], BF16, {2}), A.alloc([128, T], BF16, {2})]
        NPT = 5
        PT = [A.alloc([128, 512], BF16, {2}) for _ in range(NPT)]
        sqq = A.alloc([128, 512], BF16, {2})
        t2a = A.alloc([128, 512], F32, {2})
        t2b = A.alloc([128, 512], F32, {2})
        mxq = A.alloc([128, 16], F32, {2})
        negm = A.alloc([128, 8], F32, {2})
        rec = A.alloc([128, 8], F32, {2})
        w_ukv_h = w_ukv.rearrange("p k (h c) -> p k h c", h=8)

        def build_qk(h):
            hp = h % 2
            qt, kt = QT[hp], KT[hp]
            S.op("pool", lambda e: e.tensor_copy(out=kt[R, :], in_=kpeT[R, :]), reads=["kpeT"], writes=["KT%d" % hp])
            for c in range(NCH):
                cs = slice(c * 512, (c + 1) * 512)
                for kc in range(4):
                    S.op("pe", lambda e, kc=kc, cs=cs: e.matmul(bk(0)[0:96, :], lhsT=w_uq[:, kc, h * 96:(h + 1) * 96],
                                                                rhs=qnT[:, kc, cs], start=(kc == 0), stop=(kc == 3)),
                         reads=["qnT", "w_uq"], writes=["b0"])
                for kc in range(4):
                    S.op("pe", lambda e, kc=kc, cs=cs: e.matmul(bk(1)[0:96, :], lhsT=w_uqr[:, kc, h, :],
                                                                rhs=qnT[:, kc, cs], start=(kc == 0), stop=(kc == 3)),
                         reads=["qnT", "w_uqr"], writes=["b1"])
                for kc in range(2):
                    S.op("pe", lambda e, kc=kc, cs=cs: e.matmul(bk(2)[0:64, :], lhsT=w_ukv_h[:, kc, h, 0:64],
                                                                rhs=kvnT[:, kc, cs], start=(kc == 0), stop=(kc == 1)),
                         reads=["kvnT", "w_ukv"], writes=["b2"])
                S.op("act", lambda e, cs=cs: e.copy(out=qt[0:64, cs], in_=bk(0)[0:64, :]), reads=["b0"],
                     writes=["QT%d" % hp])
                S.op("dve", lambda e, cs=cs: e.tensor_tensor(out=t2a[R, :], in0=bk(0)[R, :], in1=cosT[R, cs], op=ALU.mult),
                     reads=["b0", "ropec"], writes=["t2a"])
                S.op("dve", lambda e, cs=cs: e.tensor_tensor(out=t2b[R, :], in0=bk(1)[R, :], in1=sinT[R, cs], op=ALU.mult),
                     reads=["b1", "ropes"], writes=["t2b"])
                S.op("dve", lambda e, cs=cs: e.tensor_tensor(out=qt[R, cs], in0=t2a[R, :], in1=t2b[R, :], op=ALU.add),
                     reads=["t2a", "t2b"], writes=["QT%d" % hp])
                S.op("act", lambda e, cs=cs: e.copy(out=kt[0:64, cs], in_=bk(2)[0:64, :]), reads=["b2"],
                     writes=["KT%d" % hp])
                for which, src, col0 in (("q", qt, 0), ("k", kt, 8)):
                    S.op("act", lambda e, src=src, cs=cs: e.activation(out=sqq[0:96, :], in_=src[0:96, cs], func=AF.Square),
                         reads=[("QT%d" if which == "q" else "KT%d") % hp], writes=["sqq"])
                    S.op("pe", lambda e: e.matmul(bk(2), lhsT=ones[0:96, :], rhs=sqq[0:96, :], start=True, stop=True),
                         reads=["sqq", "ones"], writes=["b2"])
                    S.op("dve", lambda e, col0=col0, c=c: e.reduce_max(out=mxq[:, col0 + c:col0 + c + 1], in_=bk(2), axis=AX.X),
                         reads=["b2"], writes=["mxq"])
            S.op("dve", lambda e: e.reduce_max(out=scr[:, 8:9], in_=mxq[:, 0:8], axis=AX.X), reads=["mxq"], writes=["scr8"])
            S.op("dve", lambda e: e.reduce_max(out=scr[:, 9:10], in_=mxq[:, 8:16], axis=AX.X), reads=["mxq"], writes=["scr9"])
            S.op("dve", lambda e: e.tensor_tensor(out=scr[:, 10:11], in0=scr[:, 8:9], in1=scr[:, 9:10], op=ALU.mult),
                 reads=["scr8", "scr9"], writes=["scr10"])
            S.op("act", lambda e: e.activation(out=scr[:, 11:12], in_=scr[:, 10:11], func=AF.Sqrt), reads=["scr10"],
                 writes=["scr11"])
            S.op("dve", lambda e: e.tensor_scalar(out=negm[:, h:h + 1], in0=scr[:, 11:12], scalar1=-ATT_SCALE, scalar2=None,
                                                  op0=ALU.mult), reads=["scr11"], writes=["negm%d" % h])

        def attn(h):
            hp = h % 2
            qt, kt = QT[hp], KT[hp]
            steps = []
            for j in range(NCH):
                for i in range(4 * j + 4):
                    steps.append((j, i))

            def emit_S(n):
                j, i = steps[n]
                q0 = max(512 * j, 128 * i)
                w = 512 * j + 512 - q0
                sb_ = 3 + (n % 3)
                pt = PT[n % NPT]
                ptn = "PT%d" % (n % NPT)
                S.op("pe", lambda e: e.matmul(bk(sb_)[:, 0:w], lhsT=kt[0:96, i * 128:(i + 1) * 128], rhs=qt[0:96, q0:q0 + w],
                                              start=True, stop=True),
                     reads=["QT%d" % hp, "KT%d" % hp], writes=["b%d" % sb_])
                S.op("act", lambda e: e.activation(out=pt[:, 0:w], in_=bk(sb_)[:, 0:w], func=AF.Exp, scale=ATT_SCALE,
                                                   bias=negm[:, h:h + 1]),
                     reads=["b%d" % sb_, "negm%d" % h], writes=[ptn])
                if 128 * i >= 512 * j:
                    S.op("pool", lambda e: e.tensor_tensor(out=pt[:, 0:128], in0=pt[:, 0:128], in1=tri, op=ALU.mult),
                         reads=[ptn, "tri"], writes=[ptn])

            def emit_PV(n):
                j, i = steps[n]
                q0 = max(512 * j, 128 * i)
                r0 = (q0 - 512 * j) // 128
                pt = PT[n % NPT]
                ptn = "PT%d" % (n % NPT)
                ob = 6 + (j % 2)
                O = bk(ob)[:, 0:260].rearrange("p (r c) -> p r c", r=4)
                for rr in range(r0, 4):
                    S.op("pe", lambda e, rr=rr: e.matmul(O[:, rr, :], lhsT=pt[:, (rr - r0) * 128:(rr - r0 + 1) * 128], rhs=Vt[:, i, h, :],
                                                         start=(i == 0 and rr == 0), stop=(i == 4 * j + 3 and rr == 3)),
                         reads=[ptn, "V", "Vones"], writes=["b%d" % ob])
                if i == 4 * j + 3:
                    S.op("dve", lambda e: e.reciprocal(out=rec[:, 0:4], in_=O[:, :, 64]), reads=["b%d" % ob], writes=["rec"])
                    S.op("dve", lambda e: e.tensor_tensor(out=att[:, 4 * j:4 * j + 4, h * 64:(h + 1) * 64], in0=O[:, :, 0:64],
                                                          in1=rec[:, 0:4].unsqueeze(2).to_broadcast([128, 4, 64]), op=ALU.mult),
                         reads=["b%d" % ob, "rec"], writes=["att"])

            emit_S(0)
            emit_S(1)
            for n in range(len(steps)):
                if n + 2 < len(steps):
                    emit_S(n + 2)
                emit_PV(n)

        if upto >= 2:
            build_qk(0)
            for h in range(8):
                if h + 1 < 8:
                    build_qk(h + 1)
                attn(h)
        if debug and upto <= 2:
            for nm, src in (("d_qnT", qnT), ("d_kvnT", kvnT), ("d_kpeT", kpeT), ("d_V", Vt), ("d_att", att)):
                flat = src
                if len(src.shape) == 3:
                    flat = src.rearrange("p a b -> p (a b)")
                elif len(src.shape) == 4:
                    flat = src.rearrange("p a b c -> p (a b c)")
                S.barrier()
                S.dma("sp", lambda e, nm=nm, flat=flat: e.dma_start(out=dbg_d[nm], in_=flat), "dbg", final=True)
        S.barrier()

        w_in3 = A.alloc([128, 8, 1024], BF16, {3})
        dg = A.alloc([128, 4, 31, 128], BF16, {3})
        xt3 = [A.alloc([128, D], F32, {3}), A.alloc([128, D], F32, {3})]
        xsb3 = A.alloc([128, D], BF16, {3})
        junkb3 = A.alloc([128, D], BF16, {3})
        hnT32 = [A.alloc([128, 8, 512], BF16, {3}) for _ in range(2)]
        glu = [A.alloc([128, 4, 544], BF16, {3}), A.alloc([128, 4, 544], BF16, {3})]
        sig = A.alloc([128, 512], F32, {3})
        y = A.alloc([128, 4, 512], F32, {3})
        ybf = A.alloc([128, 4, 512], BF16, {3})
        mean = A.alloc([128, 512], F32, {3})
        sd3 = A.alloc([128, 512], F32, {3})
        ssx3 = scr[:, 16:20]
        if upto >= 3:
            S.dma("pool", lambda e: e.dma_start(out=w_in3, in_=w_in_v[:, :, 800:1824]), "w0", writes=["w_in3"])
            cw = cols[:, C_CW:C_CW + 124].rearrange("p (j k) -> p j k", j=4)
            for j in range(4):
                for k in range(31):
                    eng = "dve" if (k % 2 == 0) else "pool"
                    S.op(eng, lambda e, j=j, k=k: e.tensor_scalar(out=dg[:, j, k, :], in0=ident, scalar1=cw[:, j, k:k + 1],
                                                                  scalar2=None, op0=ALU.mult),
                         reads=["ident", "cols"], writes=["dg"])
            S.op("pool", lambda e: e.memset(glu[1][:, :, 0:32], 0.0), writes=["glu1"])
            sig2 = [sig, A.alloc([128, 512], F32, {3})]

            def conv_AG(c, j):
                hn = hnT32[c % 2]
                HN3 = "hnT%d" % (c % 2)
                ba, bg = (1, 2) if j % 2 == 0 else (6, 7)
                for kc in range(8):
                    S.op("pe", lambda e, kc=kc: e.matmul(bk(ba), lhsT=w_in3[:, kc, j * 128:(j + 1) * 128], rhs=hn[:, kc, :],
                                                         start=(kc == 0), stop=(kc == 7)),
                         reads=[HN3, "w_in3"], writes=["b%d" % ba])
                for kc in range(8):
                    S.op("pe", lambda e, kc=kc: e.matmul(bk(bg), lhsT=w_in3[:, kc, 512 + j * 128:512 + (j + 1) * 128], rhs=hn[:, kc, :],
                                                         start=(kc == 0), stop=(kc == 7)),
                         reads=[HN3, "w_in3"], writes=["b%d" % bg])

            def conv_pre(c):
                gp = c % 2
                x_to_hnT(c, xt3, xsb3, junkb3, hnT32[c % 2], ssx3, "hnT%d" % (c % 2))
                if c > 0:
                    S.op("pool", lambda e: e.tensor_copy(out=glu[gp][:, :, 0:32], in_=glu[1 - gp][:, :, 512:544]),
                         reads=["glu%d" % (1 - gp)], writes=["glu%d" % gp])
                else:
                    S.op("pool", lambda e: e.memset(glu[0][:, :, 0:32], 0.0), writes=["glu0"])
                conv_AG(c, 0)

            def conv_body(c):
                gp = c % 2
                for j in range(4):
                    ba, bg = (1, 2) if j % 2 == 0 else (6, 7)
                    sg = sig2[j % 2]
                    sgn = "sig%d" % (j % 2)
                    S.op("act", lambda e, bg=bg, sg=sg: e.activation(out=sg, in_=bk(bg), func=AF.Sigmoid), reads=["b%d" % bg], writes=[sgn])
                    S.op("dve", lambda e, j=j, ba=ba, sg=sg: e.tensor_tensor(out=glu[gp][:, j, 32:544], in0=bk(ba), in1=sg, op=ALU.mult),
                         reads=["b%d" % ba, sgn], writes=["glu%d" % gp])
                    if j + 1 < 4:
                        conv_AG(c, j + 1)
                    yb = 3 + (j % 2)
                    for k in range(31):
                        S.op("pe", lambda e, j=j, k=k, yb=yb: e.matmul(bk(yb), lhsT=dg[:, j, k, :], rhs=glu[gp][:, j, 2 + k:2 + k + 512],
                                                                       start=(k == 0), stop=(k == 30)),
                             reads=["glu%d" % gp, "dg"], writes=["b%d" % yb])
                    S.op("act", lambda e, j=j, yb=yb: e.activation(out=y[:, j, :], in_=bk(yb), func=AF.Identity, bias=col(C_CB + j)),
                         reads=["b%d" % yb, "cols"], writes=["y%d" % j])
                    S.op("act", lambda e, j=j: e.copy(out=ybf[:, j, :], in_=y[:, j, :]), reads=["y%d" % j], writes=["ybf"])

            def conv_tail(c):
                for j in range(4):
                    S.op("pe", lambda e, j=j: e.matmul(bk(5), lhsT=ones, rhs=ybf[:, j, :], start=(j == 0), stop=(j == 3)),
                         reads=["ybf", "ones"], writes=["b5"])
                S.op("act", lambda e: e.activation(out=mean, in_=bk(5), func=AF.Copy, scale=1.0 / 512), reads=["b5"],
                     writes=["mean"])
                for j in range(4):
                    S.op("dve", lambda e, j=j: e.tensor_tensor(out=y[:, j, :], in0=y[:, j, :], in1=mean, op=ALU.subtract),
                         reads=["y%d" % j, "mean"], writes=["y%d" % j])
                    S.op("act", lambda e, j=j: e.activation(out=ybf[:, j, :], in_=y[:, j, :], func=AF.Square),
                         reads=["y%d" % j], writes=["ybf"])
                for j in range(4):
                    S.op("pe", lambda e, j=j: e.matmul(bk(5), lhsT=ones, rhs=ybf[:, j, :], start=(j == 0), stop=(j == 3)),
                         reads=["ybf", "ones"], writes=["b5"])
                S.op("act", lambda e: e.activation(out=sd3, in_=bk(5), func=AF.Sqrt, scale=1.0 / 512, bias=EPS),
                     reads=["b5"], writes=["sd3"])
                S.op("dve", lambda e: e.reciprocal(out=sd3, in_=sd3), reads=["sd3"], writes=["sd3"])
                for j in range(4):
                    S.op("dve", lambda e, j=j: e.tensor_tensor(out=y[:, j, :], in0=y[:, j, :], in1=sd3, op=ALU.mult),
                         reads=["y%d" % j, "sd3"], writes=["y%d" % j])
                    S.op("act", lambda e, j=j: e.activation(out=y[:, j, :], in_=y[:, j, :], func=AF.Silu,
                                                            scale=col(C_LNG + j), bias=col(C_LNB + j)),
                         reads=["y%d" % j, "cols"], writes=["y%d" % j])
                    S.op("act", lambda e, j=j: e.activation(out=ybf[:, j, :], in_=y[:, j, :], func=AF.Square),
                         reads=["y%d" % j], writes=["ybf"])
                for j in range(4):
                    S.op("pe", lambda e, j=j: e.matmul(bk(5), lhsT=ones, rhs=ybf[:, j, :], start=(j == 0), stop=(j == 3)),
                         reads=["ybf", "ones"], writes=["b5"])
                S.op("act", lambda e: e.activation(out=sd3, in_=bk(5), func=AF.Sqrt, scale=1.0 / 512, bias=EPS),
                     reads=["b5"], writes=["sd3"])
                S.op("dve", lambda e: e.reciprocal(out=sd3, in_=sd3), reads=["sd3"], writes=["sd3"])
                for j in range(4):
                    S.op("dve", lambda e, j=j, c=c: e.scalar_tensor_tensor(
                        out=cnT[:, j, c * 512:(c + 1) * 512], in0=y[:, j, :], scalar=col(C_CG + j), in1=sd3,
                        op0=ALU.mult, op1=ALU.mult), reads=["y%d" % j, "sd3", "cols"], writes=["cnT"])
            conv_pre(0)
            for c in range(NCH):
                conv_body(c)
                if c + 1 < NCH:
                    conv_pre(c + 1)
                conv_tail(c)
        if debug and upto == 3:
            S.barrier()
            S.dma("sp", lambda e: e.dma_start(out=dbg_d["d_cnT"], in_=cnT.rearrange("p a b -> p (a b)")), "dbg", final=True)
            S.dma("sp", lambda e: e.dma_start(out=dbg_d["d_att"], in_=att.rearrange("p a b -> p (a b)")), "dbg", final=True)
        S.barrier()

        if upto >= 4:
            w_out = A.alloc([128, 8, D], BF16, {4})
            wq = A.alloc([128, 8, D], BF16, {4})
            kbd = A.alloc([128, 8, 256], BF16, {4})
            gw = A.alloc([128, 8, D], BF16, {4})
            pw = A.alloc([128, 2, D], BF16, {4})
            g_ffn = A.alloc([128, D], BF16, {4})
            g_b = A.alloc([128, D], F32, {4})
            g_fin = A.alloc([128, D], F32, {4})
            NB, GRP = 9, 2
            gball = A.alloc([128, NB, 2 * D], BF16, {4})
            gb = [gball[:, b, :] for b in range(NB)]
            dgs = [A.alloc([128, 128], BF16, {4}) for _ in range(4)]
            gl = A.alloc([128, 128], F32, {4})
            h1s = [A.alloc([128, D], F32, {4}) for _ in range(2)]
            xnbs = [A.alloc([128, D], BF16, {4}) for _ in range(2)]
            tmp = A.alloc([128, D], F32, {4})
            bfa = A.alloc([128, D], BF16, {4})
            xT = A.alloc([128, 8, 128], BF16, {4})
            mp = A.alloc([128, 256], F32, {4})
            mixT = mp.bitcast(BF16).rearrange("p (a b) -> p a b", a=4)
            tk = A.alloc([128, 2048], F32, {4})
            sc = tk.rearrange("p (a b) -> p a b", a=16)
            cand = tk.rearrange("p (h c) -> p h c", h=8)
            oh = tk.rearrange("p (h a b) -> p h a b", h=8, a=16)
            wk = A.alloc([128, 256], F32, {4})
            m16 = A.alloc([128, 16, 16], F32, {4})
            ix16 = A.alloc([128, 16, 16], U32, {4})
            ixf = ix16.bitcast(F32)
            best = A.alloc([128, 8, 16], F32, {4})
            posu = A.alloc([128, 8, 16], U32, {4})
            posf = A.alloc([128, 8, 16], F32, {4})
            ki4 = posu.bitcast(I32)
            k1f = A.alloc([128, 8, 16], F32, {4})
            k2f = A.alloc([128, 8, 16], F32, {4})
            e1 = A.alloc([128, 8, 16], F32, {4})
            e2 = posf
            eidxs = [A.alloc([128, 128], I32, {4}) for _ in range(2)]
            gate16s = [A.alloc([128, 8, 16], F32, {4}) for _ in range(2)]
            actv = A.alloc([128, 128], F32, {4})
            coef = A.alloc([128, 128], F32, {4})
            pt_ = mp
            pbf = A.alloc([128, 256], BF16, {4})
            pT = A.alloc([128, 2, 128], BF16, {4})
            gsum = A.alloc([128, 8], F32, {4})
            ss4 = scr[:, 24:40]

            def wload(dst, src, ch, name):
                S.dma("pool", lambda e: e.dma_start(out=dst, in_=src.rearrange("(k p) c -> p k c", p=128)), ch, writes=[name])
            wload(w_out, w_out_d, "w0", "w_out")
            wload(wq, wq_d, "w1", "wq")
            wload(gw, gw_d, "w2", "gw")
            wload(pw, pw_d, "w3", "pw")
            S.op("dve", lambda e: e.memset(kbd, 0.0), writes=["kbd"])
            S.dma("pool", lambda e: e.dma_start(out=kbd[0:64, :, 0:128], in_=keysT_d[0:64]), "w4", writes=["kbd"])
            S.dma("pool", lambda e: e.dma_start(out=kbd[64:128, :, 128:256], in_=keysT_d[64:128]), "w5", writes=["kbd"])
            S.dma("pool", lambda e: e.dma_start(out=g_ffn, in_=rows_d[0]), "c0", writes=["g_ffn"])
            S.dma("sp", lambda e: e.dma_start(out=g_b, in_=rows_d[1]), "c1", writes=["g_b"])
            S.dma("sp", lambda e: e.dma_start(out=g_fin, in_=rows_d[2]), "c2", writes=["g_fin"])

            class Rec:
                def __init__(self):
                    self.q = []
                def op(self, *a, **k):
                    self.q.append(lambda: S.op(*a, **k))
                def dma(self, *a, **k):
                    self.q.append(lambda: S.dma(*a, **k))
                def brk(self):
                    self.q.append(None)

            def rms_tile(X, src, srcname, k, n_feat, junk, junkname):
                X.op("act", lambda e: e.activation(out=junk, in_=src, func=AF.Square, accum_out=ss4[:, k:k + 1]),
                     reads=[srcname], writes=[junkname, "r%d_ss" % k])
                X.op("act", lambda e: e.activation(out=ss4[:, k + 8:k + 9], in_=ss4[:, k:k + 1], func=AF.Sqrt, scale=1.0 / n_feat, bias=EPS),
                     reads=["r%d_ss" % k], writes=["r%d_r" % k])
                X.brk()
                X.op("dve", lambda e: e.reciprocal(out=ss4[:, k + 8:k + 9], in_=ss4[:, k + 8:k + 9]), reads=["r%d_r" % k], writes=["r%d_r" % k])
                return ss4[:, k + 8:k + 9], "r%d_r" % k

            Tb = bkb(0).rearrange("p (a b) -> p a b", a=8)
            Tp = bkb(3).rearrange("p (a b) -> p a b", a=8)

            def A_ops(g):
                X = Rec()
                pp = g % 2
                H, Hn = h1s[pp], "h1_%d" % pp
                xnb, xnbn = xnbs[pp], "xnb_%d" % pp
                eidx, eidxn = eidxs[pp], "eidx_%d" % pp
                gate16, gaten = gate16s[pp], "gate_%d" % pp
                ts_ = slice(g * 128, (g + 1) * 128)
                X.dma("sp", lambda e: e.dma_start(out=H, in_=x_d[ts_, :]), "x0", writes=[Hn])
                att_t = att[:, g, :]
                ra, ran = rms_tile(X, att_t, "att", 0, 512, bfa[:, 0:512], "bfa")
                X.op("dve", lambda e: e.tensor_scalar(out=bfa[:, 0:512], in0=att_t, scalar1=ra, scalar2=None, op0=ALU.mult),
                     reads=["att", ran], writes=["bfa"])
                for kc in range(4):
                    X.op("pe", lambda e, kc=kc: e.transpose(out=Tb[:, kc, :], in_=bfa[:, kc * 128:(kc + 1) * 128], identity=ident),
                         reads=["bfa", "ident"], writes=["b0"])
                X.brk()
                X.op("dve", lambda e: e.tensor_tensor(out=mixT, in0=Tb[:, 0:4, :],
                                                      in1=col(C_GAO, 4).unsqueeze(2).to_broadcast([128, 4, 128]), op=ALU.mult),
                     reads=["b0", "cols"], writes=["mixT"])
                for half in range(2):
                    for kc in range(8):
                        lhs = mixT[:, kc, :] if kc < 4 else cnT[:, kc - 4, ts_]
                        X.op("pe", lambda e, half=half, kc=kc, lhs=lhs: e.matmul(bk(3 + half), lhsT=lhs,
                                                                                 rhs=w_out[:, kc, half * 512:(half + 1) * 512],
                                                                                 start=(kc == 0), stop=(kc == 7)),
                             reads=["mixT", "cnT", "w_out"], writes=["b%d" % (3 + half)])
                X.brk()
                for half in range(2):
                    X.op("dve", lambda e, half=half: e.tensor_tensor(out=H[:, half * 512:(half + 1) * 512],
                                                                     in0=H[:, half * 512:(half + 1) * 512], in1=bk(3 + half), op=ALU.add),
                         reads=[Hn, "b%d" % (3 + half)], writes=[Hn])
                r1, r1n = rms_tile(X, H, Hn, 1, D, tmp, "tmp")
                X.op("dve", lambda e: e.scalar_tensor_tensor(out=xnb, in0=H, scalar=r1, in1=g_ffn, op0=ALU.mult, op1=ALU.mult),
                     reads=[Hn, r1n, "g_ffn"], writes=[xnbn])
                for kc in range(8):
                    X.op("pe", lambda e, kc=kc: e.transpose(out=Tb[:, kc, :], in_=xnb[:, kc * 128:(kc + 1) * 128], identity=ident),
                         reads=[xnbn, "ident"], writes=["b0"])
                X.brk()
                X.op("act", lambda e: e.copy(out=xT, in_=Tb), reads=["b0"], writes=["xT"])
                for half in range(2):
                    for kc in range(8):
                        X.op("pe", lambda e, half=half, kc=kc: e.matmul(bk(3 + half), lhsT=xT[:, kc, :],
                                                                        rhs=wq[:, kc, half * 512:(half + 1) * 512],
                                                                        start=(kc == 0), stop=(kc == 7)),
                             reads=["xT", "wq"], writes=["b%d" % (3 + half)])
                X.brk()
                for half in range(2):
                    X.op("act", lambda e, half=half: e.copy(out=bfa[:, half * 512:(half + 1) * 512], in_=bk(3 + half)),
                         reads=["b%d" % (3 + half)], writes=["bfa"])
                for kc in range(8):
                    X.op("pe", lambda e, kc=kc: e.transpose(out=Tb[:, kc, :], in_=bfa[:, kc * 128:(kc + 1) * 128], identity=ident),
                         reads=["bfa", "ident"], writes=["b0"])
                X.brk()
                X.op("dve", lambda e: e.tensor_copy(out=xT, in_=Tb), reads=["b0"], writes=["xT"])
                for hh in range(8):
                    sbk = 4 + hh // 2
                    X.op("pe", lambda e, hh=hh, sbk=sbk: e.matmul(bk(sbk)[:, (hh % 2) * 256:(hh % 2 + 1) * 256], lhsT=xT[:, hh, :],
                                                                 rhs=kbd[:, hh, :], start=True, stop=True),
                         reads=["xT", "kbd"], writes=["b%d" % sbk])
                X.brk()
                for b4 in range(4):
                    X.op("act", lambda e, b4=b4: e.copy(out=tk[:, b4 * 512:(b4 + 1) * 512], in_=bk(4 + b4)),
                         reads=["b%d" % (4 + b4)], writes=["tk"])
                X.brk()
                for hc in range(16):
                    X.op("dve", lambda e, hc=hc: e.max(out=m16[:, hc, 0:8], in_=sc[:, hc, :]), reads=["tk"], writes=["m16"])
                    X.op("dve", lambda e, hc=hc: e.max_index(out=ix16[:, hc, 0:8], in_max=m16[:, hc, 0:8], in_values=sc[:, hc, :]),
                         reads=["tk", "m16"], writes=["ix16"])
                    X.op("dve", lambda e, hc=hc: e.match_replace(out=wk[:, 0:128], in_to_replace=m16[:, hc, 0:8],
                                                                 in_values=sc[:, hc, :], imm_value=-1e30),
                         reads=["tk", "m16"], writes=["wk"])
                    X.op("dve", lambda e, hc=hc: e.max(out=m16[:, hc, 8:16], in_=wk[:, 0:128]), reads=["wk"], writes=["m16"])
                    X.op("dve", lambda e, hc=hc: e.max_index(out=ix16[:, hc, 8:16], in_max=m16[:, hc, 8:16], in_values=wk[:, 0:128]),
                         reads=["wk", "m16"], writes=["ix16"])
                m4 = m16.rearrange("p (h c) k -> p h c k", c=2)
                X.op("dve", lambda e: e.tensor_tensor(out=cand.rearrange("p h (a b) -> p h a b", a=16),
                                                      in0=m4[:, :, 0, :].unsqueeze(3).to_broadcast([128, 8, 16, 16]),
                                                      in1=m4[:, :, 1, :].unsqueeze(2).to_broadcast([128, 8, 16, 16]), op=ALU.add),
                     reads=["m16", "tk"], writes=["tk"])
                for hh in range(8):
                    X.op("dve", lambda e, hh=hh: e.max(out=best[:, hh, 0:8], in_=cand[:, hh, :]), reads=["tk"], writes=["best"])
                    X.op("dve", lambda e, hh=hh: e.max_index(out=posu[:, hh, 0:8], in_max=best[:, hh, 0:8], in_values=cand[:, hh, :]),
                         reads=["tk", "best"], writes=["posu"])
                    X.op("dve", lambda e, hh=hh: e.match_replace(out=wk, in_to_replace=best[:, hh, 0:8], in_values=cand[:, hh, :],
                                                                 imm_value=-1e30), reads=["tk", "best"], writes=["wk"])
                    X.op("dve", lambda e, hh=hh: e.max(out=best[:, hh, 8:16], in_=wk), reads=["wk"], writes=["best"])
                    X.op("dve", lambda e, hh=hh: e.max_index(out=posu[:, hh, 8:16], in_max=best[:, hh, 8:16], in_values=wk),
                         reads=["wk", "best"], writes=["posu"])
                X.op("dve", lambda e: e.tensor_copy(out=posf, in_=posu), reads=["posu"], writes=["posf"])
                X.op("dve", lambda e: e.tensor_copy(out=ixf, in_=ix16), reads=["ix16"], writes=["ix16"])
                X.op("dve", lambda e: e.tensor_scalar(out=k1f, in0=posf, scalar1=-7.5, scalar2=0.0625, op0=ALU.add, op1=ALU.mult),
                     reads=["posf"], writes=["k1f"])
                X.op("dve", lambda e: e.tensor_copy(out=ki4, in_=k1f), reads=["k1f", "posf"], writes=["posu"])
                X.op("dve", lambda e: e.tensor_copy(out=k1f, in_=ki4), reads=["posu"], writes=["k1f"])
                X.op("dve", lambda e: e.scalar_tensor_tensor(out=k2f, in0=k1f, scalar=-16.0, in1=posf, op0=ALU.mult, op1=ALU.add),
                     reads=["k1f", "posf"], writes=["k2f"])
                ix4 = ixf.rearrange("p (h c) k -> p h c k", c=2)
                io_b = iota16.unsqueeze(1).unsqueeze(1).to_broadcast([128, 8, 16, 16])
                for kf, cc, eo, nm in ((k1f, 0, e1, "e1"), (k2f, 1, e2, "posf")):
                    X.op("dve", lambda e, kf=kf: e.tensor_tensor(out=oh, in0=kf.unsqueeze(3).to_broadcast([128, 8, 16, 16]),
                                                                 in1=io_b, op=ALU.is_equal),
                         reads=["k1f", "k2f", "iota16", "tk"], writes=["tk"])
                    X.op("dve", lambda e, cc=cc: e.tensor_tensor(out=oh, in0=oh,
                                                                 in1=ix4[:, :, cc, :].unsqueeze(2).to_broadcast([128, 8, 16, 16]),
                                                                 op=ALU.mult), reads=["tk", "ix16"], writes=["tk"])
                    X.op("dve", lambda e, eo=eo: e.reduce_sum(out=eo, in_=oh, axis=AX.X), reads=["tk"], writes=[nm])
                X.op("dve", lambda e: e.scalar_tensor_tensor(out=e1, in0=e1, scalar=128.0, in1=e2, op0=ALU.mult, op1=ALU.add),
                     reads=["e1", "posf"], writes=["e1"])
                X.op("dve", lambda e: e.tensor_copy(out=eidx, in_=e1.rearrange("p h k -> p (h k)")), reads=["e1"], writes=[eidxn])
                X.op("dve", lambda e: e.tensor_tensor(out=gate16, in0=best, in1=best[:, :, 0:1].to_broadcast([128, 8, 16]),
                                                      op=ALU.subtract), reads=["best"], writes=[gaten])
                X.op("act", lambda e: e.activation(out=gate16, in_=gate16, func=AF.Exp), reads=[gaten], writes=[gaten])
                X.brk()
                X.op("dve", lambda e: e.reduce_sum(out=gsum, in_=gate16, axis=AX.X), reads=[gaten], writes=["gsum"])
                X.op("dve", lambda e: e.reciprocal(out=gsum, in_=gsum), reads=["gsum"], writes=["gsum"])
                X.op("dve", lambda e: e.tensor_tensor(out=gate16, in0=gate16, in1=gsum.unsqueeze(2).to_broadcast([128, 8, 16]),
                                                      op=ALU.mult), reads=[gaten, "gsum"], writes=[gaten])
                return X.q

            TABS = ["tabu%d" % k for k in range(8)] + ["tabv%d" % k for k in range(8)]

            def B_emit(g, side, per_group):
                pp = g % 2
                H, Hn = h1s[pp], "h1_%d" % pp
                xnb, xnbn = xnbs[pp], "xnb_%d" % pp
                eidx, eidxn = eidxs[pp], "eidx_%d" % pp
                gflat, gaten = gate16s[pp].rearrange("p h k -> p (h k)"), "gate_%d" % pp

                def consume(k):
                    gs = slice(k * GRP, (k + 1) * GRP)
                    S.op("act", lambda e: e.activation(out=gl[:, gs], in_=actv[:, gs], func=AF.Gelu), reads=["actv"], writes=["gl"])
                    S.op("dve", lambda e: e.tensor_tensor(out=coef[:, gs], in0=gl[:, gs], in1=gflat[:, gs], op=ALU.mult),
                         reads=["gl", gaten], writes=["coef"])
                    for s2 in range(k * GRP, (k + 1) * GRP):
                        b2 = s2 % NB
                        dgi = s2 % 4
                        S.op("act", lambda e, s2=s2, dgi=dgi: e.activation(out=dgs[dgi], in_=ident, func=AF.Copy, scale=coef[:, s2:s2 + 1]),
                             reads=["ident", "coef"], writes=["dg%d" % dgi])
                        for half in range(2):
                            S.op("pe", lambda e, s2=s2, b2=b2, dgi=dgi, half=half: e.matmul(
                                bk(1 + half), lhsT=dgs[dgi], rhs=gb[b2][:, D + half * 512:D + (half + 1) * 512],
                                start=(s2 == 0), stop=(s2 == 127)),
                                reads=["dg%d" % dgi, "gb%d" % b2], writes=["b%d" % (1 + half)])

                for s_ in range(128):
                    b = s_ % NB
                    S.dma("pool", lambda e, s_=s_, b=b: e.indirect_dma_start(
                        out=gb[b], out_offset=None, in_=tab_d, in_offset=bass.IndirectOffsetOnAxis(ap=eidx[:, s_:s_ + 1], axis=0)),
                        "g%d" % b, reads=[eidxn] + TABS, writes=["gb%d" % b])
                    if s_ % 2 == 0 or not DOT_SPLIT:
                        S.op("dve", lambda e, s_=s_, b=b: e.scalar_tensor_tensor(out=gb[b][:, 0:D], in0=gb[b][:, 0:D], scalar=1.0, in1=xnb,
                                                                                 op0=ALU.mult, op1=ALU.mult, accum_out=actv[:, s_:s_ + 1]),
                             reads=["gb%d" % b, xnbn], writes=["gb%d" % b, "actv"])
                    else:
                        S.op("dve", lambda e, b=b: e.tensor_tensor(out=gb[b][:, 0:D], in0=gb[b][:, 0:D], in1=xnb, op=ALU.mult),
                             reads=["gb%d" % b, xnbn], writes=["gb%d" % b])
                        S.op("act", lambda e, s_=s_, b=b: e.activation(out=gb[b][:, 0:D], in_=gb[b][:, 0:D], func=AF.Copy,
                                                                       accum_out=actv[:, s_:s_ + 1]),
                             reads=["gb%d" % b], writes=["gb%d" % b, "actv"])
                    if (s_ + 1) % GRP == 0:
                        consume(s_ // GRP)
                        cnt = 0
                        while side:
                            th = side.pop(0)
                            if th is None:
                                if cnt >= 2:
                                    break
                                continue
                            th()
                            cnt += 1
                            if cnt >= per_group:
                                break
                while side:
                    th = side.pop(0)
                    if th is not None:
                        th()
                for half in range(2):
                    S.op("dve", lambda e, half=half: e.tensor_tensor(out=H[:, half * 512:(half + 1) * 512],
                                                                     in0=H[:, half * 512:(half + 1) * 512], in1=bk(1 + half), op=ALU.add),
                         reads=[Hn, "b%d" % (1 + half)], writes=[Hn])

            def C_ops(g):
                X = Rec()
                pp = g % 2
                H, Hn = h1s[pp], "h1_%d" % pp
                ts_ = slice(g * 128, (g + 1) * 128)
                X.dma("sp", lambda e: e.dma_start(out=pt_, in_=p_d[ts_, :]), "x1", writes=["mixT"])
                r2, r2n = rms_tile(X, H, Hn, 2, D, tmp, "tmp")
                X.op("dve", lambda e: e.tensor_scalar(out=bfa, in0=H, scalar1=r2, scalar2=None, op0=ALU.mult),
                     reads=[Hn, r2n], writes=["bfa"])
                X.op("act", lambda e: e.copy(out=pbf, in_=pt_), reads=["mixT"], writes=["pbf"])
                for kc in range(8):
                    X.op("pe", lambda e, kc=kc: e.transpose(out=Tb[:, kc, :], in_=bfa[:, kc * 128:(kc + 1) * 128], identity=ident),
                         reads=["bfa", "ident"], writes=["b0"])
                for kc in range(2):
                    X.op("pe", lambda e, kc=kc: e.transpose(out=Tp[:, kc, :], in_=pbf[:, kc * 128:(kc + 1) * 128], identity=ident),
                         reads=["pbf", "ident"], writes=["b3"])
                X.brk()
                X.op("dve", lambda e: e.tensor_tensor(out=xT, in0=Tb, in1=col(C_GPL, 8).unsqueeze(2).to_broadcast([128, 8, 128]),
                                                      op=ALU.mult), reads=["b0", "cols"], writes=["xT"])
                X.op("act", lambda e: e.copy(out=pT, in_=Tp[:, 0:2, :]), reads=["b3"], writes=["pT"])
                for half in range(2):
                    hs = slice(half * 512, (half + 1) * 512)
                    gbk = 5 + half
                    for kc in range(8):
                        X.op("pe", lambda e, hs=hs, kc=kc, gbk=gbk: e.matmul(bk(gbk), lhsT=xT[:, kc, :], rhs=gw[:, kc, hs],
                                                                             start=(kc == 0), stop=(kc == 7)),
                             reads=["xT", "gw"], writes=["b%d" % gbk])
                X.brk()
                for half in range(2):
                    hs = slice(half * 512, (half + 1) * 512)
                    gbk = 5 + half
                    X.op("dve", lambda e, hs=hs, gbk=gbk: e.tensor_tensor(out=tmp[:, hs], in0=bk(gbk), in1=g_b[:, hs], op=ALU.add),
                         reads=["b%d" % gbk, "g_b", "tmp"], writes=["tmp"])
                    X.op("act", lambda e, hs=hs: e.activation(out=tmp[:, hs], in_=tmp[:, hs], func=AF.Sigmoid), reads=["tmp"],
                         writes=["tmp"])
                    for kc in range(2):
                        X.op("pe", lambda e, hs=hs, kc=kc, gbk=gbk: e.matmul(bk(gbk), lhsT=pT[:, kc, :], rhs=pw[:, kc, hs],
                                                                             start=(kc == 0), stop=(kc == 1)),
                             reads=["pT", "pw"], writes=["b%d" % gbk])
                X.brk()
                for half in range(2):
                    hs = slice(half * 512, (half + 1) * 512)
                    gbk = 5 + half
                    X.op("dve", lambda e, hs=hs, gbk=gbk: e.tensor_tensor(out=tmp[:, hs], in0=tmp[:, hs], in1=bk(gbk), op=ALU.mult),
                         reads=["b%d" % gbk, "tmp"], writes=["tmp"])
                X.op("dve", lambda e: e.tensor_tensor(out=H, in0=H, in1=tmp, op=ALU.add), reads=[Hn, "tmp"], writes=[Hn])
                r3, r3n = rms_tile(X, H, Hn, 3, D, tmp, "tmp")
                X.op("dve", lambda e: e.scalar_tensor_tensor(out=tmp, in0=H, scalar=r3, in1=g_fin, op0=ALU.mult, op1=ALU.mult),
                     reads=[Hn, r3n, "g_fin", "tmp"], writes=["tmp"])
                X.dma("sp", lambda e: e.dma_start(out=out_d[ts_, :], in_=tmp), "st", reads=["tmp"], final=True)
                return X.q

            for th in A_ops(0):
                if th is not None:
                    th()
            for g in range(NT):
                side = []
                if g > 0:
                    side += C_ops(g - 1)
                if g + 1 < NT:
                    side += A_ops(g + 1)
                n_real = sum(1 for th in side if th is not None)
                per_group = max(4, int(1.25 * n_real / (128 // GRP - 4)) + 1)
                B_emit(g, side, per_group)
            for th in C_ops(NT - 1):
                if th is not None:
                    th()
        else:
            z = A.alloc([128, D], F32, {4})
            S.op("dve", lambda e: e.memset(z, 0.0), writes=["z"])
            S.dma("sp", lambda e: e.dma_start(out=out_d[0:128, :], in_=z), "st", reads=["z"], final=True)

        stats = S.emit()
        print("program ops per engine:", stats)
    return nc


def make_in_maps(inputs):
    f = np.float32
    g = lambda k: np.asarray(inputs[k])
    x = g("x").astype(f, copy=False)
    p = g("p").astype(f, copy=False)[0]
    pos = g("positions").astype(np.int32, copy=False)

    def colmaj(v):
        v = np.asarray(v, f).reshape(-1, 128)
        return np.ascontiguousarray(v.T)

    cols = np.zeros((128, NCOL), f)
    cols[:, C_GA:C_GA + 8] = colmaj(g("attn_norm")[0])
    cols[:, C_GQ:C_GQ + 4] = colmaj(g("q_norm")[0])
    cols[:, C_GKV:C_GKV + 2] = colmaj(g("kv_norm")[0])
    cols[:, C_CB:C_CB + 4] = colmaj(g("conv_b")[0])
    cols[:, C_LNG:C_LNG + 4] = colmaj(g("conv_ln_g")[0])
    cols[:, C_LNB:C_LNB + 4] = colmaj(g("conv_ln_b")[0])
    cols[:, C_CG:C_CG + 4] = colmaj(g("conv_out_norm")[0])
    cols[:, C_GAO:C_GAO + 4] = colmaj(g("attn_out_norm")[0])
    cols[:, C_GPL:C_GPL + 8] = colmaj(g("pl_norm")[0])
    half = 16
    freqs = (np.float32(10000.0) ** (-np.arange(half, dtype=f) / np.float32(half))).astype(f)
    for pp in range(64, 96):
        cols[pp, C_FREQ] = freqs[(pp - 64) % 16]
    cw = np.asarray(g("conv_w")[0], f)
    cols[:, C_CW:C_CW + 124] = cw.reshape(31, 4, 128).transpose(2, 1, 0).reshape(128, 124)
    rows = np.stack([np.broadcast_to(np.asarray(g(k), f).reshape(-1)[None, :], (128, D))
                     for k in ("ffn_norm", "pl_gate_b", "final_norm")]).astype(f)
    rows = np.ascontiguousarray(rows)
    cst = np.zeros((3, 128, 128), f)
    cst[0] = np.eye(128, dtype=f)
    cst[1] = np.triu(np.ones((128, 128), f))
    cst[2, :, 0:16] = np.arange(16, dtype=f)[None, :]
    keysT = np.ascontiguousarray(np.asarray(g("peer_keys")[0], f).transpose(1, 3, 0, 2).reshape(128, 8, 128))
    shared = {
        "w_in": np.ascontiguousarray(g("w_in")[0], f), "w_uq": np.ascontiguousarray(g("w_uq")[0], f),
        "w_ukv": np.ascontiguousarray(g("w_ukv")[0], f), "w_out": np.ascontiguousarray(g("w_out")[0], f),
        "peer_wq": np.ascontiguousarray(g("peer_wq")[0], f), "keysT": keysT,
        "peer_u": np.ascontiguousarray(g("peer_u")[0], f), "peer_v": np.ascontiguousarray(g("peer_v")[0], f),
        "pl_gate_w": np.ascontiguousarray(g("pl_gate_w")[0], f), "pl_proj": np.ascontiguousarray(g("pl_proj")[0], f),
        "cols": cols, "rows": rows, "cst": cst,
    }
    maps = []
    for b in range(8):
        m = dict(shared)
        m["x"] = np.ascontiguousarray(x[b])
        m["p"] = np.ascontiguousarray(p[b])
        m["pos"] = np.ascontiguousarray(np.broadcast_to(pos[b][None, :], (32, T))).astype(np.int32)
        maps.append(m)
    return maps


_NC_CACHE = {}


def kernel(**inputs):
    maps = make_in_maps(inputs)
    if "nc" not in _NC_CACHE:
        _NC_CACHE["nc"] = build_program()
    nc = _NC_CACHE["nc"]
    res = run_bass_kernel_spmd(nc, maps, core_ids=list(range(8)))
    out = np.stack([np.asarray(r["out"], np.float32) for r in res.results], axis=0)
    return out.reshape(8, T, D)
```

```python
import contextlib
import numpy as np
import concourse.bass as bass
import concourse.mybir as mybir
from concourse.bass_utils import run_bass_kernel_spmd

F32 = mybir.dt.float32
BF16 = mybir.dt.bfloat16
I32 = mybir.dt.int32
U32 = mybir.dt.uint32
AF = mybir.ActivationFunctionType
ALU = mybir.AluOpType
AX = mybir.AxisListType

T = 4096
D = 1024
NT = 32
NCH = 8
EPS = 1e-6
ATT_SCALE = 96 ** -0.5
ENGS = ["pe", "act", "dve", "pool", "sp"]
SAME_ENGINE_RAW = True
SAME_ENGINE_WAR = False
DOT_SPLIT = False

C_GA = 0
C_GQ = 8
C_GKV = 12
C_CB = 14
C_LNG = 18
C_LNB = 22
C_CG = 26
C_GAO = 30
C_GPL = 34
C_FREQ = 42
C_CW = 44
NCOL = C_CW + 4 * 31


class Sched:
    def __init__(self, nc):
        self.nc = nc
        self.ops = {e: [] for e in ENGS}
        self.res = {}
        self.seen = {e: {} for e in ENGS}
        self.chan_n = {}
        self.pending = {e: [] for e in ENGS}
        self.final_waits = []

    def _need(self, eng, prod, waits):
        if prod is None:
            return
        kind, key, n = prod
        if kind == "e" and key == eng:
            if key == "pe" or not SAME_ENGINE_RAW:
                return
        k = (kind, key)
        if self.seen[eng].get(k, -1) >= n:
            return
        self.seen[eng][k] = n
        waits.append(prod)

    def _deps(self, eng, reads, writes, waits):
        for r in reads:
            st = self.res.setdefault(r, {"w": None, "r": {}})
            self._need(eng, st["w"], waits)
        for w in writes:
            st = self.res.setdefault(w, {"w": None, "r": {}})
            p = st["w"]
            if p is not None and (SAME_ENGINE_WAR or not (p[0] == "e" and p[1] == eng)):
                self._need(eng, p, waits)
            for k, rp in st["r"].items():
                if rp[0] == "e" and rp[1] == eng and not SAME_ENGINE_WAR:
                    continue
                self._need(eng, rp, waits)

    def _commit(self, tok, reads, writes):
        for r in reads:
            self.res[r]["r"][(tok[0], tok[1])] = tok
        for w in writes:
            st = self.res[w]
            st["w"] = tok
            st["r"] = {}

    def _take_pending(self, eng):
        waits = list(self.pending[eng])
        self.pending[eng] = []
        for p in waits:
            k = (p[0], p[1])
            self.seen[eng][k] = max(self.seen[eng].get(k, -1), p[2])
        return waits

    def op(self, eng, fn, reads=(), writes=()):
        waits = self._take_pending(eng)
        self._deps(eng, reads, writes, waits)
        idx = len(self.ops[eng])
        self.ops[eng].append({"fn": fn, "waits": waits, "sig": False, "dma": None})
        self._commit(("e", eng, idx), reads, writes)

    def dma(self, eng, fn, ch, reads=(), writes=(), final=False):
        ch = "%s_%s" % (ch, eng)
        waits = self._take_pending(eng)
        n = self.chan_n.get(ch, 0)
        if n > 0:
            self._need(eng, ("d", ch, n), waits)
        self._deps(eng, reads, writes, waits)
        n += 1
        self.chan_n[ch] = n
        self.ops[eng].append({"fn": fn, "waits": waits, "sig": False, "dma": ch})
        self._commit(("d", ch, n), reads, writes)
        if final and ch not in self.final_waits:
            self.final_waits.append(ch)

    def barrier(self):
        prods = []
        for e in ENGS:
            for i in range(len(self.ops[e]) - 1, -1, -1):
                if self.ops[e][i]["dma"] is None:
                    prods.append(("e", e, i))
                    break
        for ch, n in self.chan_n.items():
            if not ch.startswith("tb"):
                prods.append(("d", ch, n))
        for e in ENGS:
            self.pending[e] = [p for p in prods if not (p[0] == "e" and p[1] == e)]
        self.res = {k: v for k, v in self.res.items() if k.startswith("tab")}

    def emit(self):
        nc = self.nc
        for e in ENGS:
            for o in self.ops[e]:
                for (kind, key, n) in o["waits"]:
                    if kind == "e":
                        self.ops[key][n]["sig"] = True
        cnt = {}
        for e in ENGS:
            c = 0
            for o in self.ops[e]:
                if o["sig"]:
                    c += 1
                o["cnt"] = c
            cnt[e] = c
        with contextlib.ExitStack() as st:
            esem = {e: st.enter_context(nc.semaphore("s_" + e)) for e in ENGS if cnt[e] > 0}
            csem = {ch: st.enter_context(nc.semaphore("c_%s" % (ch,))) for ch in self.chan_n}
            block = st.enter_context(nc.Block())
            handles = {"pe": block.tensor, "act": block.scalar, "dve": block.vector,
                       "pool": block.gpsimd, "sp": block.sync}

            def make(e):
                def body(eng):
                    for o in self.ops[e]:
                        for (kind, key, n) in o["waits"]:
                            if kind == "e":
                                eng.wait_ge(esem[key], self.ops[key][n]["cnt"])
                            else:
                                eng.wait_ge(csem[key], 16 * n)
                        ins = o["fn"](eng)
                        if o["dma"] is not None:
                            ins.then_inc(csem[o["dma"]], 16)
                        elif o["sig"]:
                            ins.then_inc(esem[e], 1)
                    if e == "sp":
                        for ch in self.final_waits:
                            eng.wait_ge(csem[ch], 16 * self.chan_n[ch])
                return body

            for e in ENGS:
                if self.ops[e] or e == "sp":
                    handles[e](make(e))
        return {e: len(self.ops[e]) for e in ENGS}, cnt


class Arena:
    def __init__(self, base_ap, nbytes):
        self.base = base_ap
        self.nbytes = nbytes
        self.allocs = []

    def alloc(self, shape, dt, phases):
        esz = {F32: 4, BF16: 2, I32: 4, U32: 4}[dt]
        n = 1
        for s in shape[1:]:
            n *= s
        size = (n * esz + 31) // 32 * 32
        phases = set(phases)
        cands = sorted({0} | {o + s for (o, s, p) in self.allocs})
        for off in cands:
            ok = off + size <= self.nbytes
            if ok:
                for (o, s, p) in self.allocs:
                    if p & phases and off < o + s and o < off + size:
                        ok = False
                        break
            if ok:
                self.allocs.append((off, size, phases))
                v = self.base[:, off // 4:(off + size) // 4]
                if dt != F32:
                    v = v.bitcast(dt)
                v = v[:, 0:n]
                if len(shape) == 3:
                    v = v.rearrange("p (a b) -> p a b", a=shape[1])
                elif len(shape) == 4:
                    v = v.rearrange("p (a b c) -> p a b c", a=shape[1], b=shape[2])
                return v
        raise RuntimeError("arena full: %s %s %s" % (shape, dt, phases))


def build_program(upto=4, debug=False):
    nc = bass.Bass("TRN2", target_bir_lowering=False)

    def din(name, shape, dt=F32):
        return nc.dram_tensor(name, shape, dt, kind="ExternalInput").ap()

    x_d = din("x", [T, D])
    p_d = din("p", [T, 256])
    pos_d = din("pos", [32, T], I32)
    w_in_d = din("w_in", [D, 1824])
    w_uq_d = din("w_uq", [512, 768])
    w_ukv_d = din("w_ukv", [256, 1024])
    w_out_d = din("w_out", [D, D])
    wq_d = din("peer_wq", [D, D])
    keysT_d = din("keysT", [128, 8, 128])
    u_d = din("peer_u", [16384, D])
    v_d = din("peer_v", [16384, D])
    gw_d = din("pl_gate_w", [D, D])
    pw_d = din("pl_proj", [256, D])
    cols_d = din("cols", [128, NCOL])
    rows_d = din("rows", [3, 128, D])
    cst_d = din("cst", [3, 128, 128])
    out_d = nc.dram_tensor("out", [T, D], F32, kind="ExternalOutput").ap()
    tab_d = nc.dram_tensor("peer_tab", [16384, 2 * D], BF16, kind="Internal").ap()
    dbg_d = None
    if debug:
        dbg_d = {
            "d_qnT": nc.dram_tensor("d_qnT", [128, 4 * T], BF16, kind="ExternalOutput").ap(),
            "d_kvnT": nc.dram_tensor("d_kvnT", [128, 2 * T], BF16, kind="ExternalOutput").ap(),
            "d_kpeT": nc.dram_tensor("d_kpeT", [128, T], BF16, kind="ExternalOutput").ap(),
            "d_V": nc.dram_tensor("d_V", [128, NT * 8 * 65], BF16, kind="ExternalOutput").ap(),
            "d_att": nc.dram_tensor("d_att", [128, NT * 512], BF16, kind="ExternalOutput").ap(),
            "d_cnT": nc.dram_tensor("d_cnT", [128, 4 * T], BF16, kind="ExternalOutput").ap(),
        }

    ARENA_BYTES = 212800
    with contextlib.ExitStack() as st:
        arena_t = st.enter_context(nc.sbuf_tensor("arena", [128, ARENA_BYTES // 4], F32))
        banks = [st.enter_context(nc.psum_tensor("bank%d" % i, [128, 512], F32)) for i in range(8)]
        A = Arena(arena_t[:, :], ARENA_BYTES)
        S = Sched(nc)
        ALLP = {1, 2, 3, 4}

        def bk(i):
            return banks[i][:, :]

        def bkb(i):
            return banks[i][:, :].bitcast(BF16)

        cols = A.alloc([128, NCOL], F32, ALLP)
        ident = A.alloc([128, 128], BF16, ALLP)
        tri = A.alloc([128, 128], BF16, {1, 2})
        ones = A.alloc([128, 128], BF16, {1, 2, 3})
        iota16 = A.alloc([128, 16], F32, ALLP)
        scr = A.alloc([128, 64], F32, ALLP)

        S.dma("sp", lambda e: e.dma_start(out=cols, in_=cols_d), "c0", writes=["cols"])
        S.dma("pool", lambda e: e.dma_start(out=ident, in_=cst_d[0]), "c1", writes=["ident"])
        S.dma("pool", lambda e: e.dma_start(out=tri, in_=cst_d[1]), "c2", writes=["tri"])
        S.dma("sp", lambda e: e.dma_start(out=iota16, in_=cst_d[2][:, 0:16]), "c3", writes=["iota16"])
        S.op("dve", lambda e: e.memset(ones, 1.0), writes=["ones"])

        def col(c0, n=1):
            return cols[:, c0:c0 + n]

        att = A.alloc([128, NT, 512], BF16, {2, 3, 4})
        cnT = A.alloc([128, 4, T], BF16, {3, 4})
        qnT = A.alloc([128, 4, T], BF16, {1, 2})
        kvnT = A.alloc([128, 2, T], BF16, {1, 2})
        kpeT = A.alloc([128, T], BF16, {1, 2})
        Vt = A.alloc([128, NT, 8, 65], BF16, {1, 2})
        cosT = A.alloc([128, T], BF16, {1, 2})
        sinT = A.alloc([128, T], BF16, {1, 2})
        w_uq = A.alloc([128, 4, 768], BF16, {1, 2})
        w_uqr = A.alloc([128, 4, 8, 96], BF16, {1, 2})
        w_ukv = A.alloc([128, 2, 1024], BF16, {1, 2})

        def rstd_from_ss(ss_ap, n_feat, out_ap, tag):
            S.op("act", lambda e: e.activation(out=out_ap, in_=ss_ap, func=AF.Sqrt, scale=1.0 / n_feat, bias=EPS),
                 reads=[tag + "_ss"], writes=[tag + "_r"])
            S.op("dve", lambda e: e.reciprocal(out=out_ap, in_=out_ap), reads=[tag + "_r"], writes=[tag + "_r"])

        def x_to_hnT(c, xt, xsb, junkb, hnT, ssx, hname="hnT"):
            for t4 in range(4):
                g = 4 * c + t4
                par = g % 2
                S.dma("sp", lambda e, g=g, par=par: e.dma_start(out=xt[par], in_=x_d[g * 128:(g + 1) * 128, :]),
                      "x%d" % par, writes=["xt%d" % par])
                S.op("act", lambda e, par=par: e.activation(out=junkb, in_=xt[par], func=AF.Square,
                                                            accum_out=ssx[:, par:par + 1]),
                     reads=["xt%d" % par], writes=["junkb", "x%d_ss" % par])
                rstd_from_ss(ssx[:, par:par + 1], D, ssx[:, 2 + par:3 + par], "x%d" % par)
                S.op("dve", lambda e, par=par: e.tensor_scalar(out=xsb, in0=xt[par], scalar1=ssx[:, 2 + par:3 + par],
                                                               scalar2=None, op0=ALU.mult),
                     reads=["xt%d" % par, "x%d_r" % par], writes=["xsb"])
                Tb = bkb(0).rearrange("p (a b) -> p a b", a=8)
                for kc in range(8):
                    S.op("pe", lambda e, kc=kc: e.transpose(out=Tb[:, kc, :], in_=xsb[:, kc * 128:(kc + 1) * 128],
                                                            identity=ident),
                         reads=["xsb", "ident"], writes=["b0"])
                S.op("dve", lambda e, t4=t4: e.tensor_tensor(
                    out=hnT[:, :, t4 * 128:(t4 + 1) * 128], in0=Tb,
                    in1=col(C_GA, 8).unsqueeze(2).to_broadcast([128, 8, 128]), op=ALU.mult),
                    reads=["b0", "cols"], writes=[hname])

        def norm_fm(zbanks, n_oc, n_feat, gcol, dst, c, sq, sd, tag):
            for oc in range(n_oc):
                S.op("act", lambda e, oc=oc: e.activation(out=sq[:, oc, :], in_=bk(zbanks[oc]), func=AF.Square),
                     reads=["b%d" % zbanks[oc]], writes=["sq"])
            for oc in range(n_oc):
                S.op("pe", lambda e, oc=oc: e.matmul(bk(5), lhsT=ones, rhs=sq[:, oc, :], start=(oc == 0),
                                                     stop=(oc == n_oc - 1)),
                     reads=["sq", "ones"], writes=["b5"])
            S.op("act", lambda e: e.activation(out=sd, in_=bk(5), func=AF.Sqrt, scale=1.0 / n_feat, bias=EPS),
                 reads=["b5"], writes=["sd"])
            S.op("dve", lambda e: e.reciprocal(out=sd, in_=sd), reads=["sd"], writes=["sd"])
            for oc in range(n_oc):
                S.op("dve", lambda e, oc=oc: e.scalar_tensor_tensor(
                    out=dst[:, oc, c * 512:(c + 1) * 512], in0=bk(zbanks[oc]), scalar=col(gcol + oc), in1=sd,
                    op0=ALU.mult, op1=ALU.mult),
                    reads=["b%d" % zbanks[oc], "sd", "cols"], writes=[tag])

        w_in1 = A.alloc([128, 8, 800], BF16, {1})
        wkpe = A.alloc([128, 8, 96], BF16, {1})
        wkper = A.alloc([128, 8, 96], BF16, {1})
        xt = [A.alloc([128, D], F32, {1}), A.alloc([128, D], F32, {1})]
        xsb = A.alloc([128, D], BF16, {1})
        junkb = A.alloc([128, D], BF16, {1})
        hnT2 = [A.alloc([128, 8, 512], BF16, {1}) for _ in range(2)]
        sq = A.alloc([128, 4, 512], BF16, {1})
        sd = A.alloc([128, 512], F32, {1})
        tmpa = A.alloc([128, 512], F32, {1})
        tmpb = A.alloc([128, 512], F32, {1})
        ssx = scr[:, 0:4]

        w_in_v = w_in_d.rearrange("(k p) c -> p k c", p=128)
        S.dma("pool", lambda e: e.dma_start(out=w_in1, in_=w_in_v[:, :, 0:800]), "w0", writes=["w_in1"])
        S.op("dve", lambda e: e.memset(wkpe, 0.0), writes=["wkpe"])
        S.op("dve", lambda e: e.memset(wkper, 0.0), writes=["wkper"])
        S.dma("pool", lambda e: e.dma_start(out=wkpe[:, :, 64:96], in_=w_in_v[:, :, 768:800]), "w1", writes=["wkpe"])
        S.dma("pool", lambda e: e.dma_start(out=wkper[:, :, 64:80], in_=w_in_v[:, :, 784:800]), "w2", writes=["wkper"])
        S.dma("pool", lambda e: e.dma_start(out=wkper[:, :, 80:96], in_=w_in_v[:, :, 768:784]), "w3", writes=["wkper"])
        S.op("dve", lambda e: e.tensor_scalar(out=wkper[:, :, 64:80], in0=wkper[:, :, 64:80], scalar1=-1.0,
                                              scalar2=None, op0=ALU.mult), reads=["wkper"], writes=["wkper"])
        S.dma("pool", lambda e: e.dma_start(out=w_uq, in_=w_uq_d.rearrange("(k p) c -> p k c", p=128)), "w4",
              writes=["w_uq"])
        S.dma("pool", lambda e: e.dma_start(out=w_ukv, in_=w_ukv_d.rearrange("(k p) c -> p k c", p=128)), "w5",
              writes=["w_ukv"])
        S.op("dve", lambda e: e.memset(w_uqr, 0.0), writes=["w_uqr"])
        w_uq_h = w_uq.rearrange("p k (h c) -> p k h c", h=8)
        S.op("dve", lambda e: e.tensor_scalar(out=w_uqr[:, :, :, 64:80], in0=w_uq_h[:, :, :, 80:96], scalar1=-1.0,
                                              scalar2=None, op0=ALU.mult), reads=["w_uq", "w_uqr"], writes=["w_uqr"])
        S.op("dve", lambda e: e.tensor_copy(out=w_uqr[:, :, :, 80:96], in_=w_uq_h[:, :, :, 64:80]),
             reads=["w_uq", "w_uqr"], writes=["w_uqr"])
        for k in range(8):
            rs = slice(k * 2048, (k + 1) * 2048)
            S.dma("pool", lambda e, rs=rs: e.dma_start(out=tab_d[rs, 0:D], in_=u_d[rs, :]), "tbu%d" % k, writes=["tabu%d" % k])
            S.dma("pool", lambda e, rs=rs: e.dma_start(out=tab_d[rs, D:2 * D], in_=v_d[rs, :]), "tbv%d" % k, writes=["tabv%d" % k])
        S.op("dve", lambda e: e.memset(Vt[:, :, :, 64:65], 1.0), writes=["Vones"])

        posi = xt[0].bitcast(I32)
        ya = xt[1]
        ki = tmpa.bitcast(I32)
        R = slice(64, 96)
        for blk in range(8):
            cs = slice(blk * 512, (blk + 1) * 512)
            S.dma("sp", lambda e, cs=cs: e.dma_start(out=posi[R, 0:512], in_=pos_d[:, cs]), "x0", writes=["posi"])
            S.op("dve", lambda e: e.tensor_copy(out=ya[R, 0:512], in_=posi[R, 0:512]), reads=["posi"], writes=["ya"])
            S.op("dve", lambda e: e.tensor_scalar(out=ya[R, 0:512], in0=ya[R, 0:512], scalar1=cols[R, C_FREQ:C_FREQ + 1],
                                                  scalar2=1.0 / (2 * np.pi), op0=ALU.mult, op1=ALU.mult),
                 reads=["ya", "cols"], writes=["ya"])
            for which, tab, shift in (("s", sinT, 0.0), ("c", cosT, 0.25)):
                if shift:
                    S.op("dve", lambda e: e.tensor_scalar(out=ya[R, 512:1024], in0=ya[R, 0:512], scalar1=0.25,
                                                          scalar2=None, op0=ALU.add), reads=["ya"], writes=["yb"])
                    src = ya[R, 512:1024]
                    rname = "yb"
                else:
                    src = ya[R, 0:512]
                    rname = "ya"
                S.op("dve", lambda e, src=src: e.tensor_copy(out=ki[R, :], in_=src), reads=[rname], writes=["ki"])
                S.op("dve", lambda e: e.tensor_copy(out=tmpb[R, :], in_=ki[R, :]), reads=["ki"], writes=["tmpb"])
                S.op("dve", lambda e, src=src: e.tensor_tensor(out=tmpb[R, :], in0=src, in1=tmpb[R, :], op=ALU.subtract),
                     reads=[rname, "tmpb"], writes=["tmpb"])
                S.op("act", lambda e, tab=tab, cs=cs: e.activation(out=tab[R, cs], in_=tmpb[R, :], func=AF.Sin,
                                                                   scale=6.28318),
                     reads=["tmpb"], writes=["rope" + which])

        S.barrier()
        if upto >= 1:
            for c in range(NCH):
                hnT = hnT2[c % 2]
                HN = "hnT%d" % (c % 2)
                x_to_hnT(c, xt, xsb, junkb, hnT, ssx, HN)
                for oc in range(4):
                    for kc in range(8):
                        S.op("pe", lambda e, oc=oc, kc=kc, hnT=hnT: e.matmul(bk(1 + oc), lhsT=w_in1[:, kc, oc * 128:(oc + 1) * 128],
                                                                    rhs=hnT[:, kc, :], start=(kc == 0), stop=(kc == 7)),
                             reads=[HN, "w_in1"], writes=["b%d" % (1 + oc)])
                norm_fm([1, 2, 3, 4], 4, 512, C_GQ, qnT, c, sq, sd, "qnT")
                for oc in range(2):
                    for kc in range(8):
                        S.op("pe", lambda e, oc=oc, kc=kc, hnT=hnT: e.matmul(bk(1 + oc), lhsT=w_in1[:, kc, 512 + oc * 128:512 + (oc + 1) * 128],
                                                                    rhs=hnT[:, kc, :], start=(kc == 0), stop=(kc == 7)),
                             reads=[HN, "w_in1"], writes=["b%d" % (1 + oc)])
                norm_fm([1, 2], 2, 256, C_GKV, kvnT, c, sq, sd, "kvnT")
                for kc in range(8):
                    S.op("pe", lambda e, kc=kc, hnT=hnT: e.matmul(bk(6)[0:96, :], lhsT=wkpe[:, kc, :], rhs=hnT[:, kc, :],
                                                         start=(kc == 0), stop=(kc == 7)),
                         reads=[HN, "wkpe"], writes=["b6"])
                for kc in range(8):
                    S.op("pe", lambda e, kc=kc, hnT=hnT: e.matmul(bk(7)[0:96, :], lhsT=wkper[:, kc, :], rhs=hnT[:, kc, :],
                                                         start=(kc == 0), stop=(kc == 7)),
                         reads=[HN, "wkper"], writes=["b7"])
                cs = slice(c * 512, (c + 1) * 512)
                S.op("dve", lambda e, cs=cs: e.tensor_tensor(out=tmpa[R, :], in0=bk(6)[R, :], in1=cosT[R, cs], op=ALU.mult),
                     reads=["b6", "ropec"], writes=["tmpa"])
                S.op("dve", lambda e, cs=cs: e.tensor_tensor(out=tmpb[R, :], in0=bk(7)[R, :], in1=sinT[R, cs], op=ALU.mult),
                     reads=["b7", "ropes"], writes=["tmpb"])
                S.op("dve", lambda e, cs=cs: e.tensor_tensor(out=kpeT[R, cs], in0=tmpa[R, :], in1=tmpb[R, :], op=ALU.add),
                     reads=["tmpa", "tmpb"], writes=["kpeT"])
                w_ukv_h = w_ukv.rearrange("p k (h c) -> p k h c", h=8)
                for t4 in range(4):
                    g = 4 * c + t4
                    vb = 6 + (t4 % 2)
                    for kc in range(2):
                        S.op("pe", lambda e, kc=kc, g=g, vb=vb: e.matmul(
                            bk(vb).rearrange("p (h c) -> p h c", h=8), lhsT=kvnT[:, kc, g * 128:(g + 1) * 128],
                            rhs=w_ukv_h[:, kc, :, 64:128], start=(kc == 0), stop=(kc == 1)),
                            reads=["kvnT", "w_ukv"], writes=["b%d" % vb])
                    S.op("act", lambda e, g=g, vb=vb: e.copy(out=Vt[:, g, :, 0:64],
                                                            in_=bk(vb).rearrange("p (h c) -> p h c", h=8)),
                         reads=["b%d" % vb], writes=["V"])
        S.barrier()

        QT = [A.alloc([128, T], BF16, {2}), A.alloc([128, T], BF16, {2})]
        KT = [A.alloc([128, T], BF16, {2}), A.alloc([128, T], BF16, {2})]
        NPT = 5
        PT = [A.alloc([128, 512], BF16, {2}) for _ in range(NPT)]
        sqq = A.alloc([128, 512], BF16, {2})
        t2a = A.alloc([128, 512], F32, {2})
        t2b = A.alloc([128, 512], F32, {2})
        mxq = A.alloc([128, 16], F32, {2})
        negm = A.alloc([128, 8], F32, {2})
        rec = A.alloc([128, 8], F32, {2})
        w_ukv_h = w_ukv.rearrange("p k (h c) -> p k h c", h=8)

        class Defer:
            def __init__(self):
                self.q = []
            def op(self, *a, **k):
                self.q.append(lambda: S.op(*a, **k))

        def build_qk(h, X=None):
            X = X or S
            hp = h % 2
            qt, kt = QT[hp], KT[hp]
            X.op("pool", lambda e: e.tensor_copy(out=kt[R, :], in_=kpeT[R, :]), reads=["kpeT"], writes=["KT%d" % hp])
            for c in range(NCH):
                cs = slice(c * 512, (c + 1) * 512)
                for kc in range(4):
                    X.op("pe", lambda e, kc=kc, cs=cs: e.matmul(bk(0)[0:96, :], lhsT=w_uq[:, kc, h * 96:(h + 1) * 96],
                                                                rhs=qnT[:, kc, cs], start=(kc == 0), stop=(kc == 3)),
                         reads=["qnT", "w_uq"], writes=["b0"])
                for kc in range(4):
                    X.op("pe", lambda e, kc=kc, cs=cs: e.matmul(bk(1)[0:96, :], lhsT=w_uqr[:, kc, h, :],
                                                                rhs=qnT[:, kc, cs], start=(kc == 0), stop=(kc == 3)),
                         reads=["qnT", "w_uqr"], writes=["b1"])
                for kc in range(2):
                    X.op("pe", lambda e, kc=kc, cs=cs: e.matmul(bk(2)[0:64, :], lhsT=w_ukv_h[:, kc, h, 0:64],
                                                                rhs=kvnT[:, kc, cs], start=(kc == 0), stop=(kc == 1)),
                         reads=["kvnT", "w_ukv"], writes=["b2"])
                X.op("act", lambda e, cs=cs: e.copy(out=qt[0:64, cs], in_=bk(0)[0:64, :]), reads=["b0"],
                     writes=["QT%d" % hp])
                X.op("dve", lambda e, cs=cs: e.tensor_tensor(out=t2a[R, :], in0=bk(0)[R, :], in1=cosT[R, cs], op=ALU.mult),
                     reads=["b0", "ropec"], writes=["t2a"])
                X.op("dve", lambda e, cs=cs: e.tensor_tensor(out=t2b[R, :], in0=bk(1)[R, :], in1=sinT[R, cs], op=ALU.mult),
                     reads=["b1", "ropes"], writes=["t2b"])
                X.op("dve", lambda e, cs=cs: e.tensor_tensor(out=qt[R, cs], in0=t2a[R, :], in1=t2b[R, :], op=ALU.add),
                     reads=["t2a", "t2b"], writes=["QT%d" % hp])
                X.op("act", lambda e, cs=cs: e.copy(out=kt[0:64, cs], in_=bk(2)[0:64, :]), reads=["b2"],
                     writes=["KT%d" % hp])
                for which, src, col0 in (("q", qt, 0), ("k", kt, 8)):
                    X.op("act", lambda e, src=src, cs=cs: e.activation(out=sqq[0:96, :], in_=src[0:96, cs], func=AF.Square),
                         reads=[("QT%d" if which == "q" else "KT%d") % hp], writes=["sqq"])
                    X.op("pe", lambda e: e.matmul(bk(2), lhsT=ones[0:96, :], rhs=sqq[0:96, :], start=True, stop=True),
                         reads=["sqq", "ones"], writes=["b2"])
                    X.op("dve", lambda e, col0=col0, c=c: e.reduce_max(out=mxq[:, col0 + c:col0 + c + 1], in_=bk(2), axis=AX.X),
                         reads=["b2"], writes=["mxq"])
            X.op("dve", lambda e: e.reduce_max(out=scr[:, 8:9], in_=mxq[:, 0:8], axis=AX.X), reads=["mxq"], writes=["scr8"])
            X.op("dve", lambda e: e.reduce_max(out=scr[:, 9:10], in_=mxq[:, 8:16], axis=AX.X), reads=["mxq"], writes=["scr9"])
            X.op("dve", lambda e: e.tensor_tensor(out=scr[:, 10:11], in0=scr[:, 8:9], in1=scr[:, 9:10], op=ALU.mult),
                 reads=["scr8", "scr9"], writes=["scr10"])
            X.op("act", lambda e: e.activation(out=scr[:, 11:12], in_=scr[:, 10:11], func=AF.Sqrt), reads=["scr10"],
                 writes=["scr11"])
            X.op("dve", lambda e: e.tensor_scalar(out=negm[:, h:h + 1], in0=scr[:, 11:12], scalar1=-ATT_SCALE, scalar2=None,
                                                  op0=ALU.mult), reads=["scr11"], writes=["negm%d" % h])

        def attn(h, side=None):
            side = side or []
            hp = h % 2
            qt, kt = QT[hp], KT[hp]
            steps = []
            for j in range(NCH):
                for i in range(4 * j + 4):
                    steps.append((j, i))

            def emit_S(n):
                j, i = steps[n]
                q0 = max(512 * j, 128 * i)
                w = 512 * j + 512 - q0
                sb_ = 3 + (n % 3)
                pt = PT[n % NPT]
                ptn = "PT%d" % (n % NPT)
                S.op("pe", lambda e: e.matmul(bk(sb_)[:, 0:w], lhsT=kt[0:96, i * 128:(i + 1) * 128], rhs=qt[0:96, q0:q0 + w],
                                              start=True, stop=True),
                     reads=["QT%d" % hp, "KT%d" % hp], writes=["b%d" % sb_])
                S.op("act", lambda e: e.activation(out=pt[:, 0:w], in_=bk(sb_)[:, 0:w], func=AF.Exp, scale=ATT_SCALE,
                                                   bias=negm[:, h:h + 1]),
                     reads=["b%d" % sb_, "negm%d" % h], writes=[ptn])
                if 128 * i >= 512 * j:
                    S.op("pool", lambda e: e.tensor_tensor(out=pt[:, 0:128], in0=pt[:, 0:128], in1=tri, op=ALU.mult),
                         reads=[ptn, "tri"], writes=[ptn])

            def emit_PV(n):
                j, i = steps[n]
                q0 = max(512 * j, 128 * i)
                r0 = (q0 - 512 * j) // 128
                pt = PT[n % NPT]
                ptn = "PT%d" % (n % NPT)
                ob = 6 + (j % 2)
                O = bk(ob)[:, 0:260].rearrange("p (r c) -> p r c", r=4)
                for rr in range(r0, 4):
                    S.op("pe", lambda e, rr=rr: e.matmul(O[:, rr, :], lhsT=pt[:, (rr - r0) * 128:(rr - r0 + 1) * 128], rhs=Vt[:, i, h, :],
                                                         start=(i == 0 and rr == 0), stop=(i == 4 * j + 3 and rr == 3)),
                         reads=[ptn, "V", "Vones"], writes=["b%d" % ob])
                if i == 4 * j + 3:
                    S.op("dve", lambda e: e.reciprocal(out=rec[:, 0:4], in_=O[:, :, 64]), reads=["b%d" % ob], writes=["rec"])
                    S.op("dve", lambda e: e.tensor_tensor(out=att[:, 4 * j:4 * j + 4, h * 64:(h + 1) * 64], in0=O[:, :, 0:64],
                                                          in1=rec[:, 0:4].unsqueeze(2).to_broadcast([128, 4, 64]), op=ALU.mult),
                         reads=["b%d" % ob, "rec"], writes=["att"])

            emit_S(0)
            emit_S(1)
            per_step = (len(side) + 99) // 100
            for n in range(len(steps)):
                if n + 2 < len(steps):
                    emit_S(n + 2)
                emit_PV(n)
                for _ in range(per_step):
                    if side:
                        side.pop(0)()
            while side:
                side.pop(0)()

        if upto >= 2:
            build_qk(0)
            for h in range(8):
                side = []
                if h + 1 < 8:
                    dq = Defer()
                    build_qk(h + 1, dq)
                    side = dq.q
                attn(h, side)
        if debug and upto <= 2:
            for nm, src in (("d_qnT", qnT), ("d_kvnT", kvnT), ("d_kpeT", kpeT), ("d_V", Vt), ("d_att", att)):
                flat = src
                if len(src.shape) == 3:
                    flat = src.rearrange("p a b -> p (a b)")
                elif len(src.shape) == 4:
                    flat = src.rearrange("p a b c -> p (a b c)")
                S.barrier()
                S.dma("sp", lambda e, nm=nm, flat=flat: e.dma_start(out=dbg_d[nm], in_=flat), "dbg", final=True)
        S.barrier()

        w_in3 = A.alloc([128, 8, 1024], BF16, {3})
        dg = A.alloc([128, 4, 31, 128], BF16, {3})
        xt3 = [A.alloc([128, D], F32, {3}), A.alloc([128, D], F32, {3})]
        xsb3 = A.alloc([128, D], BF16, {3})
        junkb3 = A.alloc([128, D], BF16, {3})
        hnT32 = [A.alloc([128, 8, 512], BF16, {3}) for _ in range(2)]
        glu = [A.alloc([128, 4, 544], BF16, {3}), A.alloc([128, 4, 544], BF16, {3})]
        sig = A.alloc([128, 512], F32, {3})
        y = A.alloc([128, 4, 512], F32, {3})
        ybf = A.alloc([128, 4, 512], BF16, {3})
        mean = A.alloc([128, 512], F32, {3})
        sd3 = A.alloc([128, 512], F32, {3})
        ssx3 = scr[:, 16:20]
        if upto >= 3:
            S.dma("pool", lambda e: e.dma_start(out=w_in3, in_=w_in_v[:, :, 800:1824]), "w0", writes=["w_in3"])
            cw = cols[:, C_CW:C_CW + 124].rearrange("p (j k) -> p j k", j=4)
            for j in range(4):
                for k in range(31):
                    eng = "dve" if (k % 2 == 0) else "pool"
                    S.op(eng, lambda e, j=j, k=k: e.tensor_scalar(out=dg[:, j, k, :], in0=ident, scalar1=cw[:, j, k:k + 1],
                                                                  scalar2=None, op0=ALU.mult),
                         reads=["ident", "cols"], writes=["dg"])
            S.op("pool", lambda e: e.memset(glu[1][:, :, 0:32], 0.0), writes=["glu1"])
            sig2 = [sig, A.alloc([128, 512], F32, {3})]

            def conv_AG(c, j):
                hn = hnT32[c % 2]
                HN3 = "hnT%d" % (c % 2)
                ba, bg = (1, 2) if j % 2 == 0 else (6, 7)
                for kc in range(8):
                    S.op("pe", lambda e, kc=kc: e.matmul(bk(ba), lhsT=w_in3[:, kc, j * 128:(j + 1) * 128], rhs=hn[:, kc, :],
                                                         start=(kc == 0), stop=(kc == 7)),
                         reads=[HN3, "w_in3"], writes=["b%d" % ba])
                for kc in range(8):
                    S.op("pe", lambda e, kc=kc: e.matmul(bk(bg), lhsT=w_in3[:, kc, 512 + j * 128:512 + (j + 1) * 128], rhs=hn[:, kc, :],
                                                         start=(kc == 0), stop=(kc == 7)),
                         reads=[HN3, "w_in3"], writes=["b%d" % bg])

            def conv_pre(c):
                gp = c % 2
                x_to_hnT(c, xt3, xsb3, junkb3, hnT32[c % 2], ssx3, "hnT%d" % (c % 2))
                if c > 0:
                    S.op("pool", lambda e: e.tensor_copy(out=glu[gp][:, :, 0:32], in_=glu[1 - gp][:, :, 512:544]),
                         reads=["glu%d" % (1 - gp)], writes=["glu%d" % gp])
                else:
                    S.op("pool", lambda e: e.memset(glu[0][:, :, 0:32], 0.0), writes=["glu0"])
                conv_AG(c, 0)

            def conv_body(c):
                gp = c % 2
                for j in range(4):
                    ba, bg = (1, 2) if j % 2 == 0 else (6, 7)
                    sg = sig2[j % 2]
                    sgn = "sig%d" % (j % 2)
                    S.op("act", lambda e, bg=bg, sg=sg: e.activation(out=sg, in_=bk(bg), func=AF.Sigmoid), reads=["b%d" % bg], writes=[sgn])
                    S.op("dve", lambda e, j=j, ba=ba, sg=sg: e.tensor_tensor(out=glu[gp][:, j, 32:544], in0=bk(ba), in1=sg, op=ALU.mult),
                         reads=["b%d" % ba, sgn], writes=["glu%d" % gp])
                    if j + 1 < 4:
                        conv_AG(c, j + 1)
                    yb = 3 + (j % 2)
                    for k in range(31):
                        S.op("pe", lambda e, j=j, k=k, yb=yb: e.matmul(bk(yb), lhsT=dg[:, j, k, :], rhs=glu[gp][:, j, 2 + k:2 + k + 512],
                                                                       start=(k == 0), stop=(k == 30)),
                             reads=["glu%d" % gp, "dg"], writes=["b%d" % yb])
                    S.op("act", lambda e, j=j, yb=yb: e.activation(out=y[:, j, :], in_=bk(yb), func=AF.Identity, bias=col(C_CB + j)),
                         reads=["b%d" % yb, "cols"], writes=["y%d" % j])
                    S.op("act", lambda e, j=j: e.copy(out=ybf[:, j, :], in_=y[:, j, :]), reads=["y%d" % j], writes=["ybf"])

            def conv_tail(c):
                for j in range(4):
                    S.op("pe", lambda e, j=j: e.matmul(bk(5), lhsT=ones, rhs=ybf[:, j, :], start=(j == 0), stop=(j == 3)),
                         reads=["ybf", "ones"], writes=["b5"])
                S.op("act", lambda e: e.activation(out=mean, in_=bk(5), func=AF.Copy, scale=1.0 / 512), reads=["b5"],
                     writes=["mean"])
                for j in range(4):
                    S.op("dve", lambda e, j=j: e.tensor_tensor(out=y[:, j, :], in0=y[:, j, :], in1=mean, op=ALU.subtract),
                         reads=["y%d" % j, "mean"], writes=["y%d" % j])
                    S.op("act", lambda e, j=j: e.activation(out=ybf[:, j, :], in_=y[:, j, :], func=AF.Square),
                         reads=["y%d" % j], writes=["ybf"])
                for j in range(4):
                    S.op("pe", lambda e, j=j: e.matmul(bk(5), lhsT=ones, rhs=ybf[:, j, :], start=(j == 0), stop=(j == 3)),
                         reads=["ybf", "ones"], writes=["b5"])
                S.op("act", lambda e: e.activation(out=sd3, in_=bk(5), func=AF.Sqrt, scale=1.0 / 512, bias=EPS),
                     reads=["b5"], writes=["sd3"])
                S.op("dve", lambda e: e.reciprocal(out=sd3, in_=sd3), reads=["sd3"], writes=["sd3"])
                for j in range(4):
                    S.op("dve", lambda e, j=j: e.tensor_tensor(out=y[:, j, :], in0=y[:, j, :], in1=sd3, op=ALU.mult),
                         reads=["y%d" % j, "sd3"], writes=["y%d" % j])
                    S.op("act", lambda e, j=j: e.activation(out=y[:, j, :], in_=y[:, j, :], func=AF.Silu,
                                                            scale=col(C_LNG + j), bias=col(C_LNB + j)),
                         reads=["y%d" % j, "cols"], writes=["y%d" % j])
                    S.op("act", lambda e, j=j: e.activation(out=ybf[:, j, :], in_=y[:, j, :], func=AF.Square),
                         reads=["y%d" % j], writes=["ybf"])
                for j in range(4):
                    S.op("pe", lambda e, j=j: e.matmul(bk(5), lhsT=ones, rhs=ybf[:, j, :], start=(j == 0), stop=(j == 3)),
                         reads=["ybf", "ones"], writes=["b5"])
                S.op("act", lambda e: e.activation(out=sd3, in_=bk(5), func=AF.Sqrt, scale=1.0 / 512, bias=EPS),
                     reads=["b5"], writes=["sd3"])
                S.op("dve", lambda e: e.reciprocal(out=sd3, in_=sd3), reads=["sd3"], writes=["sd3"])
                for j in range(4):
                    S.op("dve", lambda e, j=j, c=c: e.scalar_tensor_tensor(
                        out=cnT[:, j, c * 512:(c + 1) * 512], in0=y[:, j, :], scalar=col(C_CG + j), in1=sd3,
                        op0=ALU.mult, op1=ALU.mult), reads=["y%d" % j, "sd3", "cols"], writes=["cnT"])
            conv_pre(0)
            for c in range(NCH):
                conv_body(c)
                if c + 1 < NCH:
                    conv_pre(c + 1)
                conv_tail(c)
        if debug and upto == 3:
            S.barrier()
            S.dma("sp", lambda e: e.dma_start(out=dbg_d["d_cnT"], in_=cnT.rearrange("p a b -> p (a b)")), "dbg", final=True)
            S.dma("sp", lambda e: e.dma_start(out=dbg_d["d_att"], in_=att.rearrange("p a b -> p (a b)")), "dbg", final=True)
        S.barrier()

        if upto >= 4:
            w_out = A.alloc([128, 8, D], BF16, {4})
            wq = A.alloc([128, 8, D], BF16, {4})
            kbd = A.alloc([128, 8, 256], BF16, {4})
            gw = A.alloc([128, 8, D], BF16, {4})
            pw = A.alloc([128, 2, D], BF16, {4})
            g_ffn = A.alloc([128, D], BF16, {4})
            g_b = A.alloc([128, D], F32, {4})
            g_fin = A.alloc([128, D], F32, {4})
            NB, GRP = 9, 2
            gball = A.alloc([128, NB, 2 * D], BF16, {4})
            gb = [gball[:, b, :] for b in range(NB)]
            dgs = [A.alloc([128, 128], BF16, {4}) for _ in range(4)]
            gl = A.alloc([128, 128], F32, {4})
            h1s = [A.alloc([128, D], F32, {4}) for _ in range(2)]
            xnbs = [A.alloc([128, D], BF16, {4}) for _ in range(2)]
            tmp = A.alloc([128, D], F32, {4})
            bfa = A.alloc([128, D], BF16, {4})
            xT = A.alloc([128, 8, 128], BF16, {4})
            mp = A.alloc([128, 256], F32, {4})
            mixT = mp.bitcast(BF16).rearrange("p (a b) -> p a b", a=4)
            tk = A.alloc([128, 2048], F32, {4})
            sc = tk.rearrange("p (a b) -> p a b", a=16)
            cand = tk.rearrange("p (h c) -> p h c", h=8)
            oh = tk.rearrange("p (h a b) -> p h a b", h=8, a=16)
            wk = A.alloc([128, 256], F32, {4})
            m16 = A.alloc([128, 16, 16], F32, {4})
            ix16 = A.alloc([128, 16, 16], U32, {4})
            ixf = ix16.bitcast(F32)
            best = A.alloc([128, 8, 16], F32, {4})
            posu = A.alloc([128, 8, 16], U32, {4})
            posf = A.alloc([128, 8, 16], F32, {4})
            ki4 = posu.bitcast(I32)
            k1f = A.alloc([128, 8, 16], F32, {4})
            k2f = A.alloc([128, 8, 16], F32, {4})
            e1 = A.alloc([128, 8, 16], F32, {4})
            e2 = posf
            eidxs = [A.alloc([128, 128], I32, {4}) for _ in range(2)]
            gate16s = [A.alloc([128, 8, 16], F32, {4}) for _ in range(2)]
            actv = A.alloc([128, 128], F32, {4})
            coef = A.alloc([128, 128], F32, {4})
            pt_ = mp
            pbf = A.alloc([128, 256], BF16, {4})
            pT = A.alloc([128, 2, 128], BF16, {4})
            gsum = A.alloc([128, 8], F32, {4})
            ss4 = scr[:, 24:40]

            def wload(dst, src, ch, name):
                S.dma("pool", lambda e: e.dma_start(out=dst, in_=src.rearrange("(k p) c -> p k c", p=128)), ch, writes=[name])
            wload(w_out, w_out_d, "w0", "w_out")
            wload(wq, wq_d, "w1", "wq")
            wload(gw, gw_d, "w2", "gw")
            wload(pw, pw_d, "w3", "pw")
            S.op("dve", lambda e: e.memset(kbd, 0.0), writes=["kbd"])
            S.dma("pool", lambda e: e.dma_start(out=kbd[0:64, :, 0:128], in_=keysT_d[0:64]), "w4", writes=["kbd"])
            S.dma("pool", lambda e: e.dma_start(out=kbd[64:128, :, 128:256], in_=keysT_d[64:128]), "w5", writes=["kbd"])
            S.dma("pool", lambda e: e.dma_start(out=g_ffn, in_=rows_d[0]), "c0", writes=["g_ffn"])
            S.dma("sp", lambda e: e.dma_start(out=g_b, in_=rows_d[1]), "c1", writes=["g_b"])
            S.dma("sp", lambda e: e.dma_start(out=g_fin, in_=rows_d[2]), "c2", writes=["g_fin"])

            class Rec:
                def __init__(self):
                    self.q = []
                def op(self, *a, **k):
                    c = k.pop("cost", 0.3)
                    self.q.append((lambda: S.op(*a, **k), c if a[0] == "dve" else 0.0, a[0]))
                def dma(self, *a, **k):
                    self.q.append((lambda: S.dma(*a, **k), 0.0, "dma"))
                def brk(self):
                    self.q.append(None)

            def rms_tile(X, src, srcname, k, n_feat, junk, junkname):
                X.op("act", lambda e: e.activation(out=junk, in_=src, func=AF.Square, accum_out=ss4[:, k:k + 1]),
                     reads=[srcname], writes=[junkname, "r%d_ss" % k])
                X.op("act", lambda e: e.activation(out=ss4[:, k + 8:k + 9], in_=ss4[:, k:k + 1], func=AF.Sqrt, scale=1.0 / n_feat, bias=EPS),
                     reads=["r%d_ss" % k], writes=["r%d_r" % k])
                X.brk()
                X.op("dve", lambda e: e.reciprocal(out=ss4[:, k + 8:k + 9], in_=ss4[:, k + 8:k + 9]), reads=["r%d_r" % k], writes=["r%d_r" % k])
                return ss4[:, k + 8:k + 9], "r%d_r" % k

            Tb = bkb(0).rearrange("p (a b) -> p a b", a=8)
            Tp = bkb(3).rearrange("p (a b) -> p a b", a=8)

            def A_ops(g):
                X = Rec()
                pp = g % 2
                H, Hn = h1s[pp], "h1_%d" % pp
                xnb, xnbn = xnbs[pp], "xnb_%d" % pp
                eidx, eidxn = eidxs[pp], "eidx_%d" % pp
                gate16, gaten = gate16s[pp], "gate_%d" % pp
                ts_ = slice(g * 128, (g + 1) * 128)
                X.dma("sp", lambda e: e.dma_start(out=H, in_=x_d[ts_, :]), "x0", writes=[Hn])
                att_t = att[:, g, :]
                ra, ran = rms_tile(X, att_t, "att", 0, 512, bfa[:, 0:512], "bfa")
                X.op("dve", lambda e: e.tensor_scalar(out=bfa[:, 0:512], in0=att_t, scalar1=ra, scalar2=None, op0=ALU.mult),
                     reads=["att", ran], writes=["bfa"])
                for kc in range(4):
                    X.op("pe", lambda e, kc=kc: e.transpose(out=Tb[:, kc, :], in_=bfa[:, kc * 128:(kc + 1) * 128], identity=ident),
                         reads=["bfa", "ident"], writes=["b0"])
                X.brk()
                X.op("dve", lambda e: e.tensor_tensor(out=mixT, in0=Tb[:, 0:4, :],
                                                      in1=col(C_GAO, 4).unsqueeze(2).to_broadcast([128, 4, 128]), op=ALU.mult),
                     reads=["b0", "cols"], writes=["mixT"], cost=0.6)
                for half in range(2):
                    for kc in range(8):
                        lhs = mixT[:, kc, :] if kc < 4 else cnT[:, kc - 4, ts_]
                        X.op("pe", lambda e, half=half, kc=kc, lhs=lhs: e.matmul(bk(3 + half), lhsT=lhs,
                                                                                 rhs=w_out[:, kc, half * 512:(half + 1) * 512],
                                                                                 start=(kc == 0), stop=(kc == 7)),
                             reads=["mixT", "cnT", "w_out"], writes=["b%d" % (3 + half)])
                X.brk()
                for half in range(2):
                    X.op("dve", lambda e, half=half: e.tensor_tensor(out=H[:, half * 512:(half + 1) * 512],
                                                                     in0=H[:, half * 512:(half + 1) * 512], in1=bk(3 + half), op=ALU.add),
                         reads=[Hn, "b%d" % (3 + half)], writes=[Hn], cost=0.7)
                r1, r1n = rms_tile(X, H, Hn, 1, D, tmp, "tmp")
                X.op("dve", lambda e: e.scalar_tensor_tensor(out=xnb, in0=H, scalar=r1, in1=g_ffn, op0=ALU.mult, op1=ALU.mult),
                     reads=[Hn, r1n, "g_ffn"], writes=[xnbn], cost=1.2)
                for kc in range(8):
                    X.op("pe", lambda e, kc=kc: e.transpose(out=Tb[:, kc, :], in_=xnb[:, kc * 128:(kc + 1) * 128], identity=ident),
                         reads=[xnbn, "ident"], writes=["b0"])
                X.brk()
                X.op("act", lambda e: e.copy(out=xT, in_=Tb), reads=["b0"], writes=["xT"])
                for half in range(2):
                    for kc in range(8):
                        X.op("pe", lambda e, half=half, kc=kc: e.matmul(bk(3 + half), lhsT=xT[:, kc, :],
                                                                        rhs=wq[:, kc, half * 512:(half + 1) * 512],
                                                                        start=(kc == 0), stop=(kc == 7)),
                             reads=["xT", "wq"], writes=["b%d" % (3 + half)])
                X.brk()
                for half in range(2):
                    X.op("act", lambda e, half=half: e.copy(out=bfa[:, half * 512:(half + 1) * 512], in_=bk(3 + half)),
                         reads=["b%d" % (3 + half)], writes=["bfa"])
                for kc in range(8):
                    X.op("pe", lambda e, kc=kc: e.transpose(out=Tb[:, kc, :], in_=bfa[:, kc * 128:(kc + 1) * 128], identity=ident),
                         reads=["bfa", "ident"], writes=["b0"])
                X.brk()
                X.op("dve", lambda e: e.tensor_copy(out=xT, in_=Tb), reads=["b0"], writes=["xT"], cost=1.0)
                for hh in range(8):
                    sbk = 4 + hh // 2
                    X.op("pe", lambda e, hh=hh, sbk=sbk: e.matmul(bk(sbk)[:, (hh % 2) * 256:(hh % 2 + 1) * 256], lhsT=xT[:, hh, :],
                                                                 rhs=kbd[:, hh, :], start=True, stop=True),
                         reads=["xT", "kbd"], writes=["b%d" % sbk])
                X.brk()
                for b4 in range(4):
                    X.op("act", lambda e, b4=b4: e.copy(out=tk[:, b4 * 512:(b4 + 1) * 512], in_=bk(4 + b4)),
                         reads=["b%d" % (4 + b4)], writes=["tk"])
                X.brk()
                for hc in range(16):
                    X.op("dve", lambda e, hc=hc: e.max(out=m16[:, hc, 0:8], in_=sc[:, hc, :]), reads=["tk"], writes=["m16"])
                    X.op("dve", lambda e, hc=hc: e.max_index(out=ix16[:, hc, 0:8], in_max=m16[:, hc, 0:8], in_values=sc[:, hc, :]),
                         reads=["tk", "m16"], writes=["ix16"])
                    X.op("dve", lambda e, hc=hc: e.match_replace(out=wk[:, 0:128], in_to_replace=m16[:, hc, 0:8],
                                                                 in_values=sc[:, hc, :], imm_value=-1e30),
                         reads=["tk", "m16"], writes=["wk"])
                    X.op("dve", lambda e, hc=hc: e.max(out=m16[:, hc, 8:16], in_=wk[:, 0:128]), reads=["wk"], writes=["m16"])
                    X.op("dve", lambda e, hc=hc: e.max_index(out=ix16[:, hc, 8:16], in_max=m16[:, hc, 8:16], in_values=wk[:, 0:128]),
                         reads=["wk", "m16"], writes=["ix16"])
                m4 = m16.rearrange("p (h c) k -> p h c k", c=2)
                X.op("dve", lambda e: e.tensor_tensor(out=cand.rearrange("p h (a b) -> p h a b", a=16),
                                                      in0=m4[:, :, 0, :].unsqueeze(3).to_broadcast([128, 8, 16, 16]),
                                                      in1=m4[:, :, 1, :].unsqueeze(2).to_broadcast([128, 8, 16, 16]), op=ALU.add),
                     reads=["m16", "tk"], writes=["tk"], cost=2.2)
                for hh in range(8):
                    X.op("dve", lambda e, hh=hh: e.max(out=best[:, hh, 0:8], in_=cand[:, hh, :]), reads=["tk"], writes=["best"])
                    X.op("dve", lambda e, hh=hh: e.max_index(out=posu[:, hh, 0:8], in_max=best[:, hh, 0:8], in_values=cand[:, hh, :]),
                         reads=["tk", "best"], writes=["posu"])
                    X.op("dve", lambda e, hh=hh: e.match_replace(out=wk, in_to_replace=best[:, hh, 0:8], in_values=cand[:, hh, :],
                                                                 imm_value=-1e30), reads=["tk", "best"], writes=["wk"])
                    X.op("dve", lambda e, hh=hh: e.max(out=best[:, hh, 8:16], in_=wk), reads=["wk"], writes=["best"])
                    X.op("dve", lambda e, hh=hh: e.max_index(out=posu[:, hh, 8:16], in_max=best[:, hh, 8:16], in_values=wk),
                         reads=["wk", "best"], writes=["posu"])
                X.op("dve", lambda e: e.tensor_copy(out=posf, in_=posu), reads=["posu"], writes=["posf"])
                X.op("dve", lambda e: e.tensor_copy(out=ixf, in_=ix16), reads=["ix16"], writes=["ix16"])
                X.op("dve", lambda e: e.tensor_scalar(out=k1f, in0=posf, scalar1=-7.5, scalar2=0.0625, op0=ALU.add, op1=ALU.mult),
                     reads=["posf"], writes=["k1f"])
                X.op("dve", lambda e: e.tensor_copy(out=ki4, in_=k1f), reads=["k1f", "posf"], writes=["posu"])
                X.op("dve", lambda e: e.tensor_copy(out=k1f, in_=ki4), reads=["posu"], writes=["k1f"])
                X.op("dve", lambda e: e.scalar_tensor_tensor(out=k2f, in0=k1f, scalar=-16.0, in1=posf, op0=ALU.mult, op1=ALU.add),
                     reads=["k1f", "posf"], writes=["k2f"])
                ix4 = ixf.rearrange("p (h c) k -> p h c k", c=2)
                io_b = iota16.unsqueeze(1).unsqueeze(1).to_broadcast([128, 8, 16, 16])
                for kf, cc, eo, nm in ((k1f, 0, e1, "e1"), (k2f, 1, e2, "posf")):
                    X.op("dve", lambda e, kf=kf: e.tensor_tensor(out=oh, in0=kf.unsqueeze(3).to_broadcast([128, 8, 16, 16]),
                                                                 in1=io_b, op=ALU.is_equal),
                         reads=["k1f", "k2f", "iota16", "tk"], writes=["tk"], cost=2.2)
                    X.op("dve", lambda e, cc=cc: e.tensor_tensor(out=oh, in0=oh,
                                                                 in1=ix4[:, :, cc, :].unsqueeze(2).to_broadcast([128, 8, 16, 16]),
                                                                 op=ALU.mult), reads=["tk", "ix16"], writes=["tk"], cost=2.2)
                    X.op("dve", lambda e, eo=eo: e.reduce_sum(out=eo, in_=oh, axis=AX.X), reads=["tk"], writes=[nm], cost=2.2)
                X.op("dve", lambda e: e.scalar_tensor_tensor(out=e1, in0=e1, scalar=128.0, in1=e2, op0=ALU.mult, op1=ALU.add),
                     reads=["e1", "posf"], writes=["e1"])
                X.op("dve", lambda e: e.tensor_copy(out=eidx, in_=e1.rearrange("p h k -> p (h k)")), reads=["e1"], writes=[eidxn])
                X.op("dve", lambda e: e.tensor_tensor(out=gate16, in0=best, in1=best[:, :, 0:1].to_broadcast([128, 8, 16]),
                                                      op=ALU.subtract), reads=["best"], writes=[gaten])
                X.op("act", lambda e: e.activation(out=gate16, in_=gate16, func=AF.Exp), reads=[gaten], writes=[gaten])
                X.brk()
                X.op("dve", lambda e: e.reduce_sum(out=gsum, in_=gate16, axis=AX.X), reads=[gaten], writes=["gsum"])
                X.op("dve", lambda e: e.reciprocal(out=gsum, in_=gsum), reads=["gsum"], writes=["gsum"])
                X.op("dve", lambda e: e.tensor_tensor(out=gate16, in0=gate16, in1=gsum.unsqueeze(2).to_broadcast([128, 8, 16]),
                                                      op=ALU.mult), reads=[gaten, "gsum"], writes=[gaten])
                return X.q

            TABS = ["tabu%d" % k for k in range(8)] + ["tabv%d" % k for k in range(8)]

            def B_emit(g, side, per_group):
                pp = g % 2
                H, Hn = h1s[pp], "h1_%d" % pp
                xnb, xnbn = xnbs[pp], "xnb_%d" % pp
                eidx, eidxn = eidxs[pp], "eidx_%d" % pp
                gflat, gaten = gate16s[pp].rearrange("p h k -> p (h k)"), "gate_%d" % pp

                def consume(k):
                    gs = slice(k * GRP, (k + 1) * GRP)
                    S.op("act", lambda e: e.activation(out=gl[:, gs], in_=actv[:, gs], func=AF.Gelu), reads=["actv"], writes=["gl"])
                    S.op("dve", lambda e: e.tensor_tensor(out=coef[:, gs], in0=gl[:, gs], in1=gflat[:, gs], op=ALU.mult),
                         reads=["gl", gaten], writes=["coef"])
                    for s2 in range(k * GRP, (k + 1) * GRP):
                        b2 = s2 % NB
                        dgi = s2 % 4
                        S.op("act", lambda e, s2=s2, dgi=dgi: e.activation(out=dgs[dgi], in_=ident, func=AF.Copy, scale=coef[:, s2:s2 + 1]),
                             reads=["ident", "coef"], writes=["dg%d" % dgi])
                        for half in range(2):
                            S.op("pe", lambda e, s2=s2, b2=b2, dgi=dgi, half=half: e.matmul(
                                bk(1 + half), lhsT=dgs[dgi], rhs=gb[b2][:, D + half * 512:D + (half + 1) * 512],
                                start=(s2 == 0), stop=(s2 == 127)),
                                reads=["dg%d" % dgi, "gb%d" % b2], writes=["b%d" % (1 + half)])

                for s_ in range(128):
                    b = s_ % NB
                    S.dma("pool", lambda e, s_=s_, b=b: e.indirect_dma_start(
                        out=gb[b], out_offset=None, in_=tab_d, in_offset=bass.IndirectOffsetOnAxis(ap=eidx[:, s_:s_ + 1], axis=0)),
                        "g%d" % b, reads=[eidxn] + TABS, writes=["gb%d" % b])
                    if s_ % 2 == 0 or not DOT_SPLIT:
                        S.op("dve", lambda e, s_=s_, b=b: e.scalar_tensor_tensor(out=gb[b][:, 0:D], in0=gb[b][:, 0:D], scalar=1.0, in1=xnb,
                                                                                 op0=ALU.mult, op1=ALU.mult, accum_out=actv[:, s_:s_ + 1]),
                             reads=["gb%d" % b, xnbn], writes=["gb%d" % b, "actv"])
                    else:
                        S.op("dve", lambda e, b=b: e.tensor_tensor(out=gb[b][:, 0:D], in0=gb[b][:, 0:D], in1=xnb, op=ALU.mult),
                             reads=["gb%d" % b, xnbn], writes=["gb%d" % b])
                        S.op("act", lambda e, s_=s_, b=b: e.activation(out=gb[b][:, 0:D], in_=gb[b][:, 0:D], func=AF.Copy,
                                                                       accum_out=actv[:, s_:s_ + 1]),
                             reads=["gb%d" % b], writes=["gb%d" % b, "actv"])
                    if (s_ + 1) % GRP == 0:
                        consume(s_ // GRP)
                        dve_c, other = 0.0, 0
                        while side:
                            it = side[0]
                            if it is None:
                                side.pop(0)
                                if dve_c >= 0.4 * per_group or other >= 4:
                                    break
                                continue
                            if it[2] == "dve":
                                if dve_c > 0 and dve_c + it[1] > per_group:
                                    break
                            elif other >= 10:
                                break
                            side.pop(0)
                            it[0]()
                            if it[2] == "dve":
                                dve_c += it[1]
                            else:
                                other += 1
                while side:
                    it = side.pop(0)
                    if it is not None:
                        it[0]()
                for half in range(2):
                    S.op("dve", lambda e, half=half: e.tensor_tensor(out=H[:, half * 512:(half + 1) * 512],
                                                                     in0=H[:, half * 512:(half + 1) * 512], in1=bk(1 + half), op=ALU.add),
                         reads=[Hn, "b%d" % (1 + half)], writes=[Hn])

            def C_ops(g):
                X = Rec()
                pp = g % 2
                H, Hn = h1s[pp], "h1_%d" % pp
                ts_ = slice(g * 128, (g + 1) * 128)
                X.dma("sp", lambda e: e.dma_start(out=pt_, in_=p_d[ts_, :]), "x1", writes=["mixT"])
                r2, r2n = rms_tile(X, H, Hn, 2, D, tmp, "tmp")
                X.op("dve", lambda e: e.tensor_scalar(out=bfa, in0=H, scalar1=r2, scalar2=None, op0=ALU.mult),
                     reads=[Hn, r2n], writes=["bfa"], cost=0.8)
                X.op("act", lambda e: e.copy(out=pbf, in_=pt_), reads=["mixT"], writes=["pbf"])
                for kc in range(8):
                    X.op("pe", lambda e, kc=kc: e.transpose(out=Tb[:, kc, :], in_=bfa[:, kc * 128:(kc + 1) * 128], identity=ident),
                         reads=["bfa", "ident"], writes=["b0"])
                for kc in range(2):
                    X.op("pe", lambda e, kc=kc: e.transpose(out=Tp[:, kc, :], in_=pbf[:, kc * 128:(kc + 1) * 128], identity=ident),
                         reads=["pbf", "ident"], writes=["b3"])
                X.brk()
                X.op("dve", lambda e: e.tensor_tensor(out=xT, in0=Tb, in1=col(C_GPL, 8).unsqueeze(2).to_broadcast([128, 8, 128]),
                                                      op=ALU.mult), reads=["b0", "cols"], writes=["xT"], cost=1.0)
                X.op("act", lambda e: e.copy(out=pT, in_=Tp[:, 0:2, :]), reads=["b3"], writes=["pT"])
                for half in range(2):
                    hs = slice(half * 512, (half + 1) * 512)
                    gbk = 5 + half
                    for kc in range(8):
                        X.op("pe", lambda e, hs=hs, kc=kc, gbk=gbk: e.matmul(bk(gbk), lhsT=xT[:, kc, :], rhs=gw[:, kc, hs],
                                                                             start=(kc == 0), stop=(kc == 7)),
                             reads=["xT", "gw"], writes=["b%d" % gbk])
                X.brk()
                for half in range(2):
                    hs = slice(half * 512, (half + 1) * 512)
                    gbk = 5 + half
                    X.op("dve", lambda e, hs=hs, gbk=gbk: e.tensor_tensor(out=tmp[:, hs], in0=bk(gbk), in1=g_b[:, hs], op=ALU.add),
                         reads=["b%d" % gbk, "g_b", "tmp"], writes=["tmp"], cost=0.7)
                    X.op("act", lambda e, hs=hs: e.activation(out=tmp[:, hs], in_=tmp[:, hs], func=AF.Sigmoid), reads=["tmp"],
                         writes=["tmp"])
                    for kc in range(2):
                        X.op("pe", lambda e, hs=hs, kc=kc, gbk=gbk: e.matmul(bk(gbk), lhsT=pT[:, kc, :], rhs=pw[:, kc, hs],
                                                                             start=(kc == 0), stop=(kc == 1)),
                             reads=["pT", "pw"], writes=["b%d" % gbk])
                X.brk()
                for half in range(2):
                    hs = slice(half * 512, (half + 1) * 512)
                    gbk = 5 + half
                    X.op("dve", lambda e, hs=hs, gbk=gbk: e.tensor_tensor(out=tmp[:, hs], in0=tmp[:, hs], in1=bk(gbk), op=ALU.mult),
                         reads=["b%d" % gbk, "tmp"], writes=["tmp"], cost=0.7)
                X.op("dve", lambda e: e.tensor_tensor(out=H, in0=H, in1=tmp, op=ALU.add), reads=[Hn, "tmp"], writes=[Hn], cost=1.1)
                r3, r3n = rms_tile(X, H, Hn, 3, D, tmp, "tmp")
                X.op("dve", lambda e: e.scalar_tensor_tensor(out=tmp, in0=H, scalar=r3, in1=g_fin, op0=ALU.mult, op1=ALU.mult),
                     reads=[Hn, r3n, "g_fin", "tmp"], writes=["tmp"], cost=1.2)
                X.dma("sp", lambda e: e.dma_start(out=out_d[ts_, :], in_=tmp), "st", reads=["tmp"], final=True)
                return X.q

            for it in A_ops(0):
                if it is not None:
                    it[0]()
            for g in range(NT):
                side = []
                if g > 0:
                    side += C_ops(g - 1)
                if g + 1 < NT:
                    side += A_ops(g + 1)
                tot_c = sum(it[1] for it in side if it is not None)
                n_other = sum(1 for it in side if it is not None and it[2] != "dve")
                per_group = 1.1 * tot_c / max(8, 128 // GRP - 3 - n_other // 14)
                B_emit(g, side, per_group)
            for it in C_ops(NT - 1):
                if it is not None:
                    it[0]()
        else:
            z = A.alloc([128, D], F32, {4})
            S.op("dve", lambda e: e.memset(z, 0.0), writes=["z"])
            S.dma("sp", lambda e: e.dma_start(out=out_d[0:128, :], in_=z), "st", reads=["z"], final=True)

        stats = S.emit()
        print("program ops per engine:", stats)
    return nc


def make_in_maps(inputs):
    f = np.float32
    g = lambda k: np.asarray(inputs[k])
    x = g("x").astype(f, copy=False)
    p = g("p").astype(f, copy=False)[0]
    pos = g("positions").astype(np.int32, copy=False)

    def colmaj(v):
        v = np.asarray(v, f).reshape(-1, 128)
        return np.ascontiguousarray(v.T)

    cols = np.zeros((128, NCOL), f)
    cols[:, C_GA:C_GA + 8] = colmaj(g("attn_norm")[0])
    cols[:, C_GQ:C_GQ + 4] = colmaj(g("q_norm")[0])
    cols[:, C_GKV:C_GKV + 2] = colmaj(g("kv_norm")[0])
    cols[:, C_CB:C_CB + 4] = colmaj(g("conv_b")[0])
    cols[:, C_LNG:C_LNG + 4] = colmaj(g("conv_ln_g")[0])
    cols[:, C_LNB:C_LNB + 4] = colmaj(g("conv_ln_b")[0])
    cols[:, C_CG:C_CG + 4] = colmaj(g("conv_out_norm")[0])
    cols[:, C_GAO:C_GAO + 4] = colmaj(g("attn_out_norm")[0])
    cols[:, C_GPL:C_GPL + 8] = colmaj(g("pl_norm")[0])
    half = 16
    freqs = (np.float32(10000.0) ** (-np.arange(half, dtype=f) / np.float32(half))).astype(f)
    for pp in range(64, 96):
        cols[pp, C_FREQ] = freqs[(pp - 64) % 16]
    cw = np.asarray(g("conv_w")[0], f)
    cols[:, C_CW:C_CW + 124] = cw.reshape(31, 4, 128).transpose(2, 1, 0).reshape(128, 124)
    rows = np.stack([np.broadcast_to(np.asarray(g(k), f).reshape(-1)[None, :], (128, D))
                     for k in ("ffn_norm", "pl_gate_b", "final_norm")]).astype(f)
    rows = np.ascontiguousarray(rows)
    cst = np.zeros((3, 128, 128), f)
    cst[0] = np.eye(128, dtype=f)
    cst[1] = np.triu(np.ones((128, 128), f))
    cst[2, :, 0:16] = np.arange(16, dtype=f)[None, :]
    keysT = np.ascontiguousarray(np.asarray(g("peer_keys")[0], f).transpose(1, 3, 0, 2).reshape(128, 8, 128))
    shared = {
        "w_in": np.ascontiguousarray(g("w_in")[0], f), "w_uq": np.ascontiguousarray(g("w_uq")[0], f),
        "w_ukv": np.ascontiguousarray(g("w_ukv")[0], f), "w_out": np.ascontiguousarray(g("w_out")[0], f),
        "peer_wq": np.ascontiguousarray(g("peer_wq")[0], f), "keysT": keysT,
        "peer_u": np.ascontiguousarray(g("peer_u")[0], f), "peer_v": np.ascontiguousarray(g("peer_v")[0], f),
        "pl_gate_w": np.ascontiguousarray(g("pl_gate_w")[0], f), "pl_proj": np.ascontiguousarray(g("pl_proj")[0], f),
        "cols": cols, "rows": rows, "cst": cst,
    }
    maps = []
    for b in range(8):
        m = dict(shared)
        m["x"] = np.ascontiguousarray(x[b])
        m["p"] = np.ascontiguousarray(p[b])
        m["pos"] = np.ascontiguousarray(np.broadcast_to(pos[b][None, :], (32, T))).astype(np.int32)
        maps.append(m)
    return maps


_NC_CACHE = {}


def kernel(**inputs):
    maps = make_in_maps(inputs)
    if "nc" not in _NC_CACHE:
        _NC_CACHE["nc"] = build_program()
    nc = _NC_CACHE["nc"]
    res = run_bass_kernel_spmd(nc, maps, core_ids=list(range(8)))
    out = np.stack([np.asarray(r["out"], np.float32) for r in res.results], axis=0)
    return out.reshape(8, T, D)
```

```python
import contextlib
import numpy as np
import concourse.bass as bass
import concourse.mybir as mybir
from concourse.bass_utils import run_bass_kernel_spmd

F32 = mybir.dt.float32
BF16 = mybir.dt.bfloat16
I32 = mybir.dt.int32
U32 = mybir.dt.uint32
AF = mybir.ActivationFunctionType
ALU = mybir.AluOpType
AX = mybir.AxisListType

T = 4096
D = 1024
NT = 32
NCH = 8
EPS = 1e-6
ATT_SCALE = 96 ** -0.5
ENGS = ["pe", "act", "dve", "pool", "sp"]
SAME_ENGINE_RAW = True
SAME_ENGINE_WAR = False
DOT_SPLIT = False

C_GA = 0
C_GQ = 8
C_GKV = 12
C_CB = 14
C_LNG = 18
C_LNB = 22
C_CG = 26
C_GAO = 30
C_GPL = 34
C_FREQ = 42
C_CW = 44
NCOL = C_CW + 4 * 31


class Sched:
    def __init__(self, nc):
        self.nc = nc
        self.ops = {e: [] for e in ENGS}
        self.res = {}
        self.seen = {e: {} for e in ENGS}
        self.chan_n = {}
        self.pending = {e: [] for e in ENGS}
        self.final_waits = []

    def _need(self, eng, prod, waits):
        if prod is None:
            return
        kind, key, n = prod
        if kind == "e" and key == eng:
            if key == "pe" or not SAME_ENGINE_RAW:
                return
        k = (kind, key)
        if self.seen[eng].get(k, -1) >= n:
            return
        self.seen[eng][k] = n
        waits.append(prod)

    def _deps(self, eng, reads, writes, waits):
        for r in reads:
            st = self.res.setdefault(r, {"w": None, "r": {}})
            self._need(eng, st["w"], waits)
        for w in writes:
            st = self.res.setdefault(w, {"w": None, "r": {}})
            p = st["w"]
            if p is not None and (SAME_ENGINE_WAR or not (p[0] == "e" and p[1] == eng)):
                self._need(eng, p, waits)
            for k, rp in st["r"].items():
                if rp[0] == "e" and rp[1] == eng and not SAME_ENGINE_WAR:
                    continue
                self._need(eng, rp, waits)

    def _commit(self, tok, reads, writes):
        for r in reads:
            self.res[r]["r"][(tok[0], tok[1])] = tok
        for w in writes:
            st = self.res[w]
            st["w"] = tok
            st["r"] = {}

    def _take_pending(self, eng):
        waits = list(self.pending[eng])
        self.pending[eng] = []
        for p in waits:
            k = (p[0], p[1])
            self.seen[eng][k] = max(self.seen[eng].get(k, -1), p[2])
        return waits

    def op(self, eng, fn, reads=(), writes=()):
        waits = self._take_pending(eng)
        self._deps(eng, reads, writes, waits)
        idx = len(self.ops[eng])
        self.ops[eng].append({"fn": fn, "waits": waits, "sig": False, "dma": None})
        self._commit(("e", eng, idx), reads, writes)

    def dma(self, eng, fn, ch, reads=(), writes=(), final=False):
        ch = "%s_%s" % (ch, eng)
        waits = self._take_pending(eng)
        n = self.chan_n.get(ch, 0)
        if n > 0:
            self._need(eng, ("d", ch, n), waits)
        self._deps(eng, reads, writes, waits)
        n += 1
        self.chan_n[ch] = n
        self.ops[eng].append({"fn": fn, "waits": waits, "sig": False, "dma": ch})
        self._commit(("d", ch, n), reads, writes)
        if final and ch not in self.final_waits:
            self.final_waits.append(ch)

    def barrier(self):
        prods = []
        for e in ENGS:
            for i in range(len(self.ops[e]) - 1, -1, -1):
                if self.ops[e][i]["dma"] is None:
                    prods.append(("e", e, i))
                    break
        for ch, n in self.chan_n.items():
            if not ch.startswith("tb"):
                prods.append(("d", ch, n))
        for e in ENGS:
            self.pending[e] = [p for p in prods if not (p[0] == "e" and p[1] == e)]
        self.res = {k: v for k, v in self.res.items() if k.startswith("tab")}

    def emit(self):
        nc = self.nc
        for e in ENGS:
            for o in self.ops[e]:
                for (kind, key, n) in o["waits"]:
                    if kind == "e":
                        self.ops[key][n]["sig"] = True
        cnt = {}
        for e in ENGS:
            c = 0
            for o in self.ops[e]:
                if o["sig"]:
                    c += 1
                o["cnt"] = c
            cnt[e] = c
        with contextlib.ExitStack() as st:
            esem = {e: st.enter_context(nc.semaphore("s_" + e)) for e in ENGS if cnt[e] > 0}
            csem = {ch: st.enter_context(nc.semaphore("c_%s" % (ch,))) for ch in self.chan_n}
            block = st.enter_context(nc.Block())
            handles = {"pe": block.tensor, "act": block.scalar, "dve": block.vector,
                       "pool": block.gpsimd, "sp": block.sync}

            def make(e):
                def body(eng):
                    for o in self.ops[e]:
                        for (kind, key, n) in o["waits"]:
                            if kind == "e":
                                eng.wait_ge(esem[key], self.ops[key][n]["cnt"])
                            else:
                                eng.wait_ge(csem[key], 16 * n)
                        ins = o["fn"](eng)
                        if o["dma"] is not None:
                            ins.then_inc(csem[o["dma"]], 16)
                        elif o["sig"]:
                            ins.then_inc(esem[e], 1)
                    if e == "sp":
                        for ch in self.final_waits:
                            eng.wait_ge(csem[ch], 16 * self.chan_n[ch])
                return body

            for e in ENGS:
                if self.ops[e] or e == "sp":
                    handles[e](make(e))
        return {e: len(self.ops[e]) for e in ENGS}, cnt


class Arena:
    def __init__(self, base_ap, nbytes):
        self.base = base_ap
        self.nbytes = nbytes
        self.allocs = []

    def alloc(self, shape, dt, phases):
        esz = {F32: 4, BF16: 2, I32: 4, U32: 4}[dt]
        n = 1
        for s in shape[1:]:
            n *= s
        size = (n * esz + 31) // 32 * 32
        phases = set(phases)
        cands = sorted({0} | {o + s for (o, s, p) in self.allocs})
        for off in cands:
            ok = off + size <= self.nbytes
            if ok:
                for (o, s, p) in self.allocs:
                    if p & phases and off < o + s and o < off + size:
                        ok = False
                        break
            if ok:
                self.allocs.append((off, size, phases))
                v = self.base[:, off // 4:(off + size) // 4]
                if dt != F32:
                    v = v.bitcast(dt)
                v = v[:, 0:n]
                if len(shape) == 3:
                    v = v.rearrange("p (a b) -> p a b", a=shape[1])
                elif len(shape) == 4:
                    v = v.rearrange("p (a b c) -> p a b c", a=shape[1], b=shape[2])
                return v
        raise RuntimeError("arena full: %s %s %s" % (shape, dt, phases))


def build_program(upto=4, debug=False):
    nc = bass.Bass("TRN2", target_bir_lowering=False)

    def din(name, shape, dt=F32):
        return nc.dram_tensor(name, shape, dt, kind="ExternalInput").ap()

    x_d = din("x", [T, D])
    p_d = din("p", [T, 256])
    pos_d = din("pos", [32, T], I32)
    w_in_d = din("w_in", [D, 1824])
    w_uq_d = din("w_uq", [512, 768])
    w_ukv_d = din("w_ukv", [256, 1024])
    w_out_d = din("w_out", [D, D])
    wq_d = din("peer_wq", [D, D])
    keysT_d = din("keysT", [128, 8, 128])
    u_d = din("peer_u", [16384, D])
    v_d = din("peer_v", [16384, D])
    gw_d = din("pl_gate_w", [D, D])
    pw_d = din("pl_proj", [256, D])
    cols_d = din("cols", [128, NCOL])
    rows_d = din("rows", [3, 128, D])
    cst_d = din("cst", [3, 128, 128])
    out_d = nc.dram_tensor("out", [T, D], F32, kind="ExternalOutput").ap()
    tab_d = nc.dram_tensor("peer_tab", [16384, 2 * D], BF16, kind="Internal").ap()
    dbg_d = None
    if debug:
        dbg_d = {
            "d_qnT": nc.dram_tensor("d_qnT", [128, 4 * T], BF16, kind="ExternalOutput").ap(),
            "d_kvnT": nc.dram_tensor("d_kvnT", [128, 2 * T], BF16, kind="ExternalOutput").ap(),
            "d_kpeT": nc.dram_tensor("d_kpeT", [128, T], BF16, kind="ExternalOutput").ap(),
            "d_V": nc.dram_tensor("d_V", [128, NT * 8 * 65], BF16, kind="ExternalOutput").ap(),
            "d_att": nc.dram_tensor("d_att", [128, NT * 512], BF16, kind="ExternalOutput").ap(),
            "d_cnT": nc.dram_tensor("d_cnT", [128, 4 * T], BF16, kind="ExternalOutput").ap(),
        }

    ARENA_BYTES = 212800
    with contextlib.ExitStack() as st:
        arena_t = st.enter_context(nc.sbuf_tensor("arena", [128, ARENA_BYTES // 4], F32))
        banks = [st.enter_context(nc.psum_tensor("bank%d" % i, [128, 512], F32)) for i in range(8)]
        A = Arena(arena_t[:, :], ARENA_BYTES)
        S = Sched(nc)
        ALLP = {1, 2, 3, 4}

        def bk(i):
            return banks[i][:, :]

        def bkb(i):
            return banks[i][:, :].bitcast(BF16)

        cols = A.alloc([128, NCOL], F32, ALLP)
        ident = A.alloc([128, 128], BF16, ALLP)
        tri = A.alloc([128, 128], BF16, {1, 2})
        ones = A.alloc([128, 128], BF16, {1, 2, 3})
        iota16 = A.alloc([128, 16], F32, ALLP)
        scr = A.alloc([128, 64], F32, ALLP)

        S.dma("sp", lambda e: e.dma_start(out=cols, in_=cols_d), "c0", writes=["cols"])
        S.dma("pool", lambda e: e.dma_start(out=ident, in_=cst_d[0]), "c1", writes=["ident"])
        S.dma("pool", lambda e: e.dma_start(out=tri, in_=cst_d[1]), "c2", writes=["tri"])
        S.dma("sp", lambda e: e.dma_start(out=iota16, in_=cst_d[2][:, 0:16]), "c3", writes=["iota16"])
        S.op("dve", lambda e: e.memset(ones, 1.0), writes=["ones"])

        def col(c0, n=1):
            return cols[:, c0:c0 + n]

        att = A.alloc([128, NT, 512], BF16, {2, 3, 4})
        cnT = A.alloc([128, 4, T], BF16, {3, 4})
        qnT = A.alloc([128, 4, T], BF16, {1, 2})
        kvnT = A.alloc([128, 2, T], BF16, {1, 2})
        kpeT = A.alloc([128, T], BF16, {1, 2})
        Vt = A.alloc([128, NT, 8, 65], BF16, {1, 2})
        cosT = A.alloc([128, T], BF16, {1, 2})
        sinT = A.alloc([128, T], BF16, {1, 2})
        w_uq = A.alloc([128, 4, 768], BF16, {1, 2})
        w_uqr = A.alloc([128, 4, 8, 96], BF16, {1, 2})
        w_ukv = A.alloc([128, 2, 1024], BF16, {1, 2})

        def rstd_from_ss(ss_ap, n_feat, out_ap, tag):
            S.op("act", lambda e: e.activation(out=out_ap, in_=ss_ap, func=AF.Sqrt, scale=1.0 / n_feat, bias=EPS),
                 reads=[tag + "_ss"], writes=[tag + "_r"])
            S.op("dve", lambda e: e.reciprocal(out=out_ap, in_=out_ap), reads=[tag + "_r"], writes=[tag + "_r"])

        def x_to_hnT(c, xt, xsb, junkb, hnT, ssx, hname="hnT"):
            for t4 in range(4):
                g = 4 * c + t4
                par = g % 2
                S.dma("sp", lambda e, g=g, par=par: e.dma_start(out=xt[par], in_=x_d[g * 128:(g + 1) * 128, :]),
                      "x%d" % par, writes=["xt%d" % par])
                S.op("act", lambda e, par=par: e.activation(out=junkb, in_=xt[par], func=AF.Square,
                                                            accum_out=ssx[:, par:par + 1]),
                     reads=["xt%d" % par], writes=["junkb", "x%d_ss" % par])
                rstd_from_ss(ssx[:, par:par + 1], D, ssx[:, 2 + par:3 + par], "x%d" % par)
                S.op("dve", lambda e, par=par: e.tensor_scalar(out=xsb, in0=xt[par], scalar1=ssx[:, 2 + par:3 + par],
                                                               scalar2=None, op0=ALU.mult),
                     reads=["xt%d" % par, "x%d_r" % par], writes=["xsb"])
                Tb = bkb(0).rearrange("p (a b) -> p a b", a=8)
                for kc in range(8):
                    S.op("pe", lambda e, kc=kc: e.transpose(out=Tb[:, kc, :], in_=xsb[:, kc * 128:(kc + 1) * 128],
                                                            identity=ident),
                         reads=["xsb", "ident"], writes=["b0"])
                S.op("dve", lambda e, t4=t4: e.tensor_tensor(
                    out=hnT[:, :, t4 * 128:(t4 + 1) * 128], in0=Tb,
                    in1=col(C_GA, 8).unsqueeze(2).to_broadcast([128, 8, 128]), op=ALU.mult),
                    reads=["b0", "cols"], writes=[hname])

        def norm_fm(zbanks, n_oc, n_feat, gcol, dst, c, sq, sd, tag):
            for oc in range(n_oc):
                S.op("act", lambda e, oc=oc: e.activation(out=sq[:, oc, :], in_=bk(zbanks[oc]), func=AF.Square),
                     reads=["b%d" % zbanks[oc]], writes=["sq"])
            for oc in range(n_oc):
                S.op("pe", lambda e, oc=oc: e.matmul(bk(5), lhsT=ones, rhs=sq[:, oc, :], start=(oc == 0),
                                                     stop=(oc == n_oc - 1)),
                     reads=["sq", "ones"], writes=["b5"])
            S.op("act", lambda e: e.activation(out=sd, in_=bk(5), func=AF.Sqrt, scale=1.0 / n_feat, bias=EPS),
                 reads=["b5"], writes=["sd"])
            S.op("dve", lambda e: e.reciprocal(out=sd, in_=sd), reads=["sd"], writes=["sd"])
            for oc in range(n_oc):
                S.op("dve", lambda e, oc=oc: e.scalar_tensor_tensor(
                    out=dst[:, oc, c * 512:(c + 1) * 512], in0=bk(zbanks[oc]), scalar=col(gcol + oc), in1=sd,
                    op0=ALU.mult, op1=ALU.mult),
                    reads=["b%d" % zbanks[oc], "sd", "cols"], writes=[tag])

        w_in1 = A.alloc([128, 8, 800], BF16, {1})
        wkpe = A.alloc([128, 8, 96], BF16, {1})
        wkper = A.alloc([128, 8, 96], BF16, {1})
        xt = [A.alloc([128, D], F32, {1}), A.alloc([128, D], F32, {1})]
        xsb = A.alloc([128, D], BF16, {1})
        junkb = A.alloc([128, D], BF16, {1})
        hnT2 = [A.alloc([128, 8, 512], BF16, {1}) for _ in range(2)]
        sq = A.alloc([128, 4, 512], BF16, {1})
        sd = A.alloc([128, 512], F32, {1})
        tmpa = A.alloc([128, 512], F32, {1})
        tmpb = A.alloc([128, 512], F32, {1})
        ssx = scr[:, 0:4]

        w_in_v = w_in_d.rearrange("(k p) c -> p k c", p=128)
        S.dma("pool", lambda e: e.dma_start(out=w_in1, in_=w_in_v[:, :, 0:800]), "w0", writes=["w_in1"])
        S.op("dve", lambda e: e.memset(wkpe, 0.0), writes=["wkpe"])
        S.op("dve", lambda e: e.memset(wkper, 0.0), writes=["wkper"])
        S.dma("pool", lambda e: e.dma_start(out=wkpe[:, :, 64:96], in_=w_in_v[:, :, 768:800]), "w1", writes=["wkpe"])
        S.dma("pool", lambda e: e.dma_start(out=wkper[:, :, 64:80], in_=w_in_v[:, :, 784:800]), "w2", writes=["wkper"])
        S.dma("pool", lambda e: e.dma_start(out=wkper[:, :, 80:96], in_=w_in_v[:, :, 768:784]), "w3", writes=["wkper"])
        S.op("dve", lambda e: e.tensor_scalar(out=wkper[:, :, 64:80], in0=wkper[:, :, 64:80], scalar1=-1.0,
                                              scalar2=None, op0=ALU.mult), reads=["wkper"], writes=["wkper"])
        S.dma("pool", lambda e: e.dma_start(out=w_uq, in_=w_uq_d.rearrange("(k p) c -> p k c", p=128)), "w4",
              writes=["w_uq"])
        S.dma("pool", lambda e: e.dma_start(out=w_ukv, in_=w_ukv_d.rearrange("(k p) c -> p k c", p=128)), "w5",
              writes=["w_ukv"])
        S.op("dve", lambda e: e.memset(w_uqr, 0.0), writes=["w_uqr"])
        w_uq_h = w_uq.rearrange("p k (h c) -> p k h c", h=8)
        S.op("dve", lambda e: e.tensor_scalar(out=w_uqr[:, :, :, 64:80], in0=w_uq_h[:, :, :, 80:96], scalar1=-1.0,
                                              scalar2=None, op0=ALU.mult), reads=["w_uq", "w_uqr"], writes=["w_uqr"])
        S.op("dve", lambda e: e.tensor_copy(out=w_uqr[:, :, :, 80:96], in_=w_uq_h[:, :, :, 64:80]),
             reads=["w_uq", "w_uqr"], writes=["w_uqr"])
        for k in range(8):
            rs = slice(k * 2048, (k + 1) * 2048)
            S.dma("pool", lambda e, rs=rs: e.dma_start(out=tab_d[rs, 0:D], in_=u_d[rs, :]), "tbu%d" % k, writes=["tabu%d" % k])
            S.dma("pool", lambda e, rs=rs: e.dma_start(out=tab_d[rs, D:2 * D], in_=v_d[rs, :]), "tbv%d" % k, writes=["tabv%d" % k])
        S.op("dve", lambda e: e.memset(Vt[:, :, :, 64:65], 1.0), writes=["Vones"])

        posi = xt[0].bitcast(I32)
        ya = xt[1]
        ki = tmpa.bitcast(I32)
        R = slice(64, 96)
        for blk in range(8):
            cs = slice(blk * 512, (blk + 1) * 512)
            S.dma("sp", lambda e, cs=cs: e.dma_start(out=posi[R, 0:512], in_=pos_d[:, cs]), "x0", writes=["posi"])
            S.op("dve", lambda e: e.tensor_copy(out=ya[R, 0:512], in_=posi[R, 0:512]), reads=["posi"], writes=["ya"])
            S.op("dve", lambda e: e.tensor_scalar(out=ya[R, 0:512], in0=ya[R, 0:512], scalar1=cols[R, C_FREQ:C_FREQ + 1],
                                                  scalar2=1.0 / (2 * np.pi), op0=ALU.mult, op1=ALU.mult),
                 reads=["ya", "cols"], writes=["ya"])
            for which, tab, shift in (("s", sinT, 0.0), ("c", cosT, 0.25)):
                if shift:
                    S.op("dve", lambda e: e.tensor_scalar(out=ya[R, 512:1024], in0=ya[R, 0:512], scalar1=0.25,
                                                          scalar2=None, op0=ALU.add), reads=["ya"], writes=["yb"])
                    src = ya[R, 512:1024]
                    rname = "yb"
                else:
                    src = ya[R, 0:512]
                    rname = "ya"
                S.op("dve", lambda e, src=src: e.tensor_copy(out=ki[R, :], in_=src), reads=[rname], writes=["ki"])
                S.op("dve", lambda e: e.tensor_copy(out=tmpb[R, :], in_=ki[R, :]), reads=["ki"], writes=["tmpb"])
                S.op("dve", lambda e, src=src: e.tensor_tensor(out=tmpb[R, :], in0=src, in1=tmpb[R, :], op=ALU.subtract),
                     reads=[rname, "tmpb"], writes=["tmpb"])
                S.op("act", lambda e, tab=tab, cs=cs: e.activation(out=tab[R, cs], in_=tmpb[R, :], func=AF.Sin,
                                                                   scale=6.28318),
                     reads=["tmpb"], writes=["rope" + which])

        S.barrier()
        if upto >= 1:
            for c in range(NCH):
                hnT = hnT2[c % 2]
                HN = "hnT%d" % (c % 2)
                x_to_hnT(c, xt, xsb, junkb, hnT, ssx, HN)
                for oc in range(4):
                    for kc in range(8):
                        S.op("pe", lambda e, oc=oc, kc=kc, hnT=hnT: e.matmul(bk(1 + oc), lhsT=w_in1[:, kc, oc * 128:(oc + 1) * 128],
                                                                    rhs=hnT[:, kc, :], start=(kc == 0), stop=(kc == 7)),
                             reads=[HN, "w_in1"], writes=["b%d" % (1 + oc)])
                norm_fm([1, 2, 3, 4], 4, 512, C_GQ, qnT, c, sq, sd, "qnT")
                for oc in range(2):
                    for kc in range(8):
                        S.op("pe", lambda e, oc=oc, kc=kc, hnT=hnT: e.matmul(bk(1 + oc), lhsT=w_in1[:, kc, 512 + oc * 128:512 + (oc + 1) * 128],
                                                                    rhs=hnT[:, kc, :], start=(kc == 0), stop=(kc == 7)),
                             reads=[HN, "w_in1"], writes=["b%d" % (1 + oc)])
                norm_fm([1, 2], 2, 256, C_GKV, kvnT, c, sq, sd, "kvnT")
                for kc in range(8):
                    S.op("pe", lambda e, kc=kc, hnT=hnT: e.matmul(bk(6)[0:96, :], lhsT=wkpe[:, kc, :], rhs=hnT[:, kc, :],
                                                         start=(kc == 0), stop=(kc == 7)),
                         reads=[HN, "wkpe"], writes=["b6"])
                for kc in range(8):
                    S.op("pe", lambda e, kc=kc, hnT=hnT: e.matmul(bk(7)[0:96, :], lhsT=wkper[:, kc, :], rhs=hnT[:, kc, :],
                                                         start=(kc == 0), stop=(kc == 7)),
                         reads=[HN, "wkper"], writes=["b7"])
                cs = slice(c * 512, (c + 1) * 512)
                S.op("dve", lambda e, cs=cs: e.tensor_tensor(out=tmpa[R, :], in0=bk(6)[R, :], in1=cosT[R, cs], op=ALU.mult),
                     reads=["b6", "ropec"], writes=["tmpa"])
                S.op("dve", lambda e, cs=cs: e.tensor_tensor(out=tmpb[R, :], in0=bk(7)[R, :], in1=sinT[R, cs], op=ALU.mult),
                     reads=["b7", "ropes"], writes=["tmpb"])
                S.op("dve", lambda e, cs=cs: e.tensor_tensor(out=kpeT[R, cs], in0=tmpa[R, :], in1=tmpb[R, :], op=ALU.add),
                     reads=["tmpa", "tmpb"], writes=["kpeT"])
                w_ukv_h = w_ukv.rearrange("p k (h c) -> p k h c", h=8)
                for t4 in range(4):
                    g = 4 * c + t4
                    vb = 6 + (t4 % 2)
                    for kc in range(2):
                        S.op("pe", lambda e, kc=kc, g=g, vb=vb: e.matmul(
                            bk(vb).rearrange("p (h c) -> p h c", h=8), lhsT=kvnT[:, kc, g * 128:(g + 1) * 128],
                            rhs=w_ukv_h[:, kc, :, 64:128], start=(kc == 0), stop=(kc == 1)),
                            reads=["kvnT", "w_ukv"], writes=["b%d" % vb])
                    S.op("act", lambda e, g=g, vb=vb: e.copy(out=Vt[:, g, :, 0:64],
                                                            in_=bk(vb).rearrange("p (h c) -> p h c", h=8)),
                         reads=["b%d" % vb], writes=["V"])
        S.barrier()

        QT = [A.alloc([128, T], BF16, {2}), A.alloc([128, T], BF16, {2})]
        KT = [A.alloc([128, T], BF16, {2}), A.alloc([128, T], BF16, {2})]
        NPT = 5
        PT = [A.alloc([128, 512], BF16, {2}) for _ in range(NPT)]
        sqq = A.alloc([128, 512], BF16, {2})
        t2a = A.alloc([128, 512], F32, {2})
        t2b = A.alloc([128, 512], F32, {2})
        mxq = A.alloc([128, 16], F32, {2})
        negm = A.alloc([128, 8], F32, {2})
        rec = A.alloc([128, 8], F32, {2})
        w_ukv_h = w_ukv.rearrange("p k (h c) -> p k h c", h=8)

        class Defer:
            def __init__(self):
                self.q = []
            def op(self, *a, **k):
                self.q.append(lambda: S.op(*a, **k))

        def build_qk(h, X=None):
            X = X or S
            hp = h % 2
            qt, kt = QT[hp], KT[hp]
            X.op("pool", lambda e: e.tensor_copy(out=kt[R, :], in_=kpeT[R, :]), reads=["kpeT"], writes=["KT%d" % hp])
            for c in range(NCH):
                cs = slice(c * 512, (c + 1) * 512)
                for kc in range(4):
                    X.op("pe", lambda e, kc=kc, cs=cs: e.matmul(bk(0)[0:96, :], lhsT=w_uq[:, kc, h * 96:(h + 1) * 96],
                                                                rhs=qnT[:, kc, cs], start=(kc == 0), stop=(kc == 3)),
                         reads=["qnT", "w_uq"], writes=["b0"])
                for kc in range(4):
                    X.op("pe", lambda e, kc=kc, cs=cs: e.matmul(bk(1)[0:96, :], lhsT=w_uqr[:, kc, h, :],
                                                                rhs=qnT[:, kc, cs], start=(kc == 0), stop=(kc == 3)),
                         reads=["qnT", "w_uqr"], writes=["b1"])
                for kc in range(2):
                    X.op("pe", lambda e, kc=kc, cs=cs: e.matmul(bk(2)[0:64, :], lhsT=w_ukv_h[:, kc, h, 0:64],
                                                                rhs=kvnT[:, kc, cs], start=(kc == 0), stop=(kc == 1)),
                         reads=["kvnT", "w_ukv"], writes=["b2"])
                X.op("act", lambda e, cs=cs: e.copy(out=qt[0:64, cs], in_=bk(0)[0:64, :]), reads=["b0"],
                     writes=["QT%d" % hp])
                X.op("dve", lambda e, cs=cs: e.tensor_tensor(out=t2a[R, :], in0=bk(0)[R, :], in1=cosT[R, cs], op=ALU.mult),
                     reads=["b0", "ropec"], writes=["t2a"])
                X.op("dve", lambda e, cs=cs: e.tensor_tensor(out=t2b[R, :], in0=bk(1)[R, :], in1=sinT[R, cs], op=ALU.mult),
                     reads=["b1", "ropes"], writes=["t2b"])
                X.op("dve", lambda e, cs=cs: e.tensor_tensor(out=qt[R, cs], in0=t2a[R, :], in1=t2b[R, :], op=ALU.add),
                     reads=["t2a", "t2b"], writes=["QT%d" % hp])
                X.op("act", lambda e, cs=cs: e.copy(out=kt[0:64, cs], in_=bk(2)[0:64, :]), reads=["b2"],
                     writes=["KT%d" % hp])
                for which, src, col0 in (("q", qt, 0), ("k", kt, 8)):
                    X.op("act", lambda e, src=src, cs=cs: e.activation(out=sqq[0:96, :], in_=src[0:96, cs], func=AF.Square),
                         reads=[("QT%d" if which == "q" else "KT%d") % hp], writes=["sqq"])
                    X.op("pe", lambda e: e.matmul(bk(2), lhsT=ones[0:96, :], rhs=sqq[0:96, :], start=True, stop=True),
                         reads=["sqq", "ones"], writes=["b2"])
                    X.op("dve", lambda e, col0=col0, c=c: e.reduce_max(out=mxq[:, col0 + c:col0 + c + 1], in_=bk(2), axis=AX.X),
                         reads=["b2"], writes=["mxq"])
            X.op("dve", lambda e: e.reduce_max(out=scr[:, 8:9], in_=mxq[:, 0:8], axis=AX.X), reads=["mxq"], writes=["scr8"])
            X.op("dve", lambda e: e.reduce_max(out=scr[:, 9:10], in_=mxq[:, 8:16], axis=AX.X), reads=["mxq"], writes=["scr9"])
            X.op("dve", lambda e: e.tensor_tensor(out=scr[:, 10:11], in0=scr[:, 8:9], in1=scr[:, 9:10], op=ALU.mult),
                 reads=["scr8", "scr9"], writes=["scr10"])
            X.op("act", lambda e: e.activation(out=scr[:, 11:12], in_=scr[:, 10:11], func=AF.Sqrt), reads=["scr10"],
                 writes=["scr11"])
            X.op("dve", lambda e: e.tensor_scalar(out=negm[:, h:h + 1], in0=scr[:, 11:12], scalar1=-ATT_SCALE, scalar2=None,
                                                  op0=ALU.mult), reads=["scr11"], writes=["negm%d" % h])

        def attn(h, side=None):
            side = side or []
            hp = h % 2
            qt, kt = QT[hp], KT[hp]
            steps = []
            for j in range(NCH):
                for i in range(4 * j + 4):
                    steps.append((j, i))

            def emit_S(n):
                j, i = steps[n]
                q0 = max(512 * j, 128 * i)
                w = 512 * j + 512 - q0
                sb_ = 3 + (n % 3)
                pt = PT[n % NPT]
                ptn = "PT%d" % (n % NPT)
                S.op("pe", lambda e: e.matmul(bk(sb_)[:, 0:w], lhsT=kt[0:96, i * 128:(i + 1) * 128], rhs=qt[0:96, q0:q0 + w],
                                              start=True, stop=True),
                     reads=["QT%d" % hp, "KT%d" % hp], writes=["b%d" % sb_])
                S.op("act", lambda e: e.activation(out=pt[:, 0:w], in_=bk(sb_)[:, 0:w], func=AF.Exp, scale=ATT_SCALE,
                                                   bias=negm[:, h:h + 1]),
                     reads=["b%d" % sb_, "negm%d" % h], writes=[ptn])
                if 128 * i >= 512 * j:
                    S.op("pool", lambda e: e.tensor_tensor(out=pt[:, 0:128], in0=pt[:, 0:128], in1=tri, op=ALU.mult),
                         reads=[ptn, "tri"], writes=[ptn])

            def emit_PV(n):
                j, i = steps[n]
                q0 = max(512 * j, 128 * i)
                r0 = (q0 - 512 * j) // 128
                pt = PT[n % NPT]
                ptn = "PT%d" % (n % NPT)
                ob = 6 + (j % 2)
                O = bk(ob)[:, 0:260].rearrange("p (r c) -> p r c", r=4)
                for rr in range(r0, 4):
                    S.op("pe", lambda e, rr=rr: e.matmul(O[:, rr, :], lhsT=pt[:, (rr - r0) * 128:(rr - r0 + 1) * 128], rhs=Vt[:, i, h, :],
                                                         start=(i == 0 and rr == 0), stop=(i == 4 * j + 3 and rr == 3)),
                         reads=[ptn, "V", "Vones"], writes=["b%d" % ob])
                if i == 4 * j + 3:
                    S.op("dve", lambda e: e.reciprocal(out=rec[:, 0:4], in_=O[:, :, 64]), reads=["b%d" % ob], writes=["rec"])
                    S.op("dve", lambda e: e.tensor_tensor(out=att[:, 4 * j:4 * j + 4, h * 64:(h + 1) * 64], in0=O[:, :, 0:64],
                                                          in1=rec[:, 0:4].unsqueeze(2).to_broadcast([128, 4, 64]), op=ALU.mult),
                         reads=["b%d" % ob, "rec"], writes=["att"])

            emit_S(0)
            emit_S(1)
            per_step = (len(side) + 99) // 100
            for n in range(len(steps)):
                if n + 2 < len(steps):
                    emit_S(n + 2)
                emit_PV(n)
                for _ in range(per_step):
                    if side:
                        side.pop(0)()
            while side:
                side.pop(0)()

        if upto >= 2:
            build_qk(0)
            for h in range(8):
                side = []
                if h + 1 < 8:
                    dq = Defer()
                    build_qk(h + 1, dq)
                    side = dq.q
                attn(h, side)
        if debug and upto <= 2:
            for nm, src in (("d_qnT", qnT), ("d_kvnT", kvnT), ("d_kpeT", kpeT), ("d_V", Vt), ("d_att", att)):
                flat = src
                if len(src.shape) == 3:
                    flat = src.rearrange("p a b -> p (a b)")
                elif len(src.shape) == 4:
                    flat = src.rearrange("p a b c -> p (a b c)")
                S.barrier()
                S.dma("sp", lambda e, nm=nm, flat=flat: e.dma_start(out=dbg_d[nm], in_=flat), "dbg", final=True)
        S.barrier()

        w_in3 = A.alloc([128, 8, 1024], BF16, {3})
        dg = A.alloc([128, 4, 31, 128], BF16, {3})
        xt3 = [A.alloc([128, D], F32, {3}), A.alloc([128, D], F32, {3})]
        xsb3 = A.alloc([128, D], BF16, {3})
        junkb3 = A.alloc([128, D], BF16, {3})
        hnT32 = [A.alloc([128, 8, 512], BF16, {3}) for _ in range(2)]
        glu = [A.alloc([128, 4, 544], BF16, {3}), A.alloc([128, 4, 544], BF16, {3})]
        sig = A.alloc([128, 512], F32, {3})
        y = A.alloc([128, 4, 512], F32, {3})
        ybf = A.alloc([128, 4, 512], BF16, {3})
        mean = A.alloc([128, 512], F32, {3})
        sd3 = A.alloc([128, 512], F32, {3})
        ssx3 = scr[:, 16:20]
        if upto >= 3:
            S.dma("pool", lambda e: e.dma_start(out=w_in3, in_=w_in_v[:, :, 800:1824]), "w0", writes=["w_in3"])
            cw = cols[:, C_CW:C_CW + 124].rearrange("p (j k) -> p j k", j=4)
            for j in range(4):
                for k in range(31):
                    eng = "dve" if (k % 2 == 0) else "pool"
                    S.op(eng, lambda e, j=j, k=k: e.tensor_scalar(out=dg[:, j, k, :], in0=ident, scalar1=cw[:, j, k:k + 1],
                                                                  scalar2=None, op0=ALU.mult),
                         reads=["ident", "cols"], writes=["dg"])
            S.op("pool", lambda e: e.memset(glu[1][:, :, 0:32], 0.0), writes=["glu1"])
            sig2 = [sig, A.alloc([128, 512], F32, {3})]

            def conv_AG(c, j):
                hn = hnT32[c % 2]
                HN3 = "hnT%d" % (c % 2)
                ba, bg = (1, 2) if j % 2 == 0 else (6, 7)
                for kc in range(8):
                    S.op("pe", lambda e, kc=kc: e.matmul(bk(ba), lhsT=w_in3[:, kc, j * 128:(j + 1) * 128], rhs=hn[:, kc, :],
                                                         start=(kc == 0), stop=(kc == 7)),
                         reads=[HN3, "w_in3"], writes=["b%d" % ba])
                for kc in range(8):
                    S.op("pe", lambda e, kc=kc: e.matmul(bk(bg), lhsT=w_in3[:, kc, 512 + j * 128:512 + (j + 1) * 128], rhs=hn[:, kc, :],
                                                         start=(kc == 0), stop=(kc == 7)),
                         reads=[HN3, "w_in3"], writes=["b%d" % bg])

            def conv_pre(c):
                gp = c % 2
                x_to_hnT(c, xt3, xsb3, junkb3, hnT32[c % 2], ssx3, "hnT%d" % (c % 2))
                if c > 0:
                    S.op("pool", lambda e: e.tensor_copy(out=glu[gp][:, :, 0:32], in_=glu[1 - gp][:, :, 512:544]),
                         reads=["glu%d" % (1 - gp)], writes=["glu%d" % gp])
                else:
                    S.op("pool", lambda e: e.memset(glu[0][:, :, 0:32], 0.0), writes=["glu0"])
                conv_AG(c, 0)

            def conv_body(c):
                gp = c % 2
                for j in range(4):
                    ba, bg = (1, 2) if j % 2 == 0 else (6, 7)
                    sg = sig2[j % 2]
                    sgn = "sig%d" % (j % 2)
                    S.op("act", lambda e, bg=bg, sg=sg: e.activation(out=sg, in_=bk(bg), func=AF.Sigmoid), reads=["b%d" % bg], writes=[sgn])
                    S.op("dve", lambda e, j=j, ba=ba, sg=sg: e.tensor_tensor(out=glu[gp][:, j, 32:544], in0=bk(ba), in1=sg, op=ALU.mult),
                         reads=["b%d" % ba, sgn], writes=["glu%d" % gp])
                    if j + 1 < 4:
                        conv_AG(c, j + 1)
                    yb = 3 + (j % 2)
                    for k in range(31):
                        S.op("pe", lambda e, j=j, k=k, yb=yb: e.matmul(bk(yb), lhsT=dg[:, j, k, :], rhs=glu[gp][:, j, 2 + k:2 + k + 512],
                                                                       start=(k == 0), stop=(k == 30)),
                             reads=["glu%d" % gp, "dg"], writes=["b%d" % yb])
                    S.op("act", lambda e, j=j, yb=yb: e.activation(out=y[:, j, :], in_=bk(yb), func=AF.Identity, bias=col(C_CB + j)),
                         reads=["b%d" % yb, "cols"], writes=["y%d" % j])
                    S.op("act", lambda e, j=j: e.copy(out=ybf[:, j, :], in_=y[:, j, :]), reads=["y%d" % j], writes=["ybf"])

            def conv_tail(c):
                for j in range(4):
                    S.op("pe", lambda e, j=j: e.matmul(bk(5), lhsT=ones, rhs=ybf[:, j, :], start=(j == 0), stop=(j == 3)),
                         reads=["ybf", "ones"], writes=["b5"])
                S.op("act", lambda e: e.activation(out=mean, in_=bk(5), func=AF.Copy, scale=1.0 / 512), reads=["b5"],
                     writes=["mean"])
                for j in range(4):
                    S.op("dve", lambda e, j=j: e.tensor_tensor(out=y[:, j, :], in0=y[:, j, :], in1=mean, op=ALU.subtract),
                         reads=["y%d" % j, "mean"], writes=["y%d" % j])
                    S.op("act", lambda e, j=j: e.activation(out=ybf[:, j, :], in_=y[:, j, :], func=AF.Square),
                         reads=["y%d" % j], writes=["ybf"])
                for j in range(4):
                    S.op("pe", lambda e, j=j: e.matmul(bk(5), lhsT=ones, rhs=ybf[:, j, :], start=(j == 0), stop=(j == 3)),
                         reads=["ybf", "ones"], writes=["b5"])
                S.op("act", lambda e: e.activation(out=sd3, in_=bk(5), func=AF.Sqrt, scale=1.0 / 512, bias=EPS),
                     reads=["b5"], writes=["sd3"])
                S.op("dve", lambda e: e.reciprocal(out=sd3, in_=sd3), reads=["sd3"], writes=["sd3"])
                for j in range(4):
                    S.op("dve", lambda e, j=j: e.tensor_tensor(out=y[:, j, :], in0=y[:, j, :], in1=sd3, op=ALU.mult),
                         reads=["y%d" % j, "sd3"], writes=["y%d" % j])
                    S.op("act", lambda e, j=j: e.activation(out=y[:, j, :], in_=y[:, j, :], func=AF.Silu,
                                                            scale=col(C_LNG + j), bias=col(C_LNB + j)),
                         reads=["y%d" % j, "cols"], writes=["y%d" % j])
                    S.op("act", lambda e, j=j: e.activation(out=ybf[:, j, :], in_=y[:, j, :], func=AF.Square),
                         reads=["y%d" % j], writes=["ybf"])
                for j in range(4):
                    S.op("pe", lambda e, j=j: e.matmul(bk(5), lhsT=ones, rhs=ybf[:, j, :], start=(j == 0), stop=(j == 3)),
                         reads=["ybf", "ones"], writes=["b5"])
                S.op("act", lambda e: e.activation(out=sd3, in_=bk(5), func=AF.Sqrt, scale=1.0 / 512, bias=EPS),
                     reads=["b5"], writes=["sd3"])
                S.op("dve", lambda e: e.reciprocal(out=sd3, in_=sd3), reads=["sd3"], writes=["sd3"])
                for j in range(4):
                    S.op("dve", lambda e, j=j, c=c: e.scalar_tensor_tensor(
                        out=cnT[:, j, c * 512:(c + 1) * 512], in0=y[:, j, :], scalar=col(C_CG + j), in1=sd3,
                        op0=ALU.mult, op1=ALU.mult), reads=["y%d" % j, "sd3", "cols"], writes=["cnT"])
            conv_pre(0)
            for c in range(NCH):
                conv_body(c)
                if c + 1 < NCH:
                    conv_pre(c + 1)
                conv_tail(c)
        if debug and upto == 3:
            S.barrier()
            S.dma("sp", lambda e: e.dma_start(out=dbg_d["d_cnT"], in_=cnT.rearrange("p a b -> p (a b)")), "dbg", final=True)
            S.dma("sp", lambda e: e.dma_start(out=dbg_d["d_att"], in_=att.rearrange("p a b -> p (a b)")), "dbg", final=True)
        S.barrier()

        if upto >= 4:
            w_out = A.alloc([128, 8, D], BF16, {4})
            wq = A.alloc([128, 8, D], BF16, {4})
            kbd = A.alloc([128, 8, 256], BF16, {4})
            gw = A.alloc([128, 8, D], BF16, {4})
            pw = A.alloc([128, 2, D], BF16, {4})
            g_ffn = A.alloc([128, D], BF16, {4})
            g_b = A.alloc([128, D], F32, {4})
            g_fin = A.alloc([128, D], F32, {4})
            NB, GRP = 9, 2
            gball = A.alloc([128, NB, 2 * D], BF16, {4})
            gb = [gball[:, b, :] for b in range(NB)]
            dgs = [A.alloc([128, 128], BF16, {4}) for _ in range(4)]
            gl = A.alloc([128, 128], F32, {4})
            h1s = [A.alloc([128, D], F32, {4}) for _ in range(2)]
            xnbs = [A.alloc([128, D], BF16, {4}) for _ in range(2)]
            tmp = A.alloc([128, D], F32, {4})
            bfa = A.alloc([128, D], BF16, {4})
            xT = A.alloc([128, 8, 128], BF16, {4})
            mp = A.alloc([128, 256], F32, {4})
            mixT = mp.bitcast(BF16).rearrange("p (a b) -> p a b", a=4)
            tk = A.alloc([128, 2048], F32, {4})
            sc = tk.rearrange("p (a b) -> p a b", a=16)
            cand = tk.rearrange("p (h c) -> p h c", h=8)
            oh = tk.rearrange("p (h a b) -> p h a b", h=8, a=16)
            wk = A.alloc([128, 256], F32, {4})
            m16 = A.alloc([128, 16, 16], F32, {4})
            ix16 = A.alloc([128, 16, 16], U32, {4})
            ixf = ix16.bitcast(F32)
            best = A.alloc([128, 8, 16], F32, {4})
            posu = A.alloc([128, 8, 16], U32, {4})
            posf = A.alloc([128, 8, 16], F32, {4})
            ki4 = posu.bitcast(I32)
            k1f = A.alloc([128, 8, 16], F32, {4})
            k2f = A.alloc([128, 8, 16], F32, {4})
            e1 = A.alloc([128, 8, 16], F32, {4})
            e2 = posf
            eidxs = [A.alloc([128, 128], I32, {4}) for _ in range(2)]
            gate16s = [A.alloc([128, 8, 16], F32, {4}) for _ in range(2)]
            actv = A.alloc([128, 128], F32, {4})
            coef = A.alloc([128, 128], F32, {4})
            pt_ = mp
            pbf = A.alloc([128, 256], BF16, {4})
            pT = A.alloc([128, 2, 128], BF16, {4})
            gsum = A.alloc([128, 8], F32, {4})
            ss4 = scr[:, 24:40]

            def wload(dst, src, ch, name):
                S.dma("pool", lambda e: e.dma_start(out=dst, in_=src.rearrange("(k p) c -> p k c", p=128)), ch, writes=[name])
            wload(w_out, w_out_d, "w0", "w_out")
            wload(wq, wq_d, "w1", "wq")
            wload(gw, gw_d, "w2", "gw")
            wload(pw, pw_d, "w3", "pw")
            S.op("dve", lambda e: e.memset(kbd, 0.0), writes=["kbd"])
            S.dma("pool", lambda e: e.dma_start(out=kbd[0:64, :, 0:128], in_=keysT_d[0:64]), "w4", writes=["kbd"])
            S.dma("pool", lambda e: e.dma_start(out=kbd[64:128, :, 128:256], in_=keysT_d[64:128]), "w5", writes=["kbd"])
            S.dma("pool", lambda e: e.dma_start(out=g_ffn, in_=rows_d[0]), "c0", writes=["g_ffn"])
            S.dma("sp", lambda e: e.dma_start(out=g_b, in_=rows_d[1]), "c1", writes=["g_b"])
            S.dma("sp", lambda e: e.dma_start(out=g_fin, in_=rows_d[2]), "c2", writes=["g_fin"])

            class Rec:
                def __init__(self):
                    self.q = []
                def op(self, *a, **k):
                    c = k.pop("cost", 0.3)
                    self.q.append((lambda: S.op(*a, **k), c if a[0] == "dve" else 0.0, a[0]))
                def dma(self, *a, **k):
                    self.q.append((lambda: S.dma(*a, **k), 0.0, "dma"))
                def brk(self):
                    self.q.append(None)

            def rms_tile(X, src, srcname, k, n_feat, junk, junkname):
                X.op("act", lambda e: e.activation(out=junk, in_=src, func=AF.Square, accum_out=ss4[:, k:k + 1]),
                     reads=[srcname], writes=[junkname, "r%d_ss" % k])
                X.op("act", lambda e: e.activation(out=ss4[:, k + 8:k + 9], in_=ss4[:, k:k + 1], func=AF.Sqrt, scale=1.0 / n_feat, bias=EPS),
                     reads=["r%d_ss" % k], writes=["r%d_r" % k])
                X.brk()
                X.op("dve", lambda e: e.reciprocal(out=ss4[:, k + 8:k + 9], in_=ss4[:, k + 8:k + 9]), reads=["r%d_r" % k], writes=["r%d_r" % k])
                return ss4[:, k + 8:k + 9], "r%d_r" % k

            Tb = bkb(0).rearrange("p (a b) -> p a b", a=8)
            Tp = bkb(3).rearrange("p (a b) -> p a b", a=8)

            def A_ops(g):
                X = Rec()
                pp = g % 2
                H, Hn = h1s[pp], "h1_%d" % pp
                xnb, xnbn = xnbs[pp], "xnb_%d" % pp
                eidx, eidxn = eidxs[pp], "eidx_%d" % pp
                gate16, gaten = gate16s[pp], "gate_%d" % pp
                ts_ = slice(g * 128, (g + 1) * 128)
                X.dma("sp", lambda e: e.dma_start(out=H, in_=x_d[ts_, :]), "x0", writes=[Hn])
                att_t = att[:, g, :]
                ra, ran = rms_tile(X, att_t, "att", 0, 512, bfa[:, 0:512], "bfa")
                X.op("dve", lambda e: e.tensor_scalar(out=bfa[:, 0:512], in0=att_t, scalar1=ra, scalar2=None, op0=ALU.mult),
                     reads=["att", ran], writes=["bfa"])
                for kc in range(4):
                    X.op("pe", lambda e, kc=kc: e.transpose(out=Tb[:, kc, :], in_=bfa[:, kc * 128:(kc + 1) * 128], identity=ident),
                         reads=["bfa", "ident"], writes=["b0"])
                X.brk()
                X.op("dve", lambda e: e.tensor_tensor(out=mixT, in0=Tb[:, 0:4, :],
                                                      in1=col(C_GAO, 4).unsqueeze(2).to_broadcast([128, 4, 128]), op=ALU.mult),
                     reads=["b0", "cols"], writes=["mixT"], cost=0.6)
                for half in range(2):
                    for kc in range(8):
                        lhs = mixT[:, kc, :] if kc < 4 else cnT[:, kc - 4, ts_]
                        X.op("pe", lambda e, half=half, kc=kc, lhs=lhs: e.matmul(bk(3 + half), lhsT=lhs,
                                                                                 rhs=w_out[:, kc, half * 512:(half + 1) * 512],
                                                                                 start=(kc == 0), stop=(kc == 7)),
                             reads=["mixT", "cnT", "w_out"], writes=["b%d" % (3 + half)])
                X.brk()
                for half in range(2):
                    X.op("dve", lambda e, half=half: e.tensor_tensor(out=H[:, half * 512:(half + 1) * 512],
                                                                     in0=H[:, half * 512:(half + 1) * 512], in1=bk(3 + half), op=ALU.add),
                         reads=[Hn, "b%d" % (3 + half)], writes=[Hn], cost=0.7)
                r1, r1n = rms_tile(X, H, Hn, 1, D, tmp, "tmp")
                X.op("dve", lambda e: e.scalar_tensor_tensor(out=xnb, in0=H, scalar=r1, in1=g_ffn, op0=ALU.mult, op1=ALU.mult),
                     reads=[Hn, r1n, "g_ffn"], writes=[xnbn], cost=1.2)
                for kc in range(8):
                    X.op("pe", lambda e, kc=kc: e.transpose(out=Tb[:, kc, :], in_=xnb[:, kc * 128:(kc + 1) * 128], identity=ident),
                         reads=[xnbn, "ident"], writes=["b0"])
                X.brk()
                X.op("act", lambda e: e.copy(out=xT, in_=Tb), reads=["b0"], writes=["xT"])
                for half in range(2):
                    for kc in range(8):
                        X.op("pe", lambda e, half=half, kc=kc: e.matmul(bk(3 + half), lhsT=xT[:, kc, :],
                                                                        rhs=wq[:, kc, half * 512:(half + 1) * 512],
                                                                        start=(kc == 0), stop=(kc == 7)),
                             reads=["xT", "wq"], writes=["b%d" % (3 + half)])
                X.brk()
                for half in range(2):
                    X.op("act", lambda e, half=half: e.copy(out=bfa[:, half * 512:(half + 1) * 512], in_=bk(3 + half)),
                         reads=["b%d" % (3 + half)], writes=["bfa"])
                for kc in range(8):
                    X.op("pe", lambda e, kc=kc: e.transpose(out=Tb[:, kc, :], in_=bfa[:, kc * 128:(kc + 1) * 128], identity=ident),
                         reads=["bfa", "ident"], writes=["b0"])
                X.brk()
                X.op("dve", lambda e: e.tensor_copy(out=xT, in_=Tb), reads=["b0"], writes=["xT"], cost=1.0)
                for hh in range(8):
                    sbk = 4 + hh // 2
                    X.op("pe", lambda e, hh=hh, sbk=sbk: e.matmul(bk(sbk)[:, (hh % 2) * 256:(hh % 2 + 1) * 256], lhsT=xT[:, hh, :],
                                                                 rhs=kbd[:, hh, :], start=True, stop=True),
                         reads=["xT", "kbd"], writes=["b%d" % sbk])
                X.brk()
                for b4 in range(4):
                    X.op("act", lambda e, b4=b4: e.copy(out=tk[:, b4 * 512:(b4 + 1) * 512], in_=bk(4 + b4)),
                         reads=["b%d" % (4 + b4)], writes=["tk"])
                X.brk()
                for hc in range(16):
                    X.op("dve", lambda e, hc=hc: e.max(out=m16[:, hc, 0:8], in_=sc[:, hc, :]), reads=["tk"], writes=["m16"])
                    X.op("dve", lambda e, hc=hc: e.max_index(out=ix16[:, hc, 0:8], in_max=m16[:, hc, 0:8], in_values=sc[:, hc, :]),
                         reads=["tk", "m16"], writes=["ix16"])
                    X.op("dve", lambda e, hc=hc: e.match_replace(out=wk[:, 0:128], in_to_replace=m16[:, hc, 0:8],
                                                                 in_values=sc[:, hc, :], imm_value=-1e30),
                         reads=["tk", "m16"], writes=["wk"])
                    X.op("dve", lambda e, hc=hc: e.max(out=m16[:, hc, 8:16], in_=wk[:, 0:128]), reads=["wk"], writes=["m16"])
                    X.op("dve", lambda e, hc=hc: e.max_index(out=ix16[:, hc, 8:16], in_max=m16[:, hc, 8:16], in_values=wk[:, 0:128]),
                         reads=["wk", "m16"], writes=["ix16"])
                m4 = m16.rearrange("p (h c) k -> p h c k", c=2)
                X.op("dve", lambda e: e.tensor_tensor(out=cand.rearrange("p h (a b) -> p h a b", a=16),
                                                      in0=m4[:, :, 0, :].unsqueeze(3).to_broadcast([128, 8, 16, 16]),
                                                      in1=m4[:, :, 1, :].unsqueeze(2).to_broadcast([128, 8, 16, 16]), op=ALU.add),
                     reads=["m16", "tk"], writes=["tk"], cost=2.2)
                for hh in range(8):
                    X.op("dve", lambda e, hh=hh: e.max(out=best[:, hh, 0:8], in_=cand[:, hh, :]), reads=["tk"], writes=["best"])
                    X.op("dve", lambda e, hh=hh: e.max_index(out=posu[:, hh, 0:8], in_max=best[:, hh, 0:8], in_values=cand[:, hh, :]),
                         reads=["tk", "best"], writes=["posu"])
                    X.op("dve", lambda e, hh=hh: e.match_replace(out=wk, in_to_replace=best[:, hh, 0:8], in_values=cand[:, hh, :],
                                                                 imm_value=-1e30), reads=["tk", "best"], writes=["wk"])
                    X.op("dve", lambda e, hh=hh: e.max(out=best[:, hh, 8:16], in_=wk), reads=["wk"], writes=["best"])
                    X.op("dve", lambda e, hh=hh: e.max_index(out=posu[:, hh, 8:16], in_max=best[:, hh, 8:16], in_values=wk),
                         reads=["wk", "best"], writes=["posu"])
                X.op("dve", lambda e: e.tensor_copy(out=posf, in_=posu), reads=["posu"], writes=["posf"])
                X.op("dve", lambda e: e.tensor_copy(out=ixf, in_=ix16), reads=["ix16"], writes=["ix16"])
                X.op("dve", lambda e: e.tensor_scalar(out=k1f, in0=posf, scalar1=-7.5, scalar2=0.0625, op0=ALU.add, op1=ALU.mult),
                     reads=["posf"], writes=["k1f"])
                X.op("dve", lambda e: e.tensor_copy(out=ki4, in_=k1f), reads=["k1f", "posf"], writes=["posu"])
                X.op("dve", lambda e: e.tensor_copy(out=k1f, in_=ki4), reads=["posu"], writes=["k1f"])
                X.op("dve", lambda e: e.scalar_tensor_tensor(out=k2f, in0=k1f, scalar=-16.0, in1=posf, op0=ALU.mult, op1=ALU.add),
                     reads=["k1f", "posf"], writes=["k2f"])
                ix4 = ixf.rearrange("p (h c) k -> p h c k", c=2)
                io_b = iota16.unsqueeze(1).unsqueeze(1).to_broadcast([128, 8, 16, 16])
                for kf, cc, eo, nm in ((k1f, 0, e1, "e1"), (k2f, 1, e2, "posf")):
                    X.op("dve", lambda e, kf=kf: e.tensor_tensor(out=oh, in0=kf.unsqueeze(3).to_broadcast([128, 8, 16, 16]),
                                                                 in1=io_b, op=ALU.is_equal),
                         reads=["k1f", "k2f", "iota16", "tk"], writes=["tk"], cost=2.2)
                    X.op("dve", lambda e, cc=cc: e.tensor_tensor(out=oh, in0=oh,
                                                                 in1=ix4[:, :, cc, :].unsqueeze(2).to_broadcast([128, 8, 16, 16]),
                                                                 op=ALU.mult), reads=["tk", "ix16"], writes=["tk"], cost=2.2)
                    X.op("dve", lambda e, eo=eo: e.reduce_sum(out=eo, in_=oh, axis=AX.X), reads=["tk"], writes=[nm], cost=2.2)
                X.op("dve", lambda e: e.scalar_tensor_tensor(out=e1, in0=e1, scalar=128.0, in1=e2, op0=ALU.mult, op1=ALU.add),
                     reads=["e1", "posf"], writes=["e1"])
                X.op("dve", lambda e: e.tensor_copy(out=eidx, in_=e1.rearrange("p h k -> p (h k)")), reads=["e1"], writes=[eidxn])
                X.op("dve", lambda e: e.tensor_tensor(out=gate16, in0=best, in1=best[:, :, 0:1].to_broadcast([128, 8, 16]),
                                                      op=ALU.subtract), reads=["best"], writes=[gaten])
                X.op("act", lambda e: e.activation(out=gate16, in_=gate16, func=AF.Exp), reads=[gaten], writes=[gaten])
                X.brk()
                X.op("dve", lambda e: e.reduce_sum(out=gsum, in_=gate16, axis=AX.X), reads=[gaten], writes=["gsum"])
                X.op("dve", lambda e: e.reciprocal(out=gsum, in_=gsum), reads=["gsum"], writes=["gsum"])
                X.op("dve", lambda e: e.tensor_tensor(out=gate16, in0=gate16, in1=gsum.unsqueeze(2).to_broadcast([128, 8, 16]),
                                                      op=ALU.mult), reads=[gaten, "gsum"], writes=[gaten])
                return X.q

            TABS = ["tabu%d" % k for k in range(8)] + ["tabv%d" % k for k in range(8)]

            def B_emit(g, side, per_group):
                pp = g % 2
                H, Hn = h1s[pp], "h1_%d" % pp
                xnb, xnbn = xnbs[pp], "xnb_%d" % pp
                eidx, eidxn = eidxs[pp], "eidx_%d" % pp
                gflat, gaten = gate16s[pp].rearrange("p h k -> p (h k)"), "gate_%d" % pp

                def consume(k):
                    gs = slice(k * GRP, (k + 1) * GRP)
                    S.op("act", lambda e: e.activation(out=gl[:, gs], in_=actv[:, gs], func=AF.Gelu), reads=["actv"], writes=["gl"])
                    for s2 in range(k * GRP, (k + 1) * GRP):
                        b2 = s2 % NB
                        dgi = s2 % 4
                        S.op("act", lambda e, s2=s2: e.mul(out=coef[:, s2:s2 + 1], in_=gl[:, s2:s2 + 1], mul=gflat[:, s2:s2 + 1]),
                             reads=["gl", gaten], writes=["coef"])
                        S.op("act", lambda e, s2=s2, dgi=dgi: e.activation(out=dgs[dgi], in_=ident, func=AF.Copy, scale=coef[:, s2:s2 + 1]),
                             reads=["ident", "coef"], writes=["dg%d" % dgi])
                        for half in range(2):
                            S.op("pe", lambda e, s2=s2, b2=b2, dgi=dgi, half=half: e.matmul(
                                bk(1 + half), lhsT=dgs[dgi], rhs=gb[b2][:, D + half * 512:D + (half + 1) * 512],
                                start=(s2 == 0), stop=(s2 == 127)),
                                reads=["dg%d" % dgi, "gb%d" % b2], writes=["b%d" % (1 + half)])

                for s_ in range(128):
                    b = s_ % NB
                    S.dma("pool", lambda e, s_=s_, b=b: e.indirect_dma_start(
                        out=gb[b], out_offset=None, in_=tab_d, in_offset=bass.IndirectOffsetOnAxis(ap=eidx[:, s_:s_ + 1], axis=0)),
                        "g%d" % b, reads=[eidxn] + TABS, writes=["gb%d" % b])
                    if s_ % 2 == 0 or not DOT_SPLIT:
                        S.op("dve", lambda e, s_=s_, b=b: e.scalar_tensor_tensor(out=gb[b][:, 0:D], in0=gb[b][:, 0:D], scalar=1.0, in1=xnb,
                                                                                 op0=ALU.mult, op1=ALU.mult, accum_out=actv[:, s_:s_ + 1]),
                             reads=["gb%d" % b, xnbn], writes=["gb%d" % b, "actv"])
                    else:
                        S.op("dve", lambda e, b=b: e.tensor_tensor(out=gb[b][:, 0:D], in0=gb[b][:, 0:D], in1=xnb, op=ALU.mult),
                             reads=["gb%d" % b, xnbn], writes=["gb%d" % b])
                        S.op("act", lambda e, s_=s_, b=b: e.activation(out=gb[b][:, 0:D], in_=gb[b][:, 0:D], func=AF.Copy,
                                                                       accum_out=actv[:, s_:s_ + 1]),
                             reads=["gb%d" % b], writes=["gb%d" % b, "actv"])
                    if (s_ + 1) % GRP == 0:
                        consume(s_ // GRP)
                        dve_c, other = 0.0, 0
                        while side:
                            it = side[0]
                            if it is None:
                                side.pop(0)
                                if dve_c >= 0.4 * per_group or other >= 4:
                                    break
                                continue
                            if it[2] == "dve":
                                if dve_c > 0 and dve_c + it[1] > per_group:
                                    break
                            elif other >= 10:
                                break
                            side.pop(0)
                            it[0]()
                            if it[2] == "dve":
                                dve_c += it[1]
                            else:
                                other += 1
                while side:
                    it = side.pop(0)
                    if it is not None:
                        it[0]()
                for half in range(2):
                    S.op("dve", lambda e, half=half: e.tensor_tensor(out=H[:, half * 512:(half + 1) * 512],
                                                                     in0=H[:, half * 512:(half + 1) * 512], in1=bk(1 + half), op=ALU.add),
                         reads=[Hn, "b%d" % (1 + half)], writes=[Hn])

            def C_ops(g):
                X = Rec()
                pp = g % 2
                H, Hn = h1s[pp], "h1_%d" % pp
                ts_ = slice(g * 128, (g + 1) * 128)
                X.dma("sp", lambda e: e.dma_start(out=pt_, in_=p_d[ts_, :]), "x1", writes=["mixT"])
                r2, r2n = rms_tile(X, H, Hn, 2, D, tmp, "tmp")
                X.op("dve", lambda e: e.tensor_scalar(out=bfa, in0=H, scalar1=r2, scalar2=None, op0=ALU.mult),
                     reads=[Hn, r2n], writes=["bfa"], cost=0.8)
                X.op("act", lambda e: e.copy(out=pbf, in_=pt_), reads=["mixT"], writes=["pbf"])
                for kc in range(8):
                    X.op("pe", lambda e, kc=kc: e.transpose(out=Tb[:, kc, :], in_=bfa[:, kc * 128:(kc + 1) * 128], identity=ident),
                         reads=["bfa", "ident"], writes=["b0"])
                for kc in range(2):
                    X.op("pe", lambda e, kc=kc: e.transpose(out=Tp[:, kc, :], in_=pbf[:, kc * 128:(kc + 1) * 128], identity=ident),
                         reads=["pbf", "ident"], writes=["b3"])
                X.brk()
                X.op("dve", lambda e: e.tensor_tensor(out=xT, in0=Tb, in1=col(C_GPL, 8).unsqueeze(2).to_broadcast([128, 8, 128]),
                                                      op=ALU.mult), reads=["b0", "cols"], writes=["xT"], cost=1.0)
                X.op("act", lambda e: e.copy(out=pT, in_=Tp[:, 0:2, :]), reads=["b3"], writes=["pT"])
                for half in range(2):
                    hs = slice(half * 512, (half + 1) * 512)
                    gbk = 5 + half
                    for kc in range(8):
                        X.op("pe", lambda e, hs=hs, kc=kc, gbk=gbk: e.matmul(bk(gbk), lhsT=xT[:, kc, :], rhs=gw[:, kc, hs],
                                                                             start=(kc == 0), stop=(kc == 7)),
                             reads=["xT", "gw"], writes=["b%d" % gbk])
                X.brk()
                for half in range(2):
                    hs = slice(half * 512, (half + 1) * 512)
                    gbk = 5 + half
                    X.op("dve", lambda e, hs=hs, gbk=gbk: e.tensor_tensor(out=tmp[:, hs], in0=bk(gbk), in1=g_b[:, hs], op=ALU.add),
                         reads=["b%d" % gbk, "g_b", "tmp"], writes=["tmp"], cost=0.7)
                    X.op("act", lambda e, hs=hs: e.activation(out=tmp[:, hs], in_=tmp[:, hs], func=AF.Sigmoid), reads=["tmp"],
                         writes=["tmp"])
                    for kc in range(2):
                        X.op("pe", lambda e, hs=hs, kc=kc, gbk=gbk: e.matmul(bk(gbk), lhsT=pT[:, kc, :], rhs=pw[:, kc, hs],
                                                                             start=(kc == 0), stop=(kc == 1)),
                             reads=["pT", "pw"], writes=["b%d" % gbk])
                X.brk()
                for half in range(2):
                    hs = slice(half * 512, (half + 1) * 512)
                    gbk = 5 + half
                    X.op("dve", lambda e, hs=hs, gbk=gbk: e.tensor_tensor(out=tmp[:, hs], in0=tmp[:, hs], in1=bk(gbk), op=ALU.mult),
                         reads=["b%d" % gbk, "tmp"], writes=["tmp"], cost=0.7)
                X.op("dve", lambda e: e.tensor_tensor(out=H, in0=H, in1=tmp, op=ALU.add), reads=[Hn, "tmp"], writes=[Hn], cost=1.1)
                r3, r3n = rms_tile(X, H, Hn, 3, D, tmp, "tmp")
                X.op("dve", lambda e: e.scalar_tensor_tensor(out=tmp, in0=H, scalar=r3, in1=g_fin, op0=ALU.mult, op1=ALU.mult),
                     reads=[Hn, r3n, "g_fin", "tmp"], writes=["tmp"], cost=1.2)
                X.dma("sp", lambda e: e.dma_start(out=out_d[ts_, :], in_=tmp), "st", reads=["tmp"], final=True)
                return X.q

            for it in A_ops(0):
                if it is not None:
                    it[0]()
            for g in range(NT):
                side = []
                if g > 0:
                    side += C_ops(g - 1)
                if g + 1 < NT:
                    side += A_ops(g + 1)
                tot_c = sum(it[1] for it in side if it is not None)
                n_other = sum(1 for it in side if it is not None and it[2] != "dve")
                per_group = 1.1 * tot_c / max(8, 128 // GRP - 3 - n_other // 14)
                B_emit(g, side, per_group)
            for it in C_ops(NT - 1):
                if it is not None:
                    it[0]()
        else:
            z = A.alloc([128, D], F32, {4})
            S.op("dve", lambda e: e.memset(z, 0.0), writes=["z"])
            S.dma("sp", lambda e: e.dma_start(out=out_d[0:128, :], in_=z), "st", reads=["z"], final=True)

        stats = S.emit()
        print("program ops per engine:", stats)
    return nc


def make_in_maps(inputs):
    f = np.float32
    g = lambda k: np.asarray(inputs[k])
    x = g("x").astype(f, copy=False)
    p = g("p").astype(f, copy=False)[0]
    pos = g("positions").astype(np.int32, copy=False)

    def colmaj(v):
        v = np.asarray(v, f).reshape(-1, 128)
        return np.ascontiguousarray(v.T)

    cols = np.zeros((128, NCOL), f)
    cols[:, C_GA:C_GA + 8] = colmaj(g("attn_norm")[0])
    cols[:, C_GQ:C_GQ + 4] = colmaj(g("q_norm")[0])
    cols[:, C_GKV:C_GKV + 2] = colmaj(g("kv_norm")[0])
    cols[:, C_CB:C_CB + 4] = colmaj(g("conv_b")[0])
    cols[:, C_LNG:C_LNG + 4] = colmaj(g("conv_ln_g")[0])
    cols[:, C_LNB:C_LNB + 4] = colmaj(g("conv_ln_b")[0])
    cols[:, C_CG:C_CG + 4] = colmaj(g("conv_out_norm")[0])
    cols[:, C_GAO:C_GAO + 4] = colmaj(g("attn_out_norm")[0])
    cols[:, C_GPL:C_GPL + 8] = colmaj(g("pl_norm")[0])
    half = 16
    freqs = (np.float32(10000.0) ** (-np.arange(half, dtype=f) / np.float32(half))).astype(f)
    for pp in range(64, 96):
        cols[pp, C_FREQ] = freqs[(pp - 64) % 16]
    cw = np.asarray(g("conv_w")[0], f)
    cols[:, C_CW:C_CW + 124] = cw.reshape(31, 4, 128).transpose(2, 1, 0).reshape(128, 124)
    rows = np.stack([np.broadcast_to(np.asarray(g(k), f).reshape(-1)[None, :], (128, D))
                     for k in ("ffn_norm", "pl_gate_b", "final_norm")]).astype(f)
    rows = np.ascontiguousarray(rows)
    cst = np.zeros((3, 128, 128), f)
    cst[0] = np.eye(128, dtype=f)
    cst[1] = np.triu(np.ones((128, 128), f))
    cst[2, :, 0:16] = np.arange(16, dtype=f)[None, :]
    keysT = np.ascontiguousarray(np.asarray(g("peer_keys")[0], f).transpose(1, 3, 0, 2).reshape(128, 8, 128))
    shared = {
        "w_in": np.ascontiguousarray(g("w_in")[0], f), "w_uq": np.ascontiguousarray(g("w_uq")[0], f),
        "w_ukv": np.ascontiguousarray(g("w_ukv")[0], f), "w_out": np.ascontiguousarray(g("w_out")[0], f),
        "peer_wq": np.ascontiguousarray(g("peer_wq")[0], f), "keysT": keysT,
        "peer_u": np.ascontiguousarray(g("peer_u")[0], f), "peer_v": np.ascontiguousarray(g("peer_v")[0], f),
        "pl_gate_w": np.ascontiguousarray(g("pl_gate_w")[0], f), "pl_proj": np.ascontiguousarray(g("pl_proj")[0], f),
        "cols": cols, "rows": rows, "cst": cst,
    }
    maps = []
    for b in range(8):
        m = dict(shared)
        m["x"] = np.ascontiguousarray(x[b])
        m["p"] = np.ascontiguousarray(p[b])
        m["pos"] = np.ascontiguousarray(np.broadcast_to(pos[b][None, :], (32, T))).astype(np.int32)
        maps.append(m)
    return maps


_NC_CACHE = {}


def kernel(**inputs):
    maps = make_in_maps(inputs)
    if "nc" not in _NC_CACHE:
        _NC_CACHE["nc"] = build_program()
    nc = _NC_CACHE["nc"]
    res = run_bass_kernel_spmd(nc, maps, core_ids=list(range(8)))
    out = np.stack([np.asarray(r["out"], np.float32) for r in res.results], axis=0)
    return out.reshape(8, T, D)
```
